# Optimizing a Trainium2 kernel written in Bass

```python
import math
import jax, jax.numpy as jnp
from jax import lax
import numpy as np

D_MODEL = 1024
BATCH = 16
SEQ = 2048
DEPTH = 2

POOL_GROUPS = 4
POOL_WINDOWS = (2, 4, 8, 16)
POOL_WIDTH = D_MODEL // 2
POOL_GROUP_DIM = POOL_WIDTH // POOL_GROUPS
N_HEADS = D_MODEL // 128
HEAD_DIM = 64
V_HEAD_DIM = 2 * HEAD_DIM
QK_WIDTH = N_HEADS * 2 * HEAD_DIM
V_WIDTH = N_HEADS * V_HEAD_DIM
ROPE_THETA = 500000.0
ROT_DIM = HEAD_DIM // 4
Q_BLOCK = 128
N_BRANCHES = 2
IN_WIDTH = POOL_WIDTH + 2 * QK_WIDTH + V_WIDTH + N_BRANCHES * D_MODEL
N_EXPERTS = 32
TOP_K = 4
D_FF = D_MODEL
SWIGLU_ALPHA = 1.702
SWIGLU_LIMIT = 7.0
EXPERT_BLOCK = 128
LN_EPS = 1e-5
DEEPNORM_ALPHA = (2 * DEPTH) ** 0.25
DEEPNORM_BETA = (8 * DEPTH) ** -0.25

kernel_name = 'pool_diffattn_gated_moe_deepnorm'


def layer_norm(x, g, b):
    xf = x.astype(jnp.float32)
    mu = jnp.mean(xf, axis=-1, keepdims=True)
    var = jnp.mean(jnp.square(xf - mu), axis=-1, keepdims=True)
    return ((xf - mu) * lax.rsqrt(var + LN_EPS) * g + b).astype(x.dtype)


def rotary_tables(positions):
    inv_freq = ROPE_THETA ** (-jnp.arange(0, ROT_DIM, 2, dtype=jnp.float32) / ROT_DIM)
    ang = positions.astype(jnp.float32)[..., None] * inv_freq
    return jnp.cos(ang)[:, :, None, None, :], jnp.sin(ang)[:, :, None, None, :]


def partial_rotary(t, cos, sin):
    half = ROT_DIM // 2
    tf = t[..., :ROT_DIM].astype(jnp.float32)
    t1, t2 = tf[..., :half], tf[..., half:]
    rot = jnp.concatenate([t1 * cos - t2 * sin, t2 * cos + t1 * sin], axis=-1)
    return jnp.concatenate([rot.astype(t.dtype), t[..., ROT_DIM:]], axis=-1)


def multiscale_pool(u, w_grp, scale):
    B, S, _ = u.shape
    uf = u.reshape(B, S, POOL_GROUPS, POOL_GROUP_DIM).astype(jnp.float32)
    c = jnp.cumsum(uf, axis=1)
    means = []
    for g, w in enumerate(POOL_WINDOWS):
        cg = c[:, :, g]
        prev = jnp.pad(cg[:, :S - w], ((0, 0), (w, 0), (0, 0)))
        cnt = jnp.minimum(jnp.arange(1, S + 1), w).astype(jnp.float32)[None, :, None]
        means.append((cg - prev) / cnt)
    d = (jnp.stack(means, axis=2) - uf).astype(u.dtype)
    y = jnp.einsum('bsgc,gcd->bsgd', d, w_grp).reshape(B, S, POOL_WIDTH)
    return y * scale


def diff_attention(q, k, v, lam):
    B, S = q.shape[0], q.shape[1]
    nb = S // Q_BLOCK
    qb = q.reshape(B, nb, Q_BLOCK, N_HEADS, 2, HEAD_DIM).transpose(1, 0, 3, 4, 2, 5)
    kt = k.transpose(0, 2, 3, 1, 4)
    vt = v.transpose(0, 2, 1, 3)
    key_pos = jnp.arange(S)
    scale = HEAD_DIM ** -0.5

    def block(args):
        qblk, i = args
        s = jnp.einsum('bhmqd,bhmkd->bhmqk', qblk, kt).astype(jnp.float32) * scale
        qpos = i * Q_BLOCK + jnp.arange(Q_BLOCK)
        mask = key_pos[None, :] <= qpos[:, None]
        p = jax.nn.softmax(jnp.where(mask, s, -jnp.inf), axis=-1)
        a = (p[:, :, 0] - lam * p[:, :, 1]).astype(v.dtype)
        return jnp.einsum('bhqk,bhkv->bhqv', a, vt)

    o = lax.map(block, (qb, jnp.arange(nb)))
    return o.transpose(1, 0, 3, 2, 4).reshape(B, S, N_HEADS, V_HEAD_DIM)


def hybrid_mixer(x, cos, sin, layer, w_in, pool_w, pool_scale, w_pool_branch,
                 w_attn_branch, lq1, lk1, lq2, lk2, subln_w, w_out):
    B, S, D = x.shape
    proj = x @ w_in
    splits = [int(c) for c in np.cumsum([POOL_WIDTH, QK_WIDTH, QK_WIDTH, V_WIDTH])]
    u_pool, q, k, v, gates = jnp.split(proj, splits, axis=-1)
    y_pool = multiscale_pool(u_pool, pool_w, pool_scale) @ w_pool_branch
    q = partial_rotary(q.reshape(B, S, N_HEADS, 2, HEAD_DIM), cos, sin)
    k = partial_rotary(k.reshape(B, S, N_HEADS, 2, HEAD_DIM), cos, sin)
    v = v.reshape(B, S, N_HEADS, V_HEAD_DIM)
    lambda_init = 0.8 - 0.6 * math.exp(-0.3 * layer)
    lam = (jnp.exp(jnp.sum(lq1.astype(jnp.float32) * lk1.astype(jnp.float32)))
           - jnp.exp(jnp.sum(lq2.astype(jnp.float32) * lk2.astype(jnp.float32)))
           + lambda_init)
    o = diff_attention(q, k, v, lam).astype(jnp.float32)
    o = o * lax.rsqrt(jnp.mean(jnp.square(o), axis=-1, keepdims=True) + LN_EPS)
    o = (o * subln_w * (1.0 - lambda_init)).astype(x.dtype)
    y_attn = o.reshape(B, S, V_WIDTH) @ w_attn_branch
    g = jax.nn.sigmoid(gates.astype(jnp.float32)).astype(x.dtype).reshape(B, S, N_BRANCHES, D)
    merged = g[:, :, 0] * y_pool + g[:, :, 1] * y_attn
    return merged @ w_out


def moe_ffn(x2d, w_router, b_router, w_gu, b_gu, w_down, b_down):
    T, D = x2d.shape
    logits = (x2d @ w_router + b_router).astype(jnp.float32)
    top_val, top_idx = lax.top_k(logits, TOP_K)
    gate = jax.nn.softmax(top_val, axis=-1).astype(x2d.dtype)
    A = T * TOP_K
    flat_e = top_idx.reshape(A)
    order = jnp.argsort(flat_e)
    sorted_e = flat_e[order]
    counts = jnp.bincount(flat_e, length=N_EXPERTS)
    padded = (counts + EXPERT_BLOCK - 1) // EXPERT_BLOCK * EXPERT_BLOCK
    start = jnp.cumsum(counts) - counts
    pend = jnp.cumsum(padded)
    pstart = pend - padded
    dest = pstart[sorted_e] + jnp.arange(A) - start[sorted_e]
    n_blocks = -(-A // EXPERT_BLOCK) + N_EXPERTS
    rows = n_blocks * EXPERT_BLOCK
    row_tok = jnp.zeros((rows,), jnp.int32).at[dest].set((order // TOP_K).astype(jnp.int32))
    row_gate = jnp.zeros((rows,), x2d.dtype).at[dest].set(gate.reshape(A)[order])
    block_e = jnp.minimum(jnp.searchsorted(pend, jnp.arange(n_blocks) * EXPERT_BLOCK, side='right'),
                          N_EXPERTS - 1)
    xs = x2d[row_tok].reshape(n_blocks, EXPERT_BLOCK, D)

    def expert_block(args):
        xb, e = args
        h = xb @ w_gu[e] + b_gu[e]
        hg = jnp.minimum(h[:, :D_FF], SWIGLU_LIMIT)
        hu = jnp.clip(h[:, D_FF:], -SWIGLU_LIMIT, SWIGLU_LIMIT)
        act = (hu + 1.0) * hg * jax.nn.sigmoid(SWIGLU_ALPHA * hg)
        return act @ w_down[e] + b_down[e]

    ys = lax.map(expert_block, (xs, block_e)).reshape(rows, D)
    return jnp.zeros_like(x2d).at[row_tok].add(row_gate[:, None] * ys)


def setup_inputs(seed: int = 0) -> dict:
    key = jax.random.key(seed)
    ks = jax.random.split(key, 24)

    def nrm(k, shape, scale):
        return jax.random.normal(k, shape, jnp.float32) * scale

    L, D, E, F = DEPTH, D_MODEL, N_EXPERTS, D_FF
    v_lo = POOL_WIDTH + 2 * QK_WIDTH
    w_in = nrm(ks[2], (L, D, IN_WIDTH), D ** -0.5)
    w_in = w_in.at[:, :, v_lo:v_lo + V_WIDTH].multiply(DEEPNORM_BETA)
    return {
        'x': nrm(ks[0], (BATCH, SEQ, D), 1.0),
        'positions': jnp.broadcast_to(jnp.arange(SEQ, dtype=jnp.int32), (BATCH, SEQ)),
        'w_in': w_in,
        'pool_w': nrm(ks[3], (L, POOL_GROUPS, POOL_GROUP_DIM, POOL_GROUP_DIM), POOL_GROUP_DIM ** -0.5),
        'pool_scale': 1.0 + nrm(ks[4], (L, POOL_WIDTH), 0.02),
        'w_pool_branch': nrm(ks[5], (L, POOL_WIDTH, D), POOL_WIDTH ** -0.5),
        'w_attn_branch': nrm(ks[6], (L, V_WIDTH, D), V_WIDTH ** -0.5),
        'lambda_q1': nrm(ks[7], (L, HEAD_DIM), 0.1),
        'lambda_k1': nrm(ks[8], (L, HEAD_DIM), 0.1),
        'lambda_q2': nrm(ks[9], (L, HEAD_DIM), 0.1),
        'lambda_k2': nrm(ks[10], (L, HEAD_DIM), 0.1),
        'subln_w': 1.0 + nrm(ks[11], (L, V_HEAD_DIM), 0.02),
        'w_out': nrm(ks[12], (L, D, D), D ** -0.5 * DEEPNORM_BETA),
        'ln1_g': 1.0 + nrm(ks[13], (L, D), 0.02),
        'ln1_b': nrm(ks[14], (L, D), 0.02),
        'w_router': nrm(ks[15], (L, D, E), D ** -0.5),
        'b_router': nrm(ks[16], (L, E), 0.01),
        'w_gu': nrm(ks[17], (L, E, D, 2 * F), D ** -0.5 * DEEPNORM_BETA),
        'b_gu': nrm(ks[18], (L, E, 2 * F), 0.02),
        'w_down': nrm(ks[19], (L, E, F, D), F ** -0.5 * DEEPNORM_BETA),
        'b_down': nrm(ks[20], (L, E, D), 0.02),
        'ln2_g': 1.0 + nrm(ks[21], (L, D), 0.02),
        'ln2_b': nrm(ks[22], (L, D), 0.02),
    }


def reference(x, positions, w_in, pool_w, pool_scale, w_pool_branch, w_attn_branch,
              lambda_q1, lambda_k1, lambda_q2, lambda_k2, subln_w, w_out, ln1_g, ln1_b,
              w_router, b_router, w_gu, b_gu, w_down, b_down, ln2_g, ln2_b):
    B, S, D = x.shape
    cos, sin = rotary_tables(positions)
    for l in range(DEPTH):
        mix = hybrid_mixer(x, cos, sin, l, w_in[l], pool_w[l], pool_scale[l], w_pool_branch[l],
                           w_attn_branch[l], lambda_q1[l], lambda_k1[l], lambda_q2[l],
                           lambda_k2[l], subln_w[l], w_out[l])
        x = layer_norm(DEEPNORM_ALPHA * x + mix, ln1_g[l], ln1_b[l])
        ffn = moe_ffn(x.reshape(B * S, D), w_router[l], b_router[l], w_gu[l], b_gu[l],
                      w_down[l], b_down[l]).reshape(B, S, D)
        x = layer_norm(DEEPNORM_ALPHA * x + ffn, ln2_g[l], ln2_b[l])
    return x
```

```python
import math
from contextlib import ExitStack

import numpy as np
import ml_dtypes

import concourse.bass as bass
import concourse.mybir as mybir
from concourse.bass_utils import run_bass_kernel_spmd

F32 = mybir.dt.float32
BF16 = mybir.dt.bfloat16
I32 = mybir.dt.int32
AF = mybir.ActivationFunctionType
ALU = mybir.AluOpType
AX = mybir.AxisListType

NCORES = 8
D = 1024
S = 2048
NSEQ = 2
T = NSEQ * S
NT = T // 128
L = 2
NH = 8
E = 32
TOPK = 4
CAP = 768
NR = CAP // 128
LN_EPS = 1e-5
ALPHA = (2 * L) ** 0.25
ROPE_THETA = 500000.0
SW_ALPHA = 1.702
SW_LIM = 7.0
SAME_ENG_SYNC = True


class _Op:
    __slots__ = ("eng", "kind", "fn", "reads", "writes", "deps", "need", "sem", "val", "prev")

    def __init__(self, eng, kind, fn, reads, writes):
        self.eng, self.kind, self.fn = eng, kind, fn
        self.reads, self.writes = tuple(reads), tuple(writes)
        self.deps, self.need, self.sem, self.val, self.prev = (), False, None, 0, 0


class Prog:
    ENGS = ("pe", "act", "dve", "pool", "sp")

    def __init__(self, nc, es):
        self.nc = nc
        self.sems = []
        self.csem = {}
        for e in self.ENGS:
            self.csem[e] = len(self.sems)
            self.sems.append(es.enter_context(nc.semaphore("c_" + e)))
        self.ccnt = {e: 0 for e in self.ENGS}
        self.lanes = {}
        for q, n in (("sp", 14), ("pool", 14), ("act", 2)):
            self.lanes[q] = []
            for i in range(n):
                self.lanes[q].append(len(self.sems))
                self.sems.append(es.enter_context(nc.semaphore("l_%s%d" % (q, i))))
        self.lcnt = {}
        for q in self.lanes:
            for s in self.lanes[q]:
                self.lcnt[s] = 0
        self.lnext = {q: 0 for q in self.lanes}
        self.waited = {e: {} for e in self.ENGS}
        self.ops = []

    def op(self, eng, kind, fn, reads=(), writes=()):
        self.ops.append(_Op(eng, kind, fn, reads, writes))

    def flush(self):
        ops, self.ops = self.ops, []
        if not ops:
            return
        last_w, readers = {}, {}
        for i, op in enumerate(ops):
            deps = set()
            for k in op.reads:
                if k in last_w:
                    deps.add(last_w[k])
                if isinstance(k, tuple) and k[0] == "ps":
                    for r in readers.get(k, ()):
                        if ops[r].eng != op.eng:
                            deps.add(r)
            for k in op.writes:
                if k in last_w:
                    deps.add(last_w[k])
                for r in readers.get(k, ()):
                    deps.add(r)
            deps.discard(i)
            fd = []
            for d in deps:
                p = ops[d]
                if p.kind == "c" and op.kind == "c" and p.eng == op.eng:
                    if op.eng == "pe" or not SAME_ENG_SYNC:
                        continue
                fd.append(d)
            op.deps = fd
            for d in fd:
                ops[d].need = True
            for k in op.reads:
                readers.setdefault(k, []).append(i)
            for k in op.writes:
                last_w[k] = i
                readers[k] = []
        lastc = {}
        for i, op in enumerate(ops):
            if op.kind == "c" and op.fn is not None:
                lastc[op.eng] = i
        for i in lastc.values():
            ops[i].need = True
        for op in ops:
            if op.kind == "c":
                if op.need:
                    self.ccnt[op.eng] += 1
                    op.sem, op.val = self.csem[op.eng], self.ccnt[op.eng]
            else:
                q = self.lanes[op.eng]
                s = q[self.lnext[op.eng] % len(q)]
                self.lnext[op.eng] += 1
                op.prev = self.lcnt[s]
                self.lcnt[s] += 16
                op.sem, op.val = s, self.lcnt[s]
        final = {self.csem[e]: self.ccnt[e] for e in self.ENGS}
        final.update(self.lcnt)
        with self.nc.Block() as blk:
            decos = (("pe", blk.tensor), ("act", blk.scalar), ("dve", blk.vector),
                     ("pool", blk.gpsimd), ("sp", blk.sync))
            for ename, deco in decos:
                def body(eng, ename=ename):
                    self._emit(ename, eng, ops, final)
                deco(body)

    def _wait(self, ename, eng, s, v):
        if v <= 0:
            return
        w = self.waited[ename]
        if w.get(s, 0) >= v:
            return
        eng.wait_ge(self.sems[s], v)
        w[s] = v

    def _emit(self, ename, eng, ops, final):
        for op in ops:
            if op.eng != ename:
                continue
            need = {}
            for d in op.deps:
                p = ops[d]
                if need.get(p.sem, 0) < p.val:
                    need[p.sem] = p.val
            if op.kind == "d" and op.prev > 0:
                if need.get(op.sem, 0) < op.prev:
                    need[op.sem] = op.prev
            for s, v in need.items():
                self._wait(ename, eng, s, v)
            if op.fn is None:
                continue
            ins = op.fn(eng)
            if op.kind == "d":
                ins.then_inc(self.sems[op.sem], 16)
            elif op.need:
                ins.then_inc(self.sems[op.sem], 1)
        for s, v in final.items():
            self._wait(ename, eng, s, v)


class StopBuild(Exception):
    pass


class Builder:
    def __init__(self, n_layers=L, debug=False, stop_after=None):
        self.stop_after = stop_after
        self.n_layers = n_layers
        self.debug = debug
        self.nc = bass.Bass("TRN2", target_bir_lowering=False)
        self.es = ExitStack()

    def phase_end(self, name):
        if self.debug:
            print("phase", name, "sbuf cur", self._cur, "peak", self._peak)
        self._peak = self._cur
        self.P.flush()
        if self.stop_after == name:
            raise StopBuild()

    def C(self, eng, fn, r=(), w=()):
        self.P.op(eng, "c", fn, r, w)

    def Dq(self, q, fn, r=(), w=()):
        self.P.op(q, "d", fn, r, w)

    def sb(self, es, name, shape, dt):
        self._uid = getattr(self, "_uid", 0) + 1
        nb = int(np.prod(shape[1:])) * (4 if dt in (F32, I32) else 2)
        nb = (nb + 31) // 32 * 32
        self._cur = getattr(self, "_cur", 0) + nb
        self._peak = max(getattr(self, "_peak", 0), self._cur)
        t = es.enter_context(self.nc.sbuf_tensor("%s_u%d" % (name, self._uid), list(shape), dt))

        def _rel(nb=nb):
            self._cur -= nb
        es.callback(_rel)
        return t

    def din(self, name, shape, dt):
        return self.nc.dram_tensor(name, list(shape), dt, kind="ExternalInput")

    def build(self):
        nc = self.nc
        es = self.es
        nl = self.n_layers
        d = {}
        d["x"] = self.din("x", [T, D], F32)
        d["pos"] = self.din("pos", [NSEQ, 128, 16], I32)
        d["w_pool"] = self.din("w_pool", [L, D, 512], F32)
        d["w_qkv"] = self.din("w_qkv", [L, NH, D, 384], F32)
        d["w_gate"] = self.din("w_gate", [L, D, 2048], F32)
        d["pool_w"] = self.din("pool_w", [L, 4, 128, 128], F32)
        d["pool_scale"] = self.din("pool_scale", [L, 128, 4], F32)
        d["w_pb"] = self.din("w_pb", [L, 512, D], F32)
        d["w_ab"] = self.din("w_ab", [L, D, D], F32)
        d["lam4"] = self.din("lam4", [L, 4, 64], F32)
        d["subln_w"] = self.din("subln_w", [L, 128], F32)
        d["w_out"] = self.din("w_out", [L, D, D], F32)
        d["ln1_g"] = self.din("ln1_g", [L, D], F32)
        d["ln1_b"] = self.din("ln1_b", [L, D], F32)
        d["w_router"] = self.din("w_router", [L, D, E], F32)
        d["b_router"] = self.din("b_router", [L, E], F32)
        d["w_gu"] = self.din("w_gu", [L, E, D, 2 * D], F32)
        d["b_gu"] = self.din("b_gu", [L, 128, E, 16], F32)
        d["w_down"] = self.din("w_down", [L, E, D, D], F32)
        d["b_down"] = self.din("b_down", [L, E, D], F32)
        d["ln2_g"] = self.din("ln2_g", [L, D], F32)
        d["ln2_b"] = self.din("ln2_b", [L, D], F32)
        d["c_identb"] = self.din("c_identb", [128, 128], BF16)
        d["c_identf"] = self.din("c_identf", [128, 128], F32)
        d["c_mask01"] = self.din("c_mask01", [128, 128], BF16)
        d["c_ustrict"] = self.din("c_ustrict", [128, 128], F32)
        d["c_ones"] = self.din("c_ones", [128, 128], F32)
        d["c_ec"] = self.din("c_ec", [128, E], F32)
        d["c_invfreq"] = self.din("c_invfreq", [128, 8], F32)
        d["c_invcnt"] = self.din("c_invcnt", [128, 4, 16], F32)
        d["y"] = nc.dram_tensor("y", [T, D], F32, kind="ExternalOutput")
        d["xres"] = nc.dram_tensor("xres", [T, D], F32, kind="Internal")
        d["xcur"] = nc.dram_tensor("xcur", [T, D], F32, kind="Internal")
        d["xsbuf"] = nc.dram_tensor("xsbuf", [E * CAP, D], BF16, kind="Internal")
        d["ybuf"] = nc.dram_tensor("ybuf", [E * CAP, D], F32, kind="Internal")
        if self.debug:
            d["dbg_x1"] = nc.dram_tensor("dbg_x1", [T, D], F32, kind="ExternalOutput")
        self.d = d

        self.P = Prog(nc, es)
        self.ps = es.enter_context(nc.psum_tensor("ps", [128, 8, 512], F32))
        self.psb = self.ps.bitcast(BF16)
        sb = self.sb
        self.identb = sb(es, "identb", [128, 128], BF16)
        self.identf = sb(es, "identf", [128, 128], F32)
        self.mask01 = sb(es, "mask01", [128, 128], BF16)
        self.ustrict = sb(es, "ustrict", [128, 128], F32)
        self.ones = sb(es, "ones", [128, 128], F32)
        self.ec = sb(es, "ec", [128, E], F32)
        self.invfreq = sb(es, "invfreq", [128, 1, 8], F32)
        self.invcnt = sb(es, "invcnt", [128, 4, 16], F32)
        self.gate = sb(es, "gate", [128, NT, TOPK], F32)
        self.desti = sb(es, "desti", [128, NT, TOPK], I32)
        self.macc = sb(es, "macc", [128, E], F32)
        self.epsc = sb(es, "epsc", [128, 1], F32)

        def ld(dst, src, key):
            self.Dq("sp", lambda e: e.dma_start(out=dst, in_=src), w=[key])
        ld(self.identb[:], d["c_identb"].ap(), "k0")
        ld(self.identf[:], d["c_identf"].ap(), "k1")
        ld(self.mask01[:], d["c_mask01"].ap(), "k2")
        ld(self.ustrict[:], d["c_ustrict"].ap(), "k3")
        ld(self.ones[:], d["c_ones"].ap(), "k4")
        ld(self.ec[:], d["c_ec"].ap(), "k5")
        ld(self.invfreq[:, 0, :], d["c_invfreq"].ap(), "k6")
        ld(self.invcnt[:], d["c_invcnt"].ap(), "k7")
        self.C("dve", lambda e: e.memset(self.epsc[:], LN_EPS), w=["k8"])
        with ExitStack() as es2:
            z = self.sb(es2, "zrows", [128, 6, D], BF16)
            self.C("dve", lambda e: e.memset(z[:], 0.0), w=["z"])
            xv = d["xsbuf"].ap().rearrange("(n r p) d -> n p r d", p=128, r=6)
            for n in range(E * CAP // (128 * 6)):
                self.Dq("sp", lambda e, n=n: e.dma_start(out=xv[n], in_=z[:]), r=["z"], w=[("xs0", n)])
            self.phase_end("init")

        try:
            for l in range(nl):
                xsrc = d["x"] if l == 0 else d["xcur"]
                xdst = d["y"] if l == nl - 1 else d["xcur"]
                self.C("dve", lambda e: e.memset(self.macc[:], 0.0), w=["macc"])
                for b in range(NSEQ):
                    self.mixer(l, b, xsrc)
                self.moe(l)
                self.combine(l, xdst)
        except StopBuild:
            return nc
        self.es.close()
        return nc

    def layer_norm_tile(self, es_tiles, z, gb, bb, out, zkeys, outkey, tag):
        st, mv, sd, rstd, xn = es_tiles
        C = self.C
        C("dve", lambda e: [e.bn_stats(out=st[:, 0, :], in_=z[:, 0:512]),
                             e.bn_stats(out=st[:, 1, :], in_=z[:, 512:1024])][-1],
          r=list(zkeys), w=[tag + "st"])
        C("dve", lambda e: e.bn_aggr(out=mv[:], in_=st[:].rearrange("p a b -> p (a b)")), r=[tag + "st"], w=[tag + "mv"])
        C("act", lambda e: e.activation(out=sd[:], in_=mv[:, 1:2], func=AF.Sqrt, bias=self.epsc[:], scale=1.0),
          r=[tag + "mv"], w=[tag + "sd"])
        C("dve", lambda e: e.reciprocal(out=rstd[:], in_=sd[:]), r=[tag + "sd"], w=[tag + "rs"])
        C("dve", lambda e: e.tensor_scalar(out=xn[:], in0=z[:], scalar1=mv[:, 0:1], scalar2=rstd[:],
                                           op0=ALU.subtract, op1=ALU.mult), r=list(zkeys) + [tag + "mv", tag + "rs"], w=[tag + "xn"])
        C("pool", lambda e: e.tensor_tensor(out=xn[:], in0=xn[:], in1=gb[:], op=ALU.mult), r=[tag + "xn", "lnp"], w=[tag + "xn"])
        C("pool", lambda e: e.tensor_tensor(out=out, in0=xn[:], in1=bb[:], op=ALU.add), r=[tag + "xn", "lnp"], w=[outkey])

    def mixer(self, l, b, xsrc):
        nc, d, C, Dq, sb = self.nc, self.d, self.C, self.Dq, self.sb
        ps, psb = self.ps, self.psb
        tok0 = b * S
        lam_init = 0.8 - 0.6 * math.exp(-0.3 * l)
        if True:
            with ExitStack() as es_o:
                xT = sb(es_o, "xT", [128, 8, S], BF16)
                ypre = sb(es_o, "ypre", [128, 4, S], BF16)
                onT = sb(es_o, "onT", [128, 8, S], BF16)
                cos = sb(es_o, "cos", [128, 16, 8], F32)
                sin = sb(es_o, "sin", [128, 16, 8], F32)
                neglam = sb(es_o, "neglam", [128, 1], F32)
                sublnw = sb(es_o, "sublnw", [128, 1, 128], F32)
                with ExitStack() as es1:
                    xst = [sb(es1, "xst%d" % i, [128, D], F32) for i in range(2)]
                    xb = [sb(es1, "xb%d" % i, [128, D], BF16) for i in range(2)]
                    for tt in range(16):
                        i2 = tt % 2
                        Dq("sp", lambda e, tt=tt, i2=i2: e.dma_start(out=xst[i2][:], in_=xsrc.ap()[tok0 + tt * 128: tok0 + (tt + 1) * 128, :]),
                           w=[("xst", i2)])
                        C("act", lambda e, i2=i2: e.activation(out=xb[i2][:], in_=xst[i2][:], func=AF.Copy),
                          r=[("xst", i2)], w=[("xb", i2)])
                        C("pe", lambda e, i2=i2: [e.transpose(out=psb[:, i2, c * 128:(c + 1) * 128], in_=xb[i2][:, c * 128:(c + 1) * 128],
                                                              identity=self.identb[:]) for c in range(8)][-1],
                          r=[("xb", i2), "k0"], w=[("ps", i2)])
                        C("dve", lambda e, tt=tt, i2=i2: e.tensor_copy(out=xT[:, :, tt * 128:(tt + 1) * 128],
                                                                     in_=psb[:, i2, :].rearrange("p (c t) -> p c t", c=8)),
                          r=[("ps", i2)], w=[("xT", tt)])
                    posi = sb(es1, "posi", [128, 16], I32)
                    posf = sb(es1, "posf", [128, 16, 1], F32)
                    ang = sb(es1, "ang", [128, 16, 8], F32)
                    kf = sb(es1, "kf", [128, 16, 8], F32)
                    ki = sb(es1, "ki", [128, 16, 8], I32)
                    r2 = sb(es1, "r2", [128, 16, 8], F32)
                    yy = sb(es1, "yy", [128, 16, 8], F32)
                    mm = sb(es1, "mm", [128, 16, 8], F32)
                    Dq("sp", lambda e: e.dma_start(out=posi[:], in_=d["pos"].ap()[b]), w=["posi"])
                    C("dve", lambda e: e.tensor_copy(out=posf[:, :, 0], in_=posi[:]), r=["posi"], w=["posf"])
                    C("dve", lambda e: e.tensor_tensor(out=ang[:], in0=posf[:].to_broadcast([128, 16, 8]),
                                                       in1=self.invfreq[:].to_broadcast([128, 16, 8]), op=ALU.mult),
                      r=["posf", "k6"], w=["ang"])
                    TWO_PI = 2.0 * math.pi
                    C1 = 6.28125
                    C2 = TWO_PI - C1
                    C("dve", lambda e: e.tensor_scalar(out=kf[:], in0=ang[:], scalar1=1.0 / TWO_PI, scalar2=None, op0=ALU.mult),
                      r=["ang"], w=["kf"])
                    C("dve", lambda e: e.tensor_copy(out=ki[:], in_=kf[:]), r=["kf"], w=["ki"])
                    C("dve", lambda e: e.tensor_copy(out=kf[:], in_=ki[:]), r=["ki"], w=["kf"])
                    C("dve", lambda e: e.scalar_tensor_tensor(out=r2[:], in0=kf[:], scalar=-C1, in1=ang[:], op0=ALU.mult, op1=ALU.add),
                      r=["kf", "ang"], w=["r2"])
                    C("dve", lambda e: e.scalar_tensor_tensor(out=r2[:], in0=kf[:], scalar=-C2, in1=r2[:], op0=ALU.mult, op1=ALU.add),
                      r=["kf", "r2"], w=["r2"])
                    for shift, dst, nm in ((0.0, sin, "sin"), (math.pi / 2, cos, "cos")):
                        C("dve", lambda e, shift=shift: e.tensor_scalar(out=yy[:], in0=r2[:], scalar1=shift, scalar2=None, op0=ALU.add),
                          r=["r2"], w=["yy"])
                        C("dve", lambda e: e.tensor_scalar(out=mm[:], in0=yy[:], scalar1=math.pi, scalar2=None, op0=ALU.is_gt),
                          r=["yy"], w=["mm"])
                        C("dve", lambda e: e.scalar_tensor_tensor(out=yy[:], in0=mm[:], scalar=-TWO_PI, in1=yy[:], op0=ALU.mult, op1=ALU.add),
                          r=["mm", "yy"], w=["yy"])
                        C("dve", lambda e: e.tensor_scalar(out=mm[:], in0=yy[:], scalar1=-math.pi, scalar2=None, op0=ALU.is_lt),
                          r=["yy"], w=["mm"])
                        C("dve", lambda e: e.scalar_tensor_tensor(out=yy[:], in0=mm[:], scalar=TWO_PI, in1=yy[:], op0=ALU.mult, op1=ALU.add),
                          r=["mm", "yy"], w=["yy"])
                        C("dve", lambda e: e.tensor_scalar(out=yy[:], in0=yy[:], scalar1=math.pi, scalar2=-math.pi, op0=ALU.min, op1=ALU.max),
                          r=["yy"], w=["yy"])
                        C("act", lambda e, dst=dst: e.activation(out=dst[:], in_=yy[:], func=AF.Sin), r=["yy"], w=[nm])
                    l4 = sb(es1, "l4", [128, 4, 64], F32)
                    lp = sb(es1, "lp", [128, 2, 64], F32)
                    lsum = sb(es1, "lsum", [128, 2], F32)
                    lex = sb(es1, "lex", [128, 2], F32)
                    Dq("sp", lambda e: e.dma_start(out=l4[:], in_=d["lam4"].ap()[l:l + 1].to_broadcast([128, 4, 64])), w=["l4"])
                    C("dve", lambda e: e.tensor_tensor(out=lp[:], in0=l4[:, 0:4:2, :], in1=l4[:, 1:4:2, :], op=ALU.mult), r=["l4"], w=["lp"])
                    C("dve", lambda e: e.tensor_reduce(out=lsum[:], in_=lp[:], axis=AX.X, op=ALU.add), r=["lp"], w=["lsum"])
                    C("act", lambda e: e.activation(out=lex[:], in_=lsum[:], func=AF.Exp), r=["lsum"], w=["lex"])
                    C("dve", lambda e: e.tensor_tensor(out=neglam[:], in0=lex[:, 1:2], in1=lex[:, 0:1], op=ALU.subtract), r=["lex"], w=["neglam"])
                    C("dve", lambda e: e.tensor_scalar(out=neglam[:], in0=neglam[:], scalar1=-lam_init, scalar2=None, op0=ALU.add),
                      r=["neglam"], w=["neglam"])
                    Dq("sp", lambda e: e.dma_start(out=sublnw[:, 0, :], in_=d["subln_w"].ap()[l:l + 1].to_broadcast([128, 128])), w=["sublnw"])
                    C("act", lambda e: e.activation(out=sublnw[:], in_=sublnw[:], func=AF.Copy, scale=(1.0 - lam_init)), r=["sublnw"], w=["sublnw"])
                    wpool = sb(es1, "wpool", [128, 8, 512], BF16)
                    poolw = sb(es1, "poolw", [128, 4, 128], BF16)
                    pscale = sb(es1, "pscale", [128, 4], F32)
                    Dq("pool", lambda e: e.dma_start(out=wpool[:], in_=d["w_pool"].ap()[l].rearrange("(kc p) n -> p kc n", p=128)), w=["wpool"])
                    Dq("pool", lambda e: e.dma_start(out=poolw[:], in_=d["pool_w"].ap()[l].rearrange("g c n -> c g n")), w=["poolw"])
                    Dq("sp", lambda e: e.dma_start(out=pscale[:], in_=d["pool_scale"].ap()[l]), w=["pscale"])
                    upad = [sb(es1, "upad%d" % i, [128, 16 + S], F32) for i in range(2)]
                    sa = sb(es1, "sa", [128, 16 + S], F32)
                    sbb = sb(es1, "sbb", [128, 16 + S], F32)
                    dbf = sb(es1, "dbf", [128, S], BF16)
                    t16 = sb(es1, "t16", [128, 16], F32)
                    for i in range(2):
                        C("pool", lambda e, i=i: e.memset(upad[i][:, 0:16], 0.0), w=[("upz", i)])
                    C("pool", lambda e: e.memset(sa[:, 0:16], 0.0), w=["saz"])
                    C("pool", lambda e: e.memset(sbb[:, 0:16], 0.0), w=["sbz"])
                    for g in range(4):
                        u = upad[g % 2]
                        for tc in range(4):
                            bk = 2 + (g * 4 + tc) % 2
                            C("pe", lambda e, g=g, tc=tc, bk=bk: [e.matmul(out=ps[:, bk, :], lhsT=wpool[:, kc, g * 128:(g + 1) * 128],
                                                                          rhs=xT[:, kc, tc * 512:(tc + 1) * 512], start=(kc == 0), stop=(kc == 7))
                                                                 for kc in range(8)][-1],
                              r=[("xT", t) for t in range(tc * 4, tc * 4 + 4)] + ["wpool"], w=[("ps", bk)])
                            C("act", lambda e, u=u, tc=tc, bk=bk: e.activation(out=u[:, 16 + tc * 512: 16 + (tc + 1) * 512], in_=ps[:, bk, :], func=AF.Copy),
                              r=[("ps", bk)], w=[("u", g % 2)])
                        w = 2 ** (g + 1)
                        src, bufs = u, [sa, sbb]
                        sh = 1
                        for st in range(g + 1):
                            dst = bufs[st % 2]
                            C("dve", lambda e, src=src, dst=dst, sh=sh: e.tensor_tensor(out=dst[:, 16:16 + S], in0=src[:, 16:16 + S],
                                                                                         in1=src[:, 16 - sh:16 - sh + S], op=ALU.add),
                              r=[("u", g % 2), ("upz", g % 2), "saz", "sbz", "sa", "sbb"], w=["sa" if st % 2 == 0 else "sbb"])
                            src = dst
                            sh *= 2
                        sfin = src
                        C("dve", lambda e, sfin=sfin, u=u, w=w: e.scalar_tensor_tensor(out=dbf[:], in0=sfin[:, 16:16 + S], scalar=1.0 / w,
                                                                                        in1=u[:, 16:16 + S], op0=ALU.mult, op1=ALU.subtract),
                          r=["sa", "sbb", ("u", g % 2)], w=["dbf"])
                        C("dve", lambda e, sfin=sfin, g=g: e.tensor_tensor(out=t16[:], in0=sfin[:, 16:32], in1=self.invcnt[:, g, :], op=ALU.mult),
                          r=["sa", "sbb", "k7"], w=["t16"])
                        C("dve", lambda e, u=u: e.tensor_tensor(out=dbf[:, 0:16], in0=t16[:], in1=u[:, 16:32], op=ALU.subtract),
                          r=["t16", ("u", g % 2), "dbf"], w=["dbf"])
                        for tc in range(4):
                            bk = 4 + tc % 2
                            C("pe", lambda e, g=g, tc=tc, bk=bk: e.matmul(out=ps[:, bk, :], lhsT=poolw[:, g, :], rhs=dbf[:, tc * 512:(tc + 1) * 512],
                                                                          start=True, stop=True),
                              r=["dbf", "poolw"], w=[("ps", bk)])
                            C("act", lambda e, g=g, tc=tc, bk=bk: e.activation(out=ypre[:, g, tc * 512:(tc + 1) * 512], in_=ps[:, bk, :], func=AF.Identity,
                                                                                scale=pscale[:, g:g + 1]),
                              r=[("ps", bk), "pscale"], w=[("ypre", g, tc)])
                    self.phase_end("A1")
                with ExitStack() as es2:
                    wqkv = [sb(es2, "wqkv%d" % i, [128, 8, 384], BF16) for i in range(2)]
                    vaug = [sb(es2, "vaug%d" % i, [128, 16, 132], BF16) for i in range(2)]
                    qT = [sb(es2, "qT%d" % i, [128, S], BF16) for i in range(2)]
                    kT = [[sb(es2, "kT%d_%d" % (i, m), [128, S], BF16) for m in range(2)] for i in range(2)]
                    PT = [sb(es2, "PT%d" % i, [128, 16, 512], BF16) for i in range(2)]
                    ost = sb(es2, "ost", [128, 16, 2, 132], F32)
                    rtmp = [sb(es2, "rtmp%d" % i, [128, 4, 4, 8], F32) for i in range(2)]
                    qkb = [sb(es2, "qkb%d" % i, [128, 256], BF16) for i in range(2)]
                    rl = sb(es2, "rl", [128, 16, 2, 1], F32)
                    r1n = sb(es2, "r1n", [128, 16, 1], F32)
                    t0 = sb(es2, "t0", [128, 8, 128], F32)
                    t1 = sb(es2, "t1", [128, 8, 128], F32)
                    ss = sb(es2, "ss", [128, 16], F32)
                    rs = sb(es2, "rs", [128, 16, 1], F32)
                    onb = sb(es2, "onb", [128, 16, 128], BF16)
                    for i in range(2):
                        C("pool", lambda e, i=i: e.memset(vaug[i][:, :, 128:132], 1.0), w=[("vone", i)])
                        C("pool", lambda e, i=i: e.memset(kT[i][0][64:128, :], 0.0), w=[("kz", i, 0)])
                        C("pool", lambda e, i=i: e.memset(kT[i][1][0:64, :], 0.0), w=[("kz", i, 1)])
                    for h in range(NH):
                        hs = h % 2
                        Dq("pool", lambda e, h=h, hs=hs: e.dma_start(out=wqkv[hs][:], in_=d["w_qkv"].ap()[l, h].rearrange("(kc p) n -> p kc n", p=128)),
                           w=[("wqkv", hs)])
                        for tt in range(16):
                            i2 = tt % 2
                            bk = i2
                            C("pe", lambda e, tt=tt, bk=bk, hs=hs: [e.matmul(out=ps[:, bk, 0:384], lhsT=xT[:, kc, tt * 128:(tt + 1) * 128],
                                                                            rhs=wqkv[hs][:, kc, :], start=(kc == 0), stop=(kc == 7)) for kc in range(8)][-1],
                              r=[("xT", tt), ("wqkv", hs)], w=[("ps", bk)])
                            qk4 = ps[:, bk, 0:256].rearrange("p (a d) -> p a d", a=4)
                            cb = cos[:, tt:tt + 1, :].to_broadcast([128, 4, 8])
                            sn = sin[:, tt:tt + 1, :].to_broadcast([128, 4, 8])
                            rt = rtmp[i2]
                            C("dve", lambda e, qk4=qk4, cb=cb, sn=sn, rt=rt: [
                                e.tensor_tensor(out=rt[:, 0], in0=qk4[:, :, 0:8], in1=cb, op=ALU.mult),
                                e.tensor_tensor(out=rt[:, 1], in0=qk4[:, :, 8:16], in1=sn, op=ALU.mult),
                                e.tensor_tensor(out=rt[:, 2], in0=qk4[:, :, 8:16], in1=cb, op=ALU.mult),
                                e.tensor_tensor(out=rt[:, 3], in0=qk4[:, :, 0:8], in1=sn, op=ALU.mult)][-1],
                              r=[("ps", bk), "cos", "sin"], w=[("rt", i2)])
                            C("act", lambda e, bk=bk, i2=i2: e.activation(out=qkb[i2][:], in_=ps[:, bk, 0:256], func=AF.Copy),
                              r=[("ps", bk)], w=[("qkb", i2)])
                            C("act", lambda e, bk=bk, tt=tt, hs=hs: e.activation(out=vaug[hs][:, tt, 0:128], in_=ps[:, bk, 256:384], func=AF.Copy),
                              r=[("ps", bk)], w=[("v", hs, tt)])
                            qb4 = qkb[i2][:].rearrange("p (a d) -> p a d", a=4)
                            C("dve", lambda e, qb4=qb4, rt=rt: [
                                e.tensor_tensor(out=qb4[:, :, 0:8], in0=rt[:, 0], in1=rt[:, 1], op=ALU.subtract),
                                e.tensor_tensor(out=qb4[:, :, 8:16], in0=rt[:, 2], in1=rt[:, 3], op=ALU.add)][-1],
                              r=[("rt", i2), ("qkb", i2)], w=[("qkb", i2)])
                            bk2 = 2 + i2
                            C("pe", lambda e, bk2=bk2, i2=i2: [e.transpose(out=psb[:, bk2, 0:128], in_=qkb[i2][:, 0:128], identity=self.identb[:]),
                                                               e.transpose(out=psb[:, bk2, 128:256], in_=qkb[i2][:, 128:256], identity=self.identb[:])][-1],
                              r=[("qkb", i2), "k0"], w=[("ps", bk2)])
                            C("dve", lambda e, bk2=bk2, tt=tt, hs=hs: [e.tensor_copy(out=qT[hs][:, tt * 128:(tt + 1) * 128], in_=psb[:, bk2, 0:128]),
                                                                       e.tensor_copy(out=kT[hs][0][0:64, tt * 128:(tt + 1) * 128], in_=psb[0:64, bk2, 128:256]),
                                                                       e.tensor_copy(out=kT[hs][1][64:128, tt * 128:(tt + 1) * 128], in_=psb[64:128, bk2, 128:256])][-1],
                              r=[("ps", bk2), ("kz", hs, 0), ("kz", hs, 1)], w=[("qk", hs, tt)])
                        if h == 0 and self.stop_after == "A2a":
                            self.phase_end("A2a")
                        chunks = [(m, qc) for qc in range(4) for m in range(2)]
                        sbank = [0]

                        def scores(m, qc, hs=hs):
                            nkb = 4 * qc + 4
                            for kb in range(nkb):
                                jp = kb - 4 * qc
                                q0 = (4 * qc + max(jp, 0)) * 128
                                n = (4 * qc + 4) * 128 - q0
                                bk = sbank[0] % 4
                                sbank[0] += 1
                                C("pe", lambda e, m=m, kb=kb, q0=q0, n=n, bk=bk: e.matmul(
                                    out=ps[:, bk, 0:n], lhsT=kT[hs][m][:, kb * 128:(kb + 1) * 128],
                                    rhs=qT[hs][:, q0:q0 + n], start=True, stop=True),
                                  r=[("qk", hs, t) for t in range(q0 // 128, 4 * qc + 4)] + [("qk", hs, kb)], w=[("ps", bk)])
                                C("act", lambda e, m=m, kb=kb, n=n, bk=bk: e.activation(out=PT[m][:, kb, 0:n], in_=ps[:, bk, 0:n], func=AF.Exp, scale=0.125),
                                  r=[("ps", bk)], w=[("PT", m, kb)])
                                if jp >= 0:
                                    C("pool", lambda e, m=m, kb=kb: e.tensor_tensor(out=PT[m][:, kb, 0:128], in0=PT[m][:, kb, 0:128],
                                                                                    in1=self.mask01[:], op=ALU.mult),
                                      r=[("PT", m, kb), "k2"], w=[("PT", m, kb)])

                        obank = [0]

                        def av(m, qc, hs=hs):
                            for j in range(4):
                                qb = 4 * qc + j
                                bk = 4 + obank[0] % 4
                                obank[0] += 1

                                def f(e, m=m, qc=qc, j=j, qb=qb, bk=bk):
                                    ins = None
                                    for kb in range(qb + 1):
                                        col = (j - max(kb - 4 * qc, 0)) * 128
                                        ins = e.matmul(out=ps[:, bk, 0:130], lhsT=PT[m][:, kb, col:col + 128], rhs=vaug[hs][:, kb, 0:130],
                                                       start=(kb == 0), stop=(kb == qb))
                                    return ins
                                C("pe", f, r=[("PT", m, kb) for kb in range(qb + 1)] + [("v", hs, kb) for kb in range(qb + 1)] + [("vone", hs)],
                                  w=[("ps", bk)])
                                C("act", lambda e, m=m, qb=qb, bk=bk: e.activation(out=ost[:, qb, m, 0:129], in_=ps[:, bk, 0:129], func=AF.Copy),
                                  r=[("ps", bk)], w=[("ost", qb, m)])

                        scores(*chunks[0])
                        for ci in range(len(chunks)):
                            if ci + 1 < len(chunks):
                                scores(*chunks[ci + 1])
                            av(*chunks[ci])
                        if h == 0 and self.stop_after == "A2b":
                            self.phase_end("A2b")
                        ostk = [("ost", qb, m) for qb in range(16) for m in range(2)]
                        C("dve", lambda e: e.reciprocal(out=rl[:], in_=ost[:, :, :, 128:129]), r=ostk, w=["rl"])
                        C("dve", lambda e: e.tensor_scalar(out=r1n[:], in0=rl[:, :, 1, :], scalar1=neglam[:, 0:1], scalar2=None, op0=ALU.mult),
                          r=["rl", "neglam"], w=["r1n"])
                        for hf in range(2):
                            qs = slice(hf * 8, hf * 8 + 8)
                            C("dve", lambda e, qs=qs: e.tensor_tensor(out=t0[:], in0=ost[:, qs, 0, 0:128], in1=rl[:, qs, 0, :].to_broadcast([128, 8, 128]), op=ALU.mult),
                              r=ostk + ["rl", "t0"], w=["t0"])
                            C("dve", lambda e, qs=qs: e.tensor_tensor(out=t1[:], in0=ost[:, qs, 1, 0:128], in1=r1n[:, qs, :].to_broadcast([128, 8, 128]), op=ALU.mult),
                              r=ostk + ["r1n", "t1"], w=["t1"])
                            C("dve", lambda e: e.tensor_tensor(out=t0[:], in0=t0[:], in1=t1[:], op=ALU.add), r=["t0", "t1"], w=["t0"])
                            C("dve", lambda e: e.tensor_tensor(out=t1[:], in0=t0[:], in1=t0[:], op=ALU.mult), r=["t0", "t1"], w=["t1"])
                            C("dve", lambda e, qs=qs: e.tensor_reduce(out=ss[:, qs], in_=t1[:], axis=AX.X, op=ALU.add), r=["t1"], w=["ss"])
                            C("act", lambda e, qs=qs: e.activation(out=rs[:, qs, 0], in_=ss[:, qs], func=AF.Sqrt, bias=self.epsc[:], scale=1.0 / 128.0),
                              r=["ss", "k8"], w=["rs"])
                            C("dve", lambda e, qs=qs: e.reciprocal(out=rs[:, qs, :], in_=rs[:, qs, :]), r=["rs"], w=["rs"])
                            C("dve", lambda e, qs=qs: e.tensor_tensor(out=t1[:], in0=t0[:], in1=rs[:, qs, :].to_broadcast([128, 8, 128]), op=ALU.mult),
                              r=["t0", "rs", "t1"], w=["t1"])
                            C("dve", lambda e, qs=qs: e.tensor_tensor(out=onb[:, qs, :], in0=t1[:], in1=sublnw[:].to_broadcast([128, 8, 128]), op=ALU.mult),
                              r=["t1", "sublnw", "onb"], w=["onb"])
                        if h == 0 and self.stop_after == "A2c":
                            self.phase_end("A2c")
                        for half in range(2):
                            bk = 2 + half
                            C("pe", lambda e, half=half, bk=bk: [e.transpose(out=psb[:, bk, c * 128:(c + 1) * 128], in_=onb[:, half * 8 + c, :],
                                                                             identity=self.identb[:]) for c in range(8)][-1],
                              r=["onb", "k0"], w=[("ps", bk)])
                            C("act", lambda e, half=half, bk=bk, h=h: e.activation(out=onT[:, h, half * 1024:(half + 1) * 1024], in_=psb[:, bk, :], func=AF.Copy),
                              r=[("ps", bk)], w=[("onT", h, half)])
                    self.phase_end("A2")
                merged = sb(es_o, "merged", [128, 8, S], BF16)
                with ExitStack() as es3:
                    wg = [sb(es3, "wg%d" % i, [128, 8, 2, 128], BF16) for i in range(2)]
                    wab = [sb(es3, "wab%d" % i, [128, 8, 128], BF16) for i in range(2)]
                    wpb = [sb(es3, "wpb%d" % i, [128, 4, 128], BF16) for i in range(2)]
                    sg = [[sb(es3, "sg%d_%d" % (i, k), [128, 512], F32) for k in range(2)] for i in range(2)]
                    mt = [[sb(es3, "mt%d_%d" % (i, k), [128, 512], F32) for k in range(2)] for i in range(2)]
                    wgv = d["w_gate"].ap()[l].rearrange("(kc p) (br jj n) -> p kc br jj n", p=128, br=2, jj=8)
                    wabv = d["w_ab"].ap()[l].rearrange("(kc p) (jj n) -> p kc jj n", p=128, jj=8)
                    wpbv = d["w_pb"].ap()[l].rearrange("(kc p) (jj n) -> p kc jj n", p=128, jj=8)
                    it = 0
                    for j in range(8):
                        js = j % 2
                        for br in range(2):
                            Dq("pool", lambda e, j=j, js=js, br=br: e.dma_start(out=wg[js][:, :, br, :], in_=wgv[:, :, br, j, :]), w=[("wg", js, br)])
                        Dq("pool", lambda e, j=j, js=js: e.dma_start(out=wab[js][:], in_=wabv[:, :, j, :]), w=[("wab", js)])
                        Dq("pool", lambda e, j=j, js=js: e.dma_start(out=wpb[js][:], in_=wpbv[:, :, j, :]), w=[("wpb", js)])
                        for tc in range(4):
                            s2 = it % 2
                            it += 1
                            b0 = s2 * 4
                            tsl = slice(tc * 512, (tc + 1) * 512)
                            C("pe", lambda e, js=js, tsl=tsl, b0=b0: [e.matmul(out=ps[:, b0, :], lhsT=wpb[js][:, g, :], rhs=ypre[:, g, tsl],
                                                                              start=(g == 0), stop=(g == 3)) for g in range(4)][-1],
                              r=[("wpb", js)], w=[("ps", b0)])
                            C("pe", lambda e, js=js, tsl=tsl, b0=b0: [e.matmul(out=ps[:, b0 + 1, :], lhsT=wab[js][:, c, :], rhs=onT[:, c, tsl],
                                                                              start=(c == 0), stop=(c == 7)) for c in range(8)][-1],
                              r=[("wab", js)], w=[("ps", b0 + 1)])
                            for br in range(2):
                                C("pe", lambda e, js=js, tsl=tsl, b0=b0, br=br: [e.matmul(out=ps[:, b0 + 2 + br, :], lhsT=wg[js][:, kc, br, :], rhs=xT[:, kc, tsl],
                                                                                         start=(kc == 0), stop=(kc == 7)) for kc in range(8)][-1],
                                  r=[("wg", js, br)], w=[("ps", b0 + 2 + br)])
                                C("act", lambda e, s2=s2, b0=b0, br=br: e.activation(out=sg[s2][br][:], in_=ps[:, b0 + 2 + br, :], func=AF.Sigmoid),
                                  r=[("ps", b0 + 2 + br)], w=[("sg", s2, br)])
                                C("dve", lambda e, s2=s2, b0=b0, br=br: e.tensor_tensor(out=mt[s2][br][:], in0=sg[s2][br][:], in1=ps[:, b0 + br, :], op=ALU.mult),
                                  r=[("sg", s2, br), ("ps", b0 + br)], w=[("mt", s2, br)])
                            C("pool", lambda e, s2=s2, j=j, tsl=tsl: e.tensor_tensor(out=merged[:, j, tsl], in0=mt[s2][0][:], in1=mt[s2][1][:], op=ALU.add),
                              r=[("mt", s2, 0), ("mt", s2, 1)], w=[("merged", j, tsl.start)])
                    self.phase_end("A3a")
                with ExitStack() as es4:
                    wout = sb(es4, "wout", [128, 8, D], BF16)
                    wr = sb(es4, "wr", [128, 8, E], F32)
                    brb = sb(es4, "brb", [128, E], F32)
                    gb = sb(es4, "g1b", [128, D], F32)
                    bb = sb(es4, "b1b", [128, D], F32)
                    Dq("pool", lambda e: e.dma_start(out=wout[:], in_=d["w_out"].ap()[l].rearrange("(kc p) n -> p kc n", p=128)), w=["wout"])
                    Dq("sp", lambda e: e.dma_start(out=wr[:], in_=d["w_router"].ap()[l].rearrange("(kc p) n -> p kc n", p=128)), w=["wr"])
                    Dq("sp", lambda e: e.dma_start(out=brb[:], in_=d["b_router"].ap()[l:l + 1].to_broadcast([128, E])), w=["brb"])
                    Dq("sp", lambda e: e.dma_start(out=gb[:], in_=d["ln1_g"].ap()[l:l + 1].to_broadcast([128, D])), w=["lnp"])
                    Dq("sp", lambda e: e.dma_start(out=bb[:], in_=d["ln1_b"].ap()[l:l + 1].to_broadcast([128, D])), w=["lnp"])
                    xin = [sb(es4, "xin%d" % i, [128, D], F32) for i in range(2)]
                    zt = [sb(es4, "zt%d" % i, [128, D], F32) for i in range(2)]
                    x1t = [sb(es4, "x1t%d" % i, [128, D], F32) for i in range(2)]
                    x1b = [sb(es4, "x1b%d" % i, [128, D], BF16) for i in range(2)]
                    x1T = [sb(es4, "x1T%d" % i, [128, 8, 128], F32) for i in range(2)]
                    lnt = [(sb(es4, "st%d" % i, [128, 2, 6], F32), sb(es4, "mv%d" % i, [128, 2], F32), sb(es4, "sd%d" % i, [128, 1], F32),
                            sb(es4, "rstd%d" % i, [128, 1], F32), sb(es4, "xn%d" % i, [128, D], F32)) for i in range(2)]
                    Lt = [sb(es4, "Lt%d" % i, [128, E], F32) for i in range(2)]
                    mx8 = [sb(es4, "mx8%d" % i, [128, 8], F32) for i in range(2)]
                    Mk = [sb(es4, "Mk%d" % i, [128, E], F32) for i in range(2)]
                    nv0 = [sb(es4, "nv0%d" % i, [128, 1], F32) for i in range(2)]
                    ek = [sb(es4, "ek%d" % i, [128, 4], F32) for i in range(2)]
                    gs = [sb(es4, "gs%d" % i, [128, 1], F32) for i in range(2)]
                    posc = [sb(es4, "posc%d" % i, [128, 1, E], F32) for i in range(2)]
                    oh = [sb(es4, "oh%d" % i, [128, 4, E], F32) for i in range(2)]
                    dstf = [sb(es4, "dstf%d" % i, [128, 4], F32) for i in range(2)]
                    for tt in range(16):
                        i2 = tt % 2
                        ti = b * 16 + tt
                        tg = "n%d_" % i2
                        rows = slice(tok0 + tt * 128, tok0 + (tt + 1) * 128)
                        Dq("sp", lambda e, i2=i2, rows=rows: e.dma_start(out=xin[i2][:], in_=xsrc.ap()[rows, :]), w=[("xin", i2)])
                        for n in range(2):
                            C("pe", lambda e, tt=tt, n=n, i2=i2: [e.matmul(out=ps[:, i2 * 2 + n, :], lhsT=merged[:, j, tt * 128:(tt + 1) * 128],
                                                                         rhs=wout[:, j, n * 512:(n + 1) * 512], start=(j == 0), stop=(j == 7)) for j in range(8)][-1],
                              r=["wout"], w=[("ps", i2 * 2 + n)])
                            C("dve", lambda e, n=n, i2=i2: e.scalar_tensor_tensor(out=zt[i2][:, n * 512:(n + 1) * 512], in0=xin[i2][:, n * 512:(n + 1) * 512], scalar=ALPHA,
                                                                                  in1=ps[:, i2 * 2 + n, :], op0=ALU.mult, op1=ALU.add),
                              r=[("xin", i2), ("ps", i2 * 2 + n)], w=[(tg + "z", n)])
                        self.layer_norm_tile(lnt[i2], zt[i2], gb, bb, x1t[i2][:], [(tg + "z", 0), (tg + "z", 1)], ("x1t", i2), tg)
                        Dq("sp", lambda e, i2=i2, rows=rows: e.dma_start(out=d["xres"].ap()[rows, :], in_=x1t[i2][:]), r=[("x1t", i2)], w=[("xres", ti)])
                        if self.debug:
                            Dq("sp", lambda e, i2=i2, rows=rows: e.dma_start(out=d["dbg_x1"].ap()[rows, :], in_=x1t[i2][:]), r=[("x1t", i2)], w=[("dbgx1", ti)])
                        C("act", lambda e, i2=i2: e.activation(out=x1b[i2][:], in_=x1t[i2][:], func=AF.Copy), r=[("x1t", i2)], w=[("x1b", i2)])
                        for hf in range(2):
                            bk = 4 + hf
                            C("pe", lambda e, i2=i2, hf=hf, bk=bk: [e.transpose(out=ps[:, bk, c * 128:(c + 1) * 128], in_=x1t[i2][:, (hf * 4 + c) * 128:(hf * 4 + c + 1) * 128],
                                                                               identity=self.identf[:]) for c in range(4)][-1],
                              r=[("x1t", i2), "k1"], w=[("ps", bk)])
                            C("act", lambda e, i2=i2, hf=hf, bk=bk: e.activation(out=x1T[i2][:, hf * 4:(hf + 1) * 4, :], in_=ps[:, bk, :].rearrange("p (c t) -> p c t", c=4),
                                                                                 func=AF.Copy),
                              r=[("ps", bk)], w=[("x1T", i2, hf)])
                        C("pe", lambda e, i2=i2: [e.matmul(out=ps[:, 6, 0:E], lhsT=x1T[i2][:, kc, :], rhs=wr[:, kc, :], start=(kc == 0), stop=(kc == 7)) for kc in range(8)][-1],
                          r=[("x1T", i2, 0), ("x1T", i2, 1), "wr"], w=[("ps", 6)])
                        C("dve", lambda e, i2=i2: e.tensor_tensor(out=Lt[i2][:], in0=ps[:, 6, 0:E], in1=brb[:], op=ALU.add), r=[("ps", 6), "brb"], w=[tg + "L"])
                        C("dve", lambda e, i2=i2: e.max(out=mx8[i2][:], in_=Lt[i2][:]), r=[tg + "L"], w=[tg + "mx"])
                        C("dve", lambda e, i2=i2: e.tensor_scalar(out=Mk[i2][:], in0=Lt[i2][:], scalar1=mx8[i2][:, 3:4], scalar2=None, op0=ALU.is_ge),
                          r=[tg + "L", tg + "mx"], w=[tg + "Mk"])
                        C("dve", lambda e, i2=i2: e.tensor_scalar(out=nv0[i2][:], in0=mx8[i2][:, 0:1], scalar1=-1.0, scalar2=None, op0=ALU.mult),
                          r=[tg + "mx"], w=[tg + "nv"])
                        C("act", lambda e, i2=i2: e.activation(out=ek[i2][:], in_=mx8[i2][:, 0:4], func=AF.Exp, bias=nv0[i2][:], scale=1.0, accum_out=gs[i2][:]),
                          r=[tg + "mx", tg + "nv"], w=[tg + "ek"])
                        C("dve", lambda e, i2=i2: e.reciprocal(out=gs[i2][:], in_=gs[i2][:]), r=[tg + "ek"], w=[tg + "gs"])
                        C("dve", lambda e, i2=i2, ti=ti: e.tensor_scalar(out=self.gate[:, ti, :], in0=ek[i2][:], scalar1=gs[i2][:], scalar2=None, op0=ALU.mult),
                          r=[tg + "ek", tg + "gs"], w=[("gate", ti)])
                        C("pe", lambda e, i2=i2: [e.matmul(out=ps[:, 7, 0:E], lhsT=self.ustrict[:], rhs=Mk[i2][:], start=True, stop=False),
                                                  e.matmul(out=ps[:, 7, 0:E], lhsT=self.ones[:], rhs=self.macc[:], start=False, stop=True)][-1],
                          r=[tg + "Mk", "macc", "k3", "k4"], w=[("ps", 7)])
                        C("dve", lambda e, i2=i2: e.scalar_tensor_tensor(out=posc[i2][:, 0, :], in0=ps[:, 7, 0:E], scalar=float(CAP - 1), in1=self.ec[:],
                                                                         op0=ALU.min, op1=ALU.add),
                          r=[("ps", 7), "k5"], w=[tg + "pc"])
                        C("dve", lambda e, i2=i2: e.tensor_tensor(out=self.macc[:], in0=self.macc[:], in1=Mk[i2][:], op=ALU.add), r=["macc", tg + "Mk"], w=["macc"])
                        C("dve", lambda e, i2=i2: [e.tensor_scalar(out=oh[i2][:, k, :], in0=Lt[i2][:], scalar1=mx8[i2][:, k:k + 1], scalar2=None, op0=ALU.is_equal)
                                                   for k in range(4)][-1],
                          r=[tg + "L", tg + "mx"], w=[tg + "oh"])
                        C("dve", lambda e, i2=i2: e.tensor_tensor(out=oh[i2][:], in0=oh[i2][:], in1=posc[i2][:].to_broadcast([128, 4, E]), op=ALU.mult),
                          r=[tg + "oh", tg + "pc"], w=[tg + "oh"])
                        C("dve", lambda e, i2=i2: e.tensor_reduce(out=dstf[i2][:], in_=oh[i2][:], axis=AX.X, op=ALU.add), r=[tg + "oh"], w=[tg + "df"])
                        C("dve", lambda e, i2=i2, ti=ti: e.tensor_copy(out=self.desti[:, ti, :], in_=dstf[i2][:]), r=[tg + "df"], w=[("desti", ti)])
                        for k in range(4):
                            Dq("pool", lambda e, i2=i2, ti=ti, k=k: e.indirect_dma_start(
                                out=d["xsbuf"].ap(), out_offset=bass.IndirectOffsetOnAxis(ap=self.desti[:, ti, k:k + 1], axis=0),
                                in_=x1b[i2][:], in_offset=None),
                               r=[("x1b", i2), ("desti", ti)], w=[("xsd", ti, k)])
                    self.phase_end("A3b")

    def moe(self, l):
        nc, d, C, Dq, sb = self.nc, self.d, self.C, self.Dq, self.sb
        ps, psb = self.ps, self.psb
        with ExitStack() as es:
            wgu = [sb(es, "wgu%d" % i, [128, 8, 2 * D], BF16) for i in range(2)]
            wdn = [sb(es, "wdn%d" % i, [128, 8, D], BF16) for i in range(2)]
            xs = sb(es, "xs", [128, NR, D], BF16)
            xsT = sb(es, "xsT", [128, 8, CAP], BF16)
            actT = sb(es, "actT", [128, 8, CAP], BF16)
            hg = [sb(es, "hg%d" % i, [128, 2, CAP // 2], F32) for i in range(2)]
            hu = [sb(es, "hu%d" % i, [128, 2, CAP // 2], F32) for i in range(2)]
            sgm = [sb(es, "sgm%d" % i, [128, 2, CAP // 2], F32) for i in range(2)]
            tt_ = [sb(es, "tt%d" % i, [128, 2, CAP // 2], F32) for i in range(2)]
            yst = [sb(es, "yst%d" % i, [128, D], F32) for i in range(2)]
            bdn = [sb(es, "bdn%d" % i, [128, D], F32) for i in range(2)]
            bgu = sb(es, "bgu", [128, E, 16], F32)
            Dq("sp", lambda e: e.dma_start(out=bgu[:], in_=d["b_gu"].ap()[l]), w=["bgu"])
            C("dve", lambda e: e.tensor_scalar(out=bgu[:, :, 8:16], in0=bgu[:, :, 8:16], scalar1=1.0, scalar2=None, op0=ALU.add), r=["bgu"], w=["bgu"])
            H = CAP // 2

            def load_w(e_):
                s = e_ % 2
                gv = d["w_gu"].ap()[l, e_].rearrange("(kc p) n -> p kc n", p=128)
                dv = d["w_down"].ap()[l, e_].rearrange("(kc p) n -> p kc n", p=128)
                for q in range(4):
                    Dq("pool", lambda e, s=s, q=q, gv=gv: e.dma_start(out=wgu[s][:, 2 * q:2 * q + 2, :], in_=gv[:, 2 * q:2 * q + 2, :]), w=[("wgu", s, q)])
                for q in range(2):
                    Dq("pool", lambda e, s=s, q=q, dv=dv: e.dma_start(out=wdn[s][:, 4 * q:4 * q + 4, :], in_=dv[:, 4 * q:4 * q + 4, :]), w=[("wdn", s, q)])
                Dq("sp", lambda e, s=s, e_=e_: e.dma_start(out=bdn[s][:], in_=d["b_down"].ap()[l, e_:e_ + 1].to_broadcast([128, D])), w=[("bdn", s)])

            xsd_keys = [("xsd", ti, k) for ti in range(NT) for k in range(4)]
            load_w(0)
            cnt = 0
            for e_ in range(E):
                s = e_ % 2
                if e_ + 1 < E:
                    load_w(e_ + 1)
                Dq("sp", lambda e, e_=e_: e.dma_start(out=xs[:], in_=d["xsbuf"].ap()[e_ * CAP:(e_ + 1) * CAP, :].rearrange("(r p) n -> p r n", p=128)),
                   w=["xs"])
                for r in range(NR):
                    bk = 6 + r % 2
                    C("pe", lambda e, r=r, bk=bk: [e.transpose(out=psb[:, bk, c * 128:(c + 1) * 128], in_=xs[:, r, c * 128:(c + 1) * 128], identity=self.identb[:])
                                                   for c in range(8)][-1],
                      r=["xs", "k0"], w=[("ps", bk)])
                    eng = "act" if r % 2 == 0 else "dve"
                    if eng == "act":
                        C("act", lambda e, r=r, bk=bk: e.activation(out=xsT[:, :, r * 128:(r + 1) * 128], in_=psb[:, bk, :].rearrange("p (c t) -> p c t", c=8), func=AF.Copy),
                          r=[("ps", bk)], w=[("xsT", r)])
                    else:
                        C("dve", lambda e, r=r, bk=bk: e.tensor_copy(out=xsT[:, :, r * 128:(r + 1) * 128], in_=psb[:, bk, :].rearrange("p (c t) -> p c t", c=8)),
                          r=[("ps", bk)], w=[("xsT", r)])
                xk = [("xsT", r) for r in range(NR)]
                for c in range(8):
                    i2 = cnt % 2
                    cnt += 1
                    for gu in range(2):
                        col0 = gu * D + c * 128
                        for hf in range(2):
                            bk = gu * 2 + hf
                            C("pe", lambda e, s=s, col0=col0, hf=hf, bk=bk: [e.matmul(out=ps[:, bk, 0:H], lhsT=wgu[s][:, kc, col0:col0 + 128],
                                                                                    rhs=xsT[:, kc, hf * H:(hf + 1) * H], start=(kc == 0), stop=(kc == 7)) for kc in range(8)][-1],
                              r=xk + [("wgu", s, q) for q in range(4)], w=[("ps", bk)])
                    C("dve", lambda e, i2=i2, e_=e_, c=c: e.tensor_scalar(out=hg[i2][:], in0=ps[:, 0:2, 0:H], scalar1=bgu[:, e_, c:c + 1], scalar2=SW_LIM,
                                                                         op0=ALU.add, op1=ALU.min),
                      r=[("ps", 0), ("ps", 1), "bgu"], w=[("hg", i2)])
                    C("dve", lambda e, i2=i2, e_=e_, c=c: e.tensor_scalar(out=hu[i2][:], in0=ps[:, 2:4, 0:H], scalar1=bgu[:, e_, 8 + c:9 + c], scalar2=SW_LIM + 1.0,
                                                                         op0=ALU.add, op1=ALU.min),
                      r=[("ps", 2), ("ps", 3), "bgu"], w=[("hu", i2)])
                    C("act", lambda e, i2=i2: e.activation(out=sgm[i2][:], in_=hg[i2][:], func=AF.Sigmoid, scale=SW_ALPHA), r=[("hg", i2)], w=[("sgm", i2)])
                    C("dve", lambda e, i2=i2: e.scalar_tensor_tensor(out=tt_[i2][:], in0=hu[i2][:], scalar=-SW_LIM + 1.0, in1=hg[i2][:], op0=ALU.max, op1=ALU.mult),
                      r=[("hu", i2), ("hg", i2)], w=[("tt", i2)])
                    C("pool", lambda e, i2=i2, c=c: e.tensor_tensor(out=actT[:, c, :].rearrange("p (a n) -> p a n", a=2), in0=tt_[i2][:], in1=sgm[i2][:], op=ALU.mult),
                      r=[("tt", i2), ("sgm", i2)], w=[("actT", c)])
                ak = [("actT", c) for c in range(8)]
                for r in range(NR):
                    ys = r % 2
                    for n in range(2):
                        bk = 4 + n
                        C("pe", lambda e, s=s, r=r, n=n, bk=bk: [e.matmul(out=ps[:, bk, :], lhsT=actT[:, c, r * 128:(r + 1) * 128], rhs=wdn[s][:, c, n * 512:(n + 1) * 512],
                                                                         start=(c == 0), stop=(c == 7)) for c in range(8)][-1],
                          r=ak + [("wdn", s, 0), ("wdn", s, 1)], w=[("ps", bk)])
                        C("dve", lambda e, s=s, ys=ys, n=n, bk=bk: e.tensor_tensor(out=yst[ys][:, n * 512:(n + 1) * 512], in0=ps[:, bk, :], in1=bdn[s][:, n * 512:(n + 1) * 512], op=ALU.add),
                          r=[("ps", bk), ("bdn", s)], w=[("yst", ys, n)])
                    Dq("sp", lambda e, ys=ys, e_=e_, r=r: e.dma_start(out=d["ybuf"].ap()[e_ * CAP + r * 128: e_ * CAP + (r + 1) * 128, :], in_=yst[ys][:]),
                       r=[("yst", ys, 0), ("yst", ys, 1)], w=[("ybuf", e_, r)])
            self.phase_end("moe")

    def combine(self, l, xdst):
        nc, d, C, Dq, sb = self.nc, self.d, self.C, self.Dq, self.sb
        with ExitStack() as es:
            gb = sb(es, "g2b", [128, D], F32)
            bb = sb(es, "b2b", [128, D], F32)
            Dq("sp", lambda e: e.dma_start(out=gb[:], in_=d["ln2_g"].ap()[l:l + 1].to_broadcast([128, D])), w=["lnp"])
            Dq("sp", lambda e: e.dma_start(out=bb[:], in_=d["ln2_b"].ap()[l:l + 1].to_broadcast([128, D])), w=["lnp"])
            yk = [[sb(es, "yk%d_%d" % (i, k), [128, D], F32) for k in range(4)] for i in range(2)]
            x1 = [sb(es, "cx1%d" % i, [128, D], F32) for i in range(2)]
            acc = [sb(es, "acc%d" % i, [128, D], F32) for i in range(2)]
            zt = [sb(es, "cz%d" % i, [128, D], F32) for i in range(2)]
            ot = [sb(es, "cot%d" % i, [128, D], F32) for i in range(2)]
            lnt = [(sb(es, "cst%d" % i, [128, 2, 6], F32), sb(es, "cmv%d" % i, [128, 2], F32), sb(es, "csd%d" % i, [128, 1], F32),
                    sb(es, "crstd%d" % i, [128, 1], F32), sb(es, "cxn%d" % i, [128, D], F32)) for i in range(2)]
            for ti in range(NT):
                i2 = ti % 2
                tg = "c%d_" % i2
                rows = slice(ti * 128, (ti + 1) * 128)
                for k in range(4):
                    gn = ti * 4 + k
                    Dq("pool", lambda e, i2=i2, ti=ti, k=k: e.indirect_dma_start(
                        out=yk[i2][k][:], out_offset=None, in_=d["ybuf"].ap(),
                        in_offset=bass.IndirectOffsetOnAxis(ap=self.desti[:, ti, k:k + 1], axis=0)),
                       r=[("gch", gn - 2)], w=[("yk", i2, k), ("gch", gn)])
                import os
                cut = os.environ.get("COMBINE_CUT", "full")
                if cut == "gather":
                    C("dve", lambda e, i2=i2: e.tensor_copy(out=ot[i2][:], in_=yk[i2][0][:]), r=[("yk", i2, 0)], w=[("cot", i2)])
                    for k in range(1, 4):
                        C("dve", lambda e, i2=i2, k=k: e.tensor_tensor(out=ot[i2][:], in0=ot[i2][:], in1=yk[i2][k][:], op=ALU.add), r=[("yk", i2, k), ("cot", i2)], w=[("cot", i2)])
                    Dq("sp", lambda e, i2=i2, rows=rows: e.dma_start(out=xdst.ap()[rows, :], in_=ot[i2][:]), r=[("cot", i2)], w=[("xdst", ti)])
                    continue
                Dq("sp", lambda e, i2=i2, rows=rows: e.dma_start(out=x1[i2][:], in_=d["xres"].ap()[rows, :]), w=[("cx1", i2)])
                C("dve", lambda e, i2=i2, ti=ti: e.tensor_scalar(out=acc[i2][:], in0=yk[i2][0][:], scalar1=self.gate[:, ti, 0:1], scalar2=None, op0=ALU.mult),
                  r=[("yk", i2, 0)], w=[("acc", i2)])
                for k in range(1, 4):
                    C("dve", lambda e, i2=i2, ti=ti, k=k: e.scalar_tensor_tensor(out=acc[i2][:], in0=yk[i2][k][:], scalar=self.gate[:, ti, k:k + 1], in1=acc[i2][:],
                                                                                 op0=ALU.mult, op1=ALU.add),
                      r=[("yk", i2, k), ("acc", i2)], w=[("acc", i2)])
                C("dve", lambda e, i2=i2: e.scalar_tensor_tensor(out=zt[i2][:], in0=x1[i2][:], scalar=ALPHA, in1=acc[i2][:], op0=ALU.mult, op1=ALU.add),
                  r=[("cx1", i2), ("acc", i2)], w=[tg + "zz"])
                if cut == "acc":
                    Dq("sp", lambda e, i2=i2, rows=rows: e.dma_start(out=xdst.ap()[rows, :], in_=zt[i2][:]), r=[tg + "zz"], w=[("xdst", ti)])
                    continue
                self.layer_norm_tile(lnt[i2], zt[i2], gb, bb, ot[i2][:], [tg + "zz"], ("cot", i2), tg)
                Dq("sp", lambda e, i2=i2, rows=rows: e.dma_start(out=xdst.ap()[rows, :], in_=ot[i2][:]), r=[("cot", i2)], w=[("xdst", ti)])
            self.phase_end("combine")


def _consts():
    c = {}
    c["c_identb"] = np.eye(128, dtype=np.float32).astype(ml_dtypes.bfloat16)
    c["c_identf"] = np.eye(128, dtype=np.float32)
    k = np.arange(128)[:, None]
    q = np.arange(128)[None, :]
    c["c_mask01"] = (q >= k).astype(np.float32).astype(ml_dtypes.bfloat16)
    c["c_ustrict"] = (k < q).astype(np.float32)
    c["c_ones"] = np.ones((128, 128), np.float32)
    c["c_ec"] = np.broadcast_to((np.arange(E, dtype=np.float32) * CAP)[None, :], (128, E)).copy()
    inv = (ROPE_THETA ** (-np.arange(0, 16, 2, dtype=np.float32) / np.float32(16))).astype(np.float32)
    c["c_invfreq"] = np.broadcast_to(inv[None, :], (128, 8)).copy()
    ic = np.zeros((4, 16), np.float32)
    for g, w in enumerate((2, 4, 8, 16)):
        ic[g] = 1.0 / np.minimum(np.arange(1, 17), w).astype(np.float32)
    c["c_invcnt"] = np.broadcast_to(ic[None], (128, 4, 16)).copy()
    return c


def _layout(inp):
    f = lambda a: np.ascontiguousarray(np.asarray(a))
    w_in = f(inp["w_in"])
    sh = {}
    sh["w_pool"] = f(w_in[:, :, 0:512])
    q = w_in[:, :, 512:1536].reshape(L, D, NH, 128)
    k = w_in[:, :, 1536:2560].reshape(L, D, NH, 128)
    v = w_in[:, :, 2560:3584].reshape(L, D, NH, 128)
    sh["w_qkv"] = f(np.concatenate([q, k, v], axis=-1).transpose(0, 2, 1, 3))
    sh["w_gate"] = f(w_in[:, :, 3584:5632])
    sh["pool_w"] = f(inp["pool_w"])
    sh["pool_scale"] = f(np.asarray(inp["pool_scale"]).reshape(L, 4, 128).transpose(0, 2, 1))
    sh["w_pb"] = f(inp["w_pool_branch"])
    sh["w_ab"] = f(inp["w_attn_branch"])
    sh["lam4"] = f(np.stack([np.asarray(inp["lambda_q1"]), np.asarray(inp["lambda_k1"]),
                             np.asarray(inp["lambda_q2"]), np.asarray(inp["lambda_k2"])], axis=1))
    sh["subln_w"] = f(inp["subln_w"])
    sh["w_out"] = f(inp["w_out"])
    for n in ("ln1_g", "ln1_b", "w_router", "b_router", "w_gu", "w_down", "b_down", "ln2_g", "ln2_b"):
        sh[n] = f(inp[n])
    sh["b_gu"] = f(np.asarray(inp["b_gu"]).reshape(L, E, 16, 128).transpose(0, 3, 1, 2))
    sh.update(_consts())
    return sh


_NC_CACHE = {}


def kernel(**inputs):
    shared = _layout(inputs)
    x = np.asarray(inputs["x"], dtype=np.float32).reshape(NCORES, T, D)
    pos = np.asarray(inputs["positions"]).astype(np.int32).reshape(NCORES, NSEQ, 16, 128).transpose(0, 1, 3, 2)
    if "nc" not in _NC_CACHE:
        _NC_CACHE["nc"] = Builder().build()
    nc = _NC_CACHE["nc"]
    in_maps = []
    for c in range(NCORES):
        m = dict(shared)
        m["x"] = np.ascontiguousarray(x[c])
        m["pos"] = np.ascontiguousarray(pos[c])
        in_maps.append(m)
    res = run_bass_kernel_spmd(nc, in_maps, core_ids=list(range(NCORES)))
    out = np.stack([np.asarray(r["y"]) for r in res.results], axis=0)
    return out.reshape(16, S, D).astype(np.float32)
```

```python
import math
from contextlib import ExitStack

import numpy as np
import ml_dtypes

import concourse.bass as bass
import concourse.mybir as mybir
from concourse.bass_utils import run_bass_kernel_spmd

F32 = mybir.dt.float32
BF16 = mybir.dt.bfloat16
I32 = mybir.dt.int32
AF = mybir.ActivationFunctionType
ALU = mybir.AluOpType
AX = mybir.AxisListType

NCORES = 8
D = 1024
S = 2048
NSEQ = 2
T = NSEQ * S
NT = T // 128
L = 2
NH = 8
E = 32
TOPK = 4
CAP = 768
NR = CAP // 128
LN_EPS = 1e-5
ALPHA = (2 * L) ** 0.25
ROPE_THETA = 500000.0
SW_ALPHA = 1.702
SW_LIM = 7.0
SAME_ENG_SYNC = True


class _Op:
    __slots__ = ("eng", "kind", "fn", "reads", "writes", "deps", "need", "sem", "val", "prev")

    def __init__(self, eng, kind, fn, reads, writes):
        self.eng, self.kind, self.fn = eng, kind, fn
        self.reads, self.writes = tuple(reads), tuple(writes)
        self.deps, self.need, self.sem, self.val, self.prev = (), False, None, 0, 0


class Prog:
    ENGS = ("pe", "act", "dve", "pool", "sp")

    def __init__(self, nc, es):
        self.nc = nc
        self.sems = []
        self.csem = {}
        for e in self.ENGS:
            self.csem[e] = len(self.sems)
            self.sems.append(es.enter_context(nc.semaphore("c_" + e)))
        self.ccnt = {e: 0 for e in self.ENGS}
        self.lanes = {}
        for q, n in (("sp", 14), ("pool", 14), ("act", 2)):
            self.lanes[q] = []
            for i in range(n):
                self.lanes[q].append(len(self.sems))
                self.sems.append(es.enter_context(nc.semaphore("l_%s%d" % (q, i))))
        self.lcnt = {}
        for q in self.lanes:
            for s in self.lanes[q]:
                self.lcnt[s] = 0
        self.lnext = {q: 0 for q in self.lanes}
        self.waited = {e: {} for e in self.ENGS}
        self.ops = []

    def op(self, eng, kind, fn, reads=(), writes=()):
        self.ops.append(_Op(eng, kind, fn, reads, writes))

    def flush(self):
        ops, self.ops = self.ops, []
        if not ops:
            return
        last_w, readers = {}, {}
        for i, op in enumerate(ops):
            deps = set()
            for k in op.reads:
                if k in last_w:
                    deps.add(last_w[k])
                if isinstance(k, tuple) and k[0] == "ps":
                    for r in readers.get(k, ()):
                        if ops[r].eng != op.eng:
                            deps.add(r)
            for k in op.writes:
                if k in last_w:
                    deps.add(last_w[k])
                for r in readers.get(k, ()):
                    deps.add(r)
            deps.discard(i)
            fd = []
            for d in deps:
                p = ops[d]
                if p.kind == "c" and op.kind == "c" and p.eng == op.eng:
                    if op.eng == "pe" or not SAME_ENG_SYNC:
                        continue
                fd.append(d)
            op.deps = fd
            for d in fd:
                ops[d].need = True
            for k in op.reads:
                readers.setdefault(k, []).append(i)
            for k in op.writes:
                last_w[k] = i
                readers[k] = []
        lastc = {}
        for i, op in enumerate(ops):
            if op.kind == "c" and op.fn is not None:
                lastc[op.eng] = i
        for i in lastc.values():
            ops[i].need = True
        for op in ops:
            if op.kind == "c":
                if op.need:
                    self.ccnt[op.eng] += 1
                    op.sem, op.val = self.csem[op.eng], self.ccnt[op.eng]
            else:
                q = self.lanes[op.eng]
                s = q[self.lnext[op.eng] % len(q)]
                self.lnext[op.eng] += 1
                op.prev = self.lcnt[s]
                self.lcnt[s] += 16
                op.sem, op.val = s, self.lcnt[s]
        final = {self.csem[e]: self.ccnt[e] for e in self.ENGS}
        final.update(self.lcnt)
        with self.nc.Block() as blk:
            decos = (("pe", blk.tensor), ("act", blk.scalar), ("dve", blk.vector),
                     ("pool", blk.gpsimd), ("sp", blk.sync))
            for ename, deco in decos:
                def body(eng, ename=ename):
                    self._emit(ename, eng, ops, final)
                deco(body)

    def _wait(self, ename, eng, s, v):
        if v <= 0:
            return
        w = self.waited[ename]
        if w.get(s, 0) >= v:
            return
        eng.wait_ge(self.sems[s], v)
        w[s] = v

    def _emit(self, ename, eng, ops, final):
        for op in ops:
            if op.eng != ename:
                continue
            need = {}
            for d in op.deps:
                p = ops[d]
                if need.get(p.sem, 0) < p.val:
                    need[p.sem] = p.val
            if op.kind == "d" and op.prev > 0:
                if need.get(op.sem, 0) < op.prev:
                    need[op.sem] = op.prev
            for s, v in need.items():
                self._wait(ename, eng, s, v)
            if op.fn is None:
                continue
            ins = op.fn(eng)
            if op.kind == "d":
                ins.then_inc(self.sems[op.sem], 16)
            elif op.need:
                ins.then_inc(self.sems[op.sem], 1)
        for s, v in final.items():
            self._wait(ename, eng, s, v)


class StopBuild(Exception):
    pass


class Builder:
    def __init__(self, n_layers=L, debug=False, stop_after=None):
        self.stop_after = stop_after
        self.n_layers = n_layers
        self.debug = debug
        self.nc = bass.Bass("TRN2", target_bir_lowering=False)
        self.es = ExitStack()

    def phase_end(self, name):
        if self.debug:
            print("phase", name, "sbuf cur", self._cur, "peak", self._peak)
        self._peak = self._cur
        self.P.flush()
        if self.stop_after == name:
            raise StopBuild()

    def C(self, eng, fn, r=(), w=()):
        self.P.op(eng, "c", fn, r, w)

    def Dq(self, q, fn, r=(), w=()):
        self.P.op(q, "d", fn, r, w)

    def sb(self, es, name, shape, dt):
        self._uid = getattr(self, "_uid", 0) + 1
        nb = int(np.prod(shape[1:])) * (4 if dt in (F32, I32) else 2)
        nb = (nb + 31) // 32 * 32
        self._cur = getattr(self, "_cur", 0) + nb
        self._peak = max(getattr(self, "_peak", 0), self._cur)
        t = es.enter_context(self.nc.sbuf_tensor("%s_u%d" % (name, self._uid), list(shape), dt))

        def _rel(nb=nb):
            self._cur -= nb
        es.callback(_rel)
        return t

    def din(self, name, shape, dt):
        return self.nc.dram_tensor(name, list(shape), dt, kind="ExternalInput")

    def build(self):
        nc = self.nc
        es = self.es
        nl = self.n_layers
        d = {}
        d["x"] = self.din("x", [T, D], F32)
        d["pos"] = self.din("pos", [NSEQ, 128, 16], I32)
        d["w_pool"] = self.din("w_pool", [L, D, 512], F32)
        d["w_qkv"] = self.din("w_qkv", [L, NH, D, 384], F32)
        d["w_gate"] = self.din("w_gate", [L, D, 2048], F32)
        d["pool_w"] = self.din("pool_w", [L, 4, 128, 128], F32)
        d["pool_scale"] = self.din("pool_scale", [L, 128, 4], F32)
        d["w_pb"] = self.din("w_pb", [L, 512, D], F32)
        d["w_ab"] = self.din("w_ab", [L, D, D], F32)
        d["lam4"] = self.din("lam4", [L, 4, 64], F32)
        d["subln_w"] = self.din("subln_w", [L, 128], F32)
        d["w_out"] = self.din("w_out", [L, D, D], F32)
        d["ln1_g"] = self.din("ln1_g", [L, D], F32)
        d["ln1_b"] = self.din("ln1_b", [L, D], F32)
        d["w_router"] = self.din("w_router", [L, D, E], F32)
        d["b_router"] = self.din("b_router", [L, E], F32)
        d["w_gu"] = self.din("w_gu", [L, E, D, 2 * D], F32)
        d["b_gu"] = self.din("b_gu", [L, 128, E, 16], F32)
        d["w_down"] = self.din("w_down", [L, E, D, D], F32)
        d["b_down"] = self.din("b_down", [L, E, D], F32)
        d["ln2_g"] = self.din("ln2_g", [L, D], F32)
        d["ln2_b"] = self.din("ln2_b", [L, D], F32)
        d["c_identb"] = self.din("c_identb", [128, 128], BF16)
        d["c_identf"] = self.din("c_identf", [128, 128], F32)
        d["c_mask01"] = self.din("c_mask01", [128, 128], BF16)
        d["c_ustrict"] = self.din("c_ustrict", [128, 128], F32)
        d["c_ones"] = self.din("c_ones", [128, 128], F32)
        d["c_ec"] = self.din("c_ec", [128, E], F32)
        d["c_invfreq"] = self.din("c_invfreq", [128, 8], F32)
        d["c_invcnt"] = self.din("c_invcnt", [128, 4, 16], F32)
        d["y"] = nc.dram_tensor("y", [T, D], F32, kind="ExternalOutput")
        d["xres"] = nc.dram_tensor("xres", [T, D], F32, kind="Internal")
        d["xcur"] = nc.dram_tensor("xcur", [T, D], F32, kind="Internal")
        d["xsbuf"] = nc.dram_tensor("xsbuf", [E * CAP, D], BF16, kind="Internal")
        d["ybuf"] = nc.dram_tensor("ybuf", [E * CAP, D], F32, kind="Internal")
        if self.debug:
            d["dbg_x1"] = nc.dram_tensor("dbg_x1", [T, D], F32, kind="ExternalOutput")
        self.d = d

        self.P = Prog(nc, es)
        self.ps = es.enter_context(nc.psum_tensor("ps", [128, 8, 512], F32))
        self.psb = self.ps.bitcast(BF16)
        sb = self.sb
        self.identb = sb(es, "identb", [128, 128], BF16)
        self.identf = sb(es, "identf", [128, 128], F32)
        self.mask01 = sb(es, "mask01", [128, 128], BF16)
        self.ustrict = sb(es, "ustrict", [128, 128], F32)
        self.ones = sb(es, "ones", [128, 128], F32)
        self.ec = sb(es, "ec", [128, E], F32)
        self.invfreq = sb(es, "invfreq", [128, 1, 8], F32)
        self.invcnt = sb(es, "invcnt", [128, 4, 16], F32)
        self.gate = sb(es, "gate", [128, NT, TOPK], F32)
        self.desti = sb(es, "desti", [128, NT, TOPK], I32)
        self.macc = sb(es, "macc", [128, E], F32)
        self.epsc = sb(es, "epsc", [128, 1], F32)

        def ld(dst, src, key):
            self.Dq("sp", lambda e: e.dma_start(out=dst, in_=src), w=[key])
        ld(self.identb[:], d["c_identb"].ap(), "k0")
        ld(self.identf[:], d["c_identf"].ap(), "k1")
        ld(self.mask01[:], d["c_mask01"].ap(), "k2")
        ld(self.ustrict[:], d["c_ustrict"].ap(), "k3")
        ld(self.ones[:], d["c_ones"].ap(), "k4")
        ld(self.ec[:], d["c_ec"].ap(), "k5")
        ld(self.invfreq[:, 0, :], d["c_invfreq"].ap(), "k6")
        ld(self.invcnt[:], d["c_invcnt"].ap(), "k7")
        self.C("dve", lambda e: e.memset(self.epsc[:], LN_EPS), w=["k8"])
        with ExitStack() as es2:
            z = self.sb(es2, "zrows", [128, 6, D], BF16)
            self.C("dve", lambda e: e.memset(z[:], 0.0), w=["z"])
            xv = d["xsbuf"].ap().rearrange("(n r p) d -> n p r d", p=128, r=6)
            for n in range(E * CAP // (128 * 6)):
                self.Dq("sp", lambda e, n=n: e.dma_start(out=xv[n], in_=z[:]), r=["z"], w=[("xs0", n)])
            self.phase_end("init")

        try:
            for l in range(nl):
                xsrc = d["x"] if l == 0 else d["xcur"]
                xdst = d["y"] if l == nl - 1 else d["xcur"]
                self.C("dve", lambda e: e.memset(self.macc[:], 0.0), w=["macc"])
                for b in range(NSEQ):
                    self.mixer(l, b, xsrc)
                self.moe(l)
                self.combine(l, xdst)
        except StopBuild:
            return nc
        self.es.close()
        return nc

    def layer_norm_tile(self, es_tiles, z, gb, bb, out, zkeys, outkey, tag):
        st, mv, sd, rstd, xn = es_tiles
        C = self.C
        C("dve", lambda e: [e.bn_stats(out=st[:, 0, :], in_=z[:, 0:512]),
                             e.bn_stats(out=st[:, 1, :], in_=z[:, 512:1024])][-1],
          r=list(zkeys), w=[tag + "st"])
        C("dve", lambda e: e.bn_aggr(out=mv[:], in_=st[:].rearrange("p a b -> p (a b)")), r=[tag + "st"], w=[tag + "mv"])
        C("act", lambda e: e.activation(out=sd[:], in_=mv[:, 1:2], func=AF.Sqrt, bias=self.epsc[:], scale=1.0),
          r=[tag + "mv"], w=[tag + "sd"])
        C("dve", lambda e: e.reciprocal(out=rstd[:], in_=sd[:]), r=[tag + "sd"], w=[tag + "rs"])
        C("dve", lambda e: e.tensor_scalar(out=xn[:], in0=z[:], scalar1=mv[:, 0:1], scalar2=rstd[:],
                                           op0=ALU.subtract, op1=ALU.mult), r=list(zkeys) + [tag + "mv", tag + "rs"], w=[tag + "xn"])
        C("pool", lambda e: e.tensor_tensor(out=xn[:], in0=xn[:], in1=gb[:], op=ALU.mult), r=[tag + "xn", "lnp"], w=[tag + "xn"])
        C("pool", lambda e: e.tensor_tensor(out=out, in0=xn[:], in1=bb[:], op=ALU.add), r=[tag + "xn", "lnp"], w=[outkey])

    def mixer(self, l, b, xsrc):
        nc, d, C, Dq, sb = self.nc, self.d, self.C, self.Dq, self.sb
        ps, psb = self.ps, self.psb
        tok0 = b * S
        lam_init = 0.8 - 0.6 * math.exp(-0.3 * l)
        if True:
            with ExitStack() as es_o:
                xT = sb(es_o, "xT", [128, 8, S], BF16)
                ypre = sb(es_o, "ypre", [128, 4, S], BF16)
                onT = sb(es_o, "onT", [128, 8, S], BF16)
                cos = sb(es_o, "cos", [128, 16, 8], F32)
                sin = sb(es_o, "sin", [128, 16, 8], F32)
                neglam = sb(es_o, "neglam", [128, 1], F32)
                sublnw = sb(es_o, "sublnw", [128, 1, 128], F32)
                with ExitStack() as es1:
                    xst = [sb(es1, "xst%d" % i, [128, D], F32) for i in range(2)]
                    xb = [sb(es1, "xb%d" % i, [128, D], BF16) for i in range(2)]
                    for tt in range(16):
                        i2 = tt % 2
                        Dq("sp", lambda e, tt=tt, i2=i2: e.dma_start(out=xst[i2][:], in_=xsrc.ap()[tok0 + tt * 128: tok0 + (tt + 1) * 128, :]),
                           w=[("xst", i2)])
                        C("act", lambda e, i2=i2: e.activation(out=xb[i2][:], in_=xst[i2][:], func=AF.Copy),
                          r=[("xst", i2)], w=[("xb", i2)])
                        C("pe", lambda e, i2=i2: [e.transpose(out=psb[:, i2, c * 128:(c + 1) * 128], in_=xb[i2][:, c * 128:(c + 1) * 128],
                                                              identity=self.identb[:]) for c in range(8)][-1],
                          r=[("xb", i2), "k0"], w=[("ps", i2)])
                        C("dve", lambda e, tt=tt, i2=i2: e.tensor_copy(out=xT[:, :, tt * 128:(tt + 1) * 128],
                                                                     in_=psb[:, i2, :].rearrange("p (c t) -> p c t", c=8)),
                          r=[("ps", i2)], w=[("xT", tt)])
                    posi = sb(es1, "posi", [128, 16], I32)
                    posf = sb(es1, "posf", [128, 16, 1], F32)
                    ang = sb(es1, "ang", [128, 16, 8], F32)
                    kf = sb(es1, "kf", [128, 16, 8], F32)
                    ki = sb(es1, "ki", [128, 16, 8], I32)
                    r2 = sb(es1, "r2", [128, 16, 8], F32)
                    yy = sb(es1, "yy", [128, 16, 8], F32)
                    mm = sb(es1, "mm", [128, 16, 8], F32)
                    Dq("sp", lambda e: e.dma_start(out=posi[:], in_=d["pos"].ap()[b]), w=["posi"])
                    C("dve", lambda e: e.tensor_copy(out=posf[:, :, 0], in_=posi[:]), r=["posi"], w=["posf"])
                    C("dve", lambda e: e.tensor_tensor(out=ang[:], in0=posf[:].to_broadcast([128, 16, 8]),
                                                       in1=self.invfreq[:].to_broadcast([128, 16, 8]), op=ALU.mult),
                      r=["posf", "k6"], w=["ang"])
                    TWO_PI = 2.0 * math.pi
                    C1 = 6.28125
                    C2 = TWO_PI - C1
                    C("dve", lambda e: e.tensor_scalar(out=kf[:], in0=ang[:], scalar1=1.0 / TWO_PI, scalar2=None, op0=ALU.mult),
                      r=["ang"], w=["kf"])
                    C("dve", lambda e: e.tensor_copy(out=ki[:], in_=kf[:]), r=["kf"], w=["ki"])
                    C("dve", lambda e: e.tensor_copy(out=kf[:], in_=ki[:]), r=["ki"], w=["kf"])
                    C("dve", lambda e: e.scalar_tensor_tensor(out=r2[:], in0=kf[:], scalar=-C1, in1=ang[:], op0=ALU.mult, op1=ALU.add),
                      r=["kf", "ang"], w=["r2"])
                    C("dve", lambda e: e.scalar_tensor_tensor(out=r2[:], in0=kf[:], scalar=-C2, in1=r2[:], op0=ALU.mult, op1=ALU.add),
                      r=["kf", "r2"], w=["r2"])
                    for shift, dst, nm in ((0.0, sin, "sin"), (math.pi / 2, cos, "cos")):
                        C("dve", lambda e, shift=shift: e.tensor_scalar(out=yy[:], in0=r2[:], scalar1=shift, scalar2=None, op0=ALU.add),
                          r=["r2"], w=["yy"])
                        C("dve", lambda e: e.tensor_scalar(out=mm[:], in0=yy[:], scalar1=math.pi, scalar2=None, op0=ALU.is_gt),
                          r=["yy"], w=["mm"])
                        C("dve", lambda e: e.scalar_tensor_tensor(out=yy[:], in0=mm[:], scalar=-TWO_PI, in1=yy[:], op0=ALU.mult, op1=ALU.add),
                          r=["mm", "yy"], w=["yy"])
                        C("dve", lambda e: e.tensor_scalar(out=mm[:], in0=yy[:], scalar1=-math.pi, scalar2=None, op0=ALU.is_lt),
                          r=["yy"], w=["mm"])
                        C("dve", lambda e: e.scalar_tensor_tensor(out=yy[:], in0=mm[:], scalar=TWO_PI, in1=yy[:], op0=ALU.mult, op1=ALU.add),
                          r=["mm", "yy"], w=["yy"])
                        C("dve", lambda e: e.tensor_scalar(out=yy[:], in0=yy[:], scalar1=math.pi, scalar2=-math.pi, op0=ALU.min, op1=ALU.max),
                          r=["yy"], w=["yy"])
                        C("act", lambda e, dst=dst: e.activation(out=dst[:], in_=yy[:], func=AF.Sin), r=["yy"], w=[nm])
                    l4 = sb(es1, "l4", [128, 4, 64], F32)
                    lp = sb(es1, "lp", [128, 2, 64], F32)
                    lsum = sb(es1, "lsum", [128, 2], F32)
                    lex = sb(es1, "lex", [128, 2], F32)
                    Dq("sp", lambda e: e.dma_start(out=l4[:], in_=d["lam4"].ap()[l:l + 1].to_broadcast([128, 4, 64])), w=["l4"])
                    C("dve", lambda e: e.tensor_tensor(out=lp[:], in0=l4[:, 0:4:2, :], in1=l4[:, 1:4:2, :], op=ALU.mult), r=["l4"], w=["lp"])
                    C("dve", lambda e: e.tensor_reduce(out=lsum[:], in_=lp[:], axis=AX.X, op=ALU.add), r=["lp"], w=["lsum"])
                    C("act", lambda e: e.activation(out=lex[:], in_=lsum[:], func=AF.Exp), r=["lsum"], w=["lex"])
                    C("dve", lambda e: e.tensor_tensor(out=neglam[:], in0=lex[:, 1:2], in1=lex[:, 0:1], op=ALU.subtract), r=["lex"], w=["neglam"])
                    C("dve", lambda e: e.tensor_scalar(out=neglam[:], in0=neglam[:], scalar1=-lam_init, scalar2=None, op0=ALU.add),
                      r=["neglam"], w=["neglam"])
                    Dq("sp", lambda e: e.dma_start(out=sublnw[:, 0, :], in_=d["subln_w"].ap()[l:l + 1].to_broadcast([128, 128])), w=["sublnw"])
                    C("act", lambda e: e.activation(out=sublnw[:], in_=sublnw[:], func=AF.Copy, scale=(1.0 - lam_init)), r=["sublnw"], w=["sublnw"])
                    wpool = sb(es1, "wpool", [128, 8, 512], BF16)
                    poolw = sb(es1, "poolw", [128, 4, 128], BF16)
                    pscale = sb(es1, "pscale", [128, 4], F32)
                    Dq("pool", lambda e: e.dma_start(out=wpool[:], in_=d["w_pool"].ap()[l].rearrange("(kc p) n -> p kc n", p=128)), w=["wpool"])
                    Dq("pool", lambda e: e.dma_start(out=poolw[:], in_=d["pool_w"].ap()[l].rearrange("g c n -> c g n")), w=["poolw"])
                    Dq("sp", lambda e: e.dma_start(out=pscale[:], in_=d["pool_scale"].ap()[l]), w=["pscale"])
                    upad = [sb(es1, "upad%d" % i, [128, 16 + S], F32) for i in range(2)]
                    sa = sb(es1, "sa", [128, 16 + S], F32)
                    sbb = sb(es1, "sbb", [128, 16 + S], F32)
                    dbf = sb(es1, "dbf", [128, S], BF16)
                    t16 = sb(es1, "t16", [128, 16], F32)
                    for i in range(2):
                        C("pool", lambda e, i=i: e.memset(upad[i][:, 0:16], 0.0), w=[("upz", i)])
                    C("pool", lambda e: e.memset(sa[:, 0:16], 0.0), w=["saz"])
                    C("pool", lambda e: e.memset(sbb[:, 0:16], 0.0), w=["sbz"])
                    for g in range(4):
                        u = upad[g % 2]
                        for tc in range(4):
                            bk = 2 + (g * 4 + tc) % 2
                            C("pe", lambda e, g=g, tc=tc, bk=bk: [e.matmul(out=ps[:, bk, :], lhsT=wpool[:, kc, g * 128:(g + 1) * 128],
                                                                          rhs=xT[:, kc, tc * 512:(tc + 1) * 512], start=(kc == 0), stop=(kc == 7))
                                                                 for kc in range(8)][-1],
                              r=[("xT", t) for t in range(tc * 4, tc * 4 + 4)] + ["wpool"], w=[("ps", bk)])
                            C("act", lambda e, u=u, tc=tc, bk=bk: e.activation(out=u[:, 16 + tc * 512: 16 + (tc + 1) * 512], in_=ps[:, bk, :], func=AF.Copy),
                              r=[("ps", bk)], w=[("u", g % 2)])
                        w = 2 ** (g + 1)
                        src, bufs = u, [sa, sbb]
                        sh = 1
                        for st in range(g + 1):
                            dst = bufs[st % 2]
                            C("dve", lambda e, src=src, dst=dst, sh=sh: e.tensor_tensor(out=dst[:, 16:16 + S], in0=src[:, 16:16 + S],
                                                                                         in1=src[:, 16 - sh:16 - sh + S], op=ALU.add),
                              r=[("u", g % 2), ("upz", g % 2), "saz", "sbz", "sa", "sbb"], w=["sa" if st % 2 == 0 else "sbb"])
                            src = dst
                            sh *= 2
                        sfin = src
                        C("dve", lambda e, sfin=sfin, u=u, w=w: e.scalar_tensor_tensor(out=dbf[:], in0=sfin[:, 16:16 + S], scalar=1.0 / w,
                                                                                        in1=u[:, 16:16 + S], op0=ALU.mult, op1=ALU.subtract),
                          r=["sa", "sbb", ("u", g % 2)], w=["dbf"])
                        C("dve", lambda e, sfin=sfin, g=g: e.tensor_tensor(out=t16[:], in0=sfin[:, 16:32], in1=self.invcnt[:, g, :], op=ALU.mult),
                          r=["sa", "sbb", "k7"], w=["t16"])
                        C("dve", lambda e, u=u: e.tensor_tensor(out=dbf[:, 0:16], in0=t16[:], in1=u[:, 16:32], op=ALU.subtract),
                          r=["t16", ("u", g % 2), "dbf"], w=["dbf"])
                        for tc in range(4):
                            bk = 4 + tc % 2
                            C("pe", lambda e, g=g, tc=tc, bk=bk: e.matmul(out=ps[:, bk, :], lhsT=poolw[:, g, :], rhs=dbf[:, tc * 512:(tc + 1) * 512],
                                                                          start=True, stop=True),
                              r=["dbf", "poolw"], w=[("ps", bk)])
                            C("act", lambda e, g=g, tc=tc, bk=bk: e.activation(out=ypre[:, g, tc * 512:(tc + 1) * 512], in_=ps[:, bk, :], func=AF.Identity,
                                                                                scale=pscale[:, g:g + 1]),
                              r=[("ps", bk), "pscale"], w=[("ypre", g, tc)])
                    self.phase_end("A1")
                with ExitStack() as es2:
                    wqkv = [sb(es2, "wqkv%d" % i, [128, 8, 384], BF16) for i in range(2)]
                    vaug = [sb(es2, "vaug%d" % i, [128, 16, 132], BF16) for i in range(2)]
                    qT = [sb(es2, "qT%d" % i, [128, S], BF16) for i in range(2)]
                    kT = [[sb(es2, "kT%d_%d" % (i, m), [128, S], BF16) for m in range(2)] for i in range(2)]
                    PT = [sb(es2, "PT%d" % i, [128, 16, 512], BF16) for i in range(2)]
                    ost = sb(es2, "ost", [128, 16, 2, 132], F32)
                    rtmp = [sb(es2, "rtmp%d" % i, [128, 4, 4, 8], F32) for i in range(4)]
                    qkb = [sb(es2, "qkb%d" % i, [128, 256], BF16) for i in range(4)]
                    rl = sb(es2, "rl", [128, 16, 2, 1], F32)
                    r1n = sb(es2, "r1n", [128, 16, 1], F32)
                    t0 = sb(es2, "t0", [128, 8, 128], F32)
                    t1 = sb(es2, "t1", [128, 8, 128], F32)
                    ss = sb(es2, "ss", [128, 16], F32)
                    rs = sb(es2, "rs", [128, 16, 1], F32)
                    onb = sb(es2, "onb", [128, 16, 128], BF16)
                    for i in range(2):
                        C("pool", lambda e, i=i: e.memset(vaug[i][:, :, 128:132], 1.0), w=[("vone", i)])
                        C("pool", lambda e, i=i: e.memset(kT[i][0][64:128, :], 0.0), w=[("kz", i, 0)])
                        C("pool", lambda e, i=i: e.memset(kT[i][1][0:64, :], 0.0), w=[("kz", i, 1)])
                    for h in range(NH):
                        hs = h % 2
                        Dq("pool", lambda e, h=h, hs=hs: e.dma_start(out=wqkv[hs][:], in_=d["w_qkv"].ap()[l, h].rearrange("(kc p) n -> p kc n", p=128)),
                           w=[("wqkv", hs)])
                        for tt in range(16):
                            i2 = tt % 4
                            bk = i2
                            C("pe", lambda e, tt=tt, bk=bk, hs=hs: [e.matmul(out=ps[:, bk, 0:384], lhsT=xT[:, kc, tt * 128:(tt + 1) * 128],
                                                                            rhs=wqkv[hs][:, kc, :], start=(kc == 0), stop=(kc == 7)) for kc in range(8)][-1],
                              r=[("xT", tt), ("wqkv", hs)], w=[("ps", bk)])
                            qk4 = ps[:, bk, 0:256].rearrange("p (a d) -> p a d", a=4)
                            cb = cos[:, tt:tt + 1, :].to_broadcast([128, 4, 8])
                            sn = sin[:, tt:tt + 1, :].to_broadcast([128, 4, 8])
                            rt = rtmp[i2]
                            C("dve", lambda e, qk4=qk4, cb=cb, sn=sn, rt=rt: [
                                e.tensor_tensor(out=rt[:, 0], in0=qk4[:, :, 0:8], in1=cb, op=ALU.mult),
                                e.tensor_tensor(out=rt[:, 1], in0=qk4[:, :, 8:16], in1=sn, op=ALU.mult),
                                e.tensor_tensor(out=rt[:, 2], in0=qk4[:, :, 8:16], in1=cb, op=ALU.mult),
                                e.tensor_tensor(out=rt[:, 3], in0=qk4[:, :, 0:8], in1=sn, op=ALU.mult)][-1],
                              r=[("ps", bk), "cos", "sin"], w=[("rt", i2)])
                            C("act", lambda e, bk=bk, i2=i2: e.activation(out=qkb[i2][:], in_=ps[:, bk, 0:256], func=AF.Copy),
                              r=[("ps", bk)], w=[("qkb", i2)])
                            C("act", lambda e, bk=bk, tt=tt, hs=hs: e.activation(out=vaug[hs][:, tt, 0:128], in_=ps[:, bk, 256:384], func=AF.Copy),
                              r=[("ps", bk)], w=[("v", hs, tt)])
                            qb4 = qkb[i2][:].rearrange("p (a d) -> p a d", a=4)
                            C("dve", lambda e, qb4=qb4, rt=rt: [
                                e.tensor_tensor(out=qb4[:, :, 0:8], in0=rt[:, 0], in1=rt[:, 1], op=ALU.subtract),
                                e.tensor_tensor(out=qb4[:, :, 8:16], in0=rt[:, 2], in1=rt[:, 3], op=ALU.add)][-1],
                              r=[("rt", i2), ("qkb", i2)], w=[("qkb", i2)])
                            bk2 = 4 + i2
                            C("pe", lambda e, bk2=bk2, i2=i2: [e.transpose(out=psb[:, bk2, 0:128], in_=qkb[i2][:, 0:128], identity=self.identb[:]),
                                                               e.transpose(out=psb[:, bk2, 128:256], in_=qkb[i2][:, 128:256], identity=self.identb[:])][-1],
                              r=[("qkb", i2), "k0"], w=[("ps", bk2)])
                            C("dve", lambda e, bk2=bk2, tt=tt, hs=hs: [e.tensor_copy(out=qT[hs][:, tt * 128:(tt + 1) * 128], in_=psb[:, bk2, 0:128]),
                                                                       e.tensor_copy(out=kT[hs][0][0:64, tt * 128:(tt + 1) * 128], in_=psb[0:64, bk2, 128:256]),
                                                                       e.tensor_copy(out=kT[hs][1][64:128, tt * 128:(tt + 1) * 128], in_=psb[64:128, bk2, 128:256])][-1],
                              r=[("ps", bk2), ("kz", hs, 0), ("kz", hs, 1)], w=[("qk", hs, tt)])
                        if h == 0 and self.stop_after == "A2a":
                            self.phase_end("A2a")
                        chunks = [(m, qc) for qc in range(4) for m in range(2)]
                        sbank = [0]

                        def scores(m, qc, hs=hs):
                            nkb = 4 * qc + 4
                            for kb in range(nkb):
                                jp = kb - 4 * qc
                                q0 = (4 * qc + max(jp, 0)) * 128
                                n = (4 * qc + 4) * 128 - q0
                                bk = sbank[0] % 4
                                sbank[0] += 1
                                C("pe", lambda e, m=m, kb=kb, q0=q0, n=n, bk=bk: e.matmul(
                                    out=ps[:, bk, 0:n], lhsT=kT[hs][m][:, kb * 128:(kb + 1) * 128],
                                    rhs=qT[hs][:, q0:q0 + n], start=True, stop=True),
                                  r=[("qk", hs, t) for t in range(q0 // 128, 4 * qc + 4)] + [("qk", hs, kb)], w=[("ps", bk)])
                                C("act", lambda e, m=m, kb=kb, n=n, bk=bk: e.activation(out=PT[m][:, kb, 0:n], in_=ps[:, bk, 0:n], func=AF.Exp, scale=0.125),
                                  r=[("ps", bk)], w=[("PT", m, kb)])
                                if jp >= 0:
                                    C("pool", lambda e, m=m, kb=kb: e.tensor_tensor(out=PT[m][:, kb, 0:128], in0=PT[m][:, kb, 0:128],
                                                                                    in1=self.mask01[:], op=ALU.mult),
                                      r=[("PT", m, kb), "k2"], w=[("PT", m, kb)])

                        obank = [0]

                        def av(m, qc, hs=hs):
                            for j in range(4):
                                qb = 4 * qc + j
                                bk = 4 + obank[0] % 4
                                obank[0] += 1

                                def f(e, m=m, qc=qc, j=j, qb=qb, bk=bk):
                                    ins = None
                                    for kb in range(qb + 1):
                                        col = (j - max(kb - 4 * qc, 0)) * 128
                                        ins = e.matmul(out=ps[:, bk, 0:130], lhsT=PT[m][:, kb, col:col + 128], rhs=vaug[hs][:, kb, 0:130],
                                                       start=(kb == 0), stop=(kb == qb))
                                    return ins
                                C("pe", f, r=[("PT", m, kb) for kb in range(qb + 1)] + [("v", hs, kb) for kb in range(qb + 1)] + [("vone", hs)],
                                  w=[("ps", bk)])
                                C("act", lambda e, m=m, qb=qb, bk=bk: e.activation(out=ost[:, qb, m, 0:129], in_=ps[:, bk, 0:129], func=AF.Copy),
                                  r=[("ps", bk)], w=[("ost", qb, m)])

                        scores(*chunks[0])
                        for ci in range(len(chunks)):
                            if ci + 1 < len(chunks):
                                scores(*chunks[ci + 1])
                            av(*chunks[ci])
                        if h == 0 and self.stop_after == "A2b":
                            self.phase_end("A2b")
                        ostk = [("ost", qb, m) for qb in range(16) for m in range(2)]
                        C("dve", lambda e: e.reciprocal(out=rl[:], in_=ost[:, :, :, 128:129]), r=ostk, w=["rl"])
                        C("dve", lambda e: e.tensor_scalar(out=r1n[:], in0=rl[:, :, 1, :], scalar1=neglam[:, 0:1], scalar2=None, op0=ALU.mult),
                          r=["rl", "neglam"], w=["r1n"])
                        for hf in range(2):
                            qs = slice(hf * 8, hf * 8 + 8)
                            C("dve", lambda e, qs=qs: e.tensor_tensor(out=t0[:], in0=ost[:, qs, 0, 0:128], in1=rl[:, qs, 0, :].to_broadcast([128, 8, 128]), op=ALU.mult),
                              r=ostk + ["rl", "t0"], w=["t0"])
                            C("dve", lambda e, qs=qs: e.tensor_tensor(out=t1[:], in0=ost[:, qs, 1, 0:128], in1=r1n[:, qs, :].to_broadcast([128, 8, 128]), op=ALU.mult),
                              r=ostk + ["r1n", "t1"], w=["t1"])
                            C("dve", lambda e: e.tensor_tensor(out=t0[:], in0=t0[:], in1=t1[:], op=ALU.add), r=["t0", "t1"], w=["t0"])
                            C("dve", lambda e: e.tensor_tensor(out=t1[:], in0=t0[:], in1=t0[:], op=ALU.mult), r=["t0", "t1"], w=["t1"])
                            C("dve", lambda e, qs=qs: e.tensor_reduce(out=ss[:, qs], in_=t1[:], axis=AX.X, op=ALU.add), r=["t1"], w=["ss"])
                            C("act", lambda e, qs=qs: e.activation(out=rs[:, qs, 0], in_=ss[:, qs], func=AF.Sqrt, bias=self.epsc[:], scale=1.0 / 128.0),
                              r=["ss", "k8"], w=["rs"])
                            C("dve", lambda e, qs=qs: e.reciprocal(out=rs[:, qs, :], in_=rs[:, qs, :]), r=["rs"], w=["rs"])
                            C("dve", lambda e, qs=qs: e.tensor_tensor(out=t1[:], in0=t0[:], in1=rs[:, qs, :].to_broadcast([128, 8, 128]), op=ALU.mult),
                              r=["t0", "rs", "t1"], w=["t1"])
                            C("dve", lambda e, qs=qs: e.tensor_tensor(out=onb[:, qs, :], in0=t1[:], in1=sublnw[:].to_broadcast([128, 8, 128]), op=ALU.mult),
                              r=["t1", "sublnw", "onb"], w=["onb"])
                        if h == 0 and self.stop_after == "A2c":
                            self.phase_end("A2c")
                        for half in range(2):
                            bk = 2 + half
                            C("pe", lambda e, half=half, bk=bk: [e.transpose(out=psb[:, bk, c * 128:(c + 1) * 128], in_=onb[:, half * 8 + c, :],
                                                                             identity=self.identb[:]) for c in range(8)][-1],
                              r=["onb", "k0"], w=[("ps", bk)])
                            C("act", lambda e, half=half, bk=bk, h=h: e.activation(out=onT[:, h, half * 1024:(half + 1) * 1024], in_=psb[:, bk, :], func=AF.Copy),
                              r=[("ps", bk)], w=[("onT", h, half)])
                    self.phase_end("A2")
                merged = sb(es_o, "merged", [128, 8, S], BF16)
                with ExitStack() as es3:
                    wg = [sb(es3, "wg%d" % i, [128, 8, 2, 128], BF16) for i in range(2)]
                    wab = [sb(es3, "wab%d" % i, [128, 8, 128], BF16) for i in range(2)]
                    wpb = [sb(es3, "wpb%d" % i, [128, 4, 128], BF16) for i in range(2)]
                    sg = [[sb(es3, "sg%d_%d" % (i, k), [128, 512], F32) for k in range(2)] for i in range(2)]
                    mt = [[sb(es3, "mt%d_%d" % (i, k), [128, 512], F32) for k in range(2)] for i in range(2)]
                    wgv = d["w_gate"].ap()[l].rearrange("(kc p) (br jj n) -> p kc br jj n", p=128, br=2, jj=8)
                    wabv = d["w_ab"].ap()[l].rearrange("(kc p) (jj n) -> p kc jj n", p=128, jj=8)
                    wpbv = d["w_pb"].ap()[l].rearrange("(kc p) (jj n) -> p kc jj n", p=128, jj=8)
                    it = 0
                    for j in range(8):
                        js = j % 2
                        for br in range(2):
                            Dq("pool", lambda e, j=j, js=js, br=br: e.dma_start(out=wg[js][:, :, br, :], in_=wgv[:, :, br, j, :]), w=[("wg", js, br)])
                        Dq("pool", lambda e, j=j, js=js: e.dma_start(out=wab[js][:], in_=wabv[:, :, j, :]), w=[("wab", js)])
                        Dq("pool", lambda e, j=j, js=js: e.dma_start(out=wpb[js][:], in_=wpbv[:, :, j, :]), w=[("wpb", js)])
                        for tc in range(4):
                            s2 = it % 2
                            it += 1
                            b0 = s2 * 4
                            tsl = slice(tc * 512, (tc + 1) * 512)
                            C("pe", lambda e, js=js, tsl=tsl, b0=b0: [e.matmul(out=ps[:, b0, :], lhsT=wpb[js][:, g, :], rhs=ypre[:, g, tsl],
                                                                              start=(g == 0), stop=(g == 3)) for g in range(4)][-1],
                              r=[("wpb", js)], w=[("ps", b0)])
                            C("pe", lambda e, js=js, tsl=tsl, b0=b0: [e.matmul(out=ps[:, b0 + 1, :], lhsT=wab[js][:, c, :], rhs=onT[:, c, tsl],
                                                                              start=(c == 0), stop=(c == 7)) for c in range(8)][-1],
                              r=[("wab", js)], w=[("ps", b0 + 1)])
                            for br in range(2):
                                C("pe", lambda e, js=js, tsl=tsl, b0=b0, br=br: [e.matmul(out=ps[:, b0 + 2 + br, :], lhsT=wg[js][:, kc, br, :], rhs=xT[:, kc, tsl],
                                                                                         start=(kc == 0), stop=(kc == 7)) for kc in range(8)][-1],
                                  r=[("wg", js, br)], w=[("ps", b0 + 2 + br)])
                                C("act", lambda e, s2=s2, b0=b0, br=br: e.activation(out=sg[s2][br][:], in_=ps[:, b0 + 2 + br, :], func=AF.Sigmoid),
                                  r=[("ps", b0 + 2 + br)], w=[("sg", s2, br)])
                                C("dve", lambda e, s2=s2, b0=b0, br=br: e.tensor_tensor(out=mt[s2][br][:], in0=sg[s2][br][:], in1=ps[:, b0 + br, :], op=ALU.mult),
                                  r=[("sg", s2, br), ("ps", b0 + br)], w=[("mt", s2, br)])
                            C("pool", lambda e, s2=s2, j=j, tsl=tsl: e.tensor_tensor(out=merged[:, j, tsl], in0=mt[s2][0][:], in1=mt[s2][1][:], op=ALU.add),
                              r=[("mt", s2, 0), ("mt", s2, 1)], w=[("merged", j, tsl.start)])
                    self.phase_end("A3a")
                with ExitStack() as es4:
                    wout = sb(es4, "wout", [128, 8, D], BF16)
                    wr = sb(es4, "wr", [128, 8, E], F32)
                    brb = sb(es4, "brb", [128, E], F32)
                    gb = sb(es4, "g1b", [128, D], F32)
                    bb = sb(es4, "b1b", [128, D], F32)
                    Dq("pool", lambda e: e.dma_start(out=wout[:], in_=d["w_out"].ap()[l].rearrange("(kc p) n -> p kc n", p=128)), w=["wout"])
                    Dq("sp", lambda e: e.dma_start(out=wr[:], in_=d["w_router"].ap()[l].rearrange("(kc p) n -> p kc n", p=128)), w=["wr"])
                    Dq("sp", lambda e: e.dma_start(out=brb[:], in_=d["b_router"].ap()[l:l + 1].to_broadcast([128, E])), w=["brb"])
                    Dq("sp", lambda e: e.dma_start(out=gb[:], in_=d["ln1_g"].ap()[l:l + 1].to_broadcast([128, D])), w=["lnp"])
                    Dq("sp", lambda e: e.dma_start(out=bb[:], in_=d["ln1_b"].ap()[l:l + 1].to_broadcast([128, D])), w=["lnp"])
                    xin = [sb(es4, "xin%d" % i, [128, D], F32) for i in range(2)]
                    zt = [sb(es4, "zt%d" % i, [128, D], F32) for i in range(2)]
                    x1t = [sb(es4, "x1t%d" % i, [128, D], F32) for i in range(2)]
                    x1b = [sb(es4, "x1b%d" % i, [128, D], BF16) for i in range(2)]
                    x1T = [sb(es4, "x1T%d" % i, [128, 8, 128], F32) for i in range(2)]
                    lnt = [(sb(es4, "st%d" % i, [128, 2, 6], F32), sb(es4, "mv%d" % i, [128, 2], F32), sb(es4, "sd%d" % i, [128, 1], F32),
                            sb(es4, "rstd%d" % i, [128, 1], F32), sb(es4, "xn%d" % i, [128, D], F32)) for i in range(2)]
                    Lt = [sb(es4, "Lt%d" % i, [128, E], F32) for i in range(2)]
                    mx8 = [sb(es4, "mx8%d" % i, [128, 8], F32) for i in range(2)]
                    Mk = [sb(es4, "Mk%d" % i, [128, E], F32) for i in range(2)]
                    nv0 = [sb(es4, "nv0%d" % i, [128, 1], F32) for i in range(2)]
                    ek = [sb(es4, "ek%d" % i, [128, 4], F32) for i in range(2)]
                    gs = [sb(es4, "gs%d" % i, [128, 1], F32) for i in range(2)]
                    posc = [sb(es4, "posc%d" % i, [128, 1, E], F32) for i in range(2)]
                    oh = [sb(es4, "oh%d" % i, [128, 4, E], F32) for i in range(2)]
                    dstf = [sb(es4, "dstf%d" % i, [128, 4], F32) for i in range(2)]
                    for tt in range(16):
                        i2 = tt % 2
                        ti = b * 16 + tt
                        tg = "n%d_" % i2
                        rows = slice(tok0 + tt * 128, tok0 + (tt + 1) * 128)
                        Dq("sp", lambda e, i2=i2, rows=rows: e.dma_start(out=xin[i2][:], in_=xsrc.ap()[rows, :]), w=[("xin", i2)])
                        for n in range(2):
                            C("pe", lambda e, tt=tt, n=n, i2=i2: [e.matmul(out=ps[:, i2 * 2 + n, :], lhsT=merged[:, j, tt * 128:(tt + 1) * 128],
                                                                         rhs=wout[:, j, n * 512:(n + 1) * 512], start=(j == 0), stop=(j == 7)) for j in range(8)][-1],
                              r=["wout"], w=[("ps", i2 * 2 + n)])
                            C("dve", lambda e, n=n, i2=i2: e.scalar_tensor_tensor(out=zt[i2][:, n * 512:(n + 1) * 512], in0=xin[i2][:, n * 512:(n + 1) * 512], scalar=ALPHA,
                                                                                  in1=ps[:, i2 * 2 + n, :], op0=ALU.mult, op1=ALU.add),
                              r=[("xin", i2), ("ps", i2 * 2 + n)], w=[(tg + "z", n)])
                        self.layer_norm_tile(lnt[i2], zt[i2], gb, bb, x1t[i2][:], [(tg + "z", 0), (tg + "z", 1)], ("x1t", i2), tg)
                        Dq("sp", lambda e, i2=i2, rows=rows: e.dma_start(out=d["xres"].ap()[rows, :], in_=x1t[i2][:]), r=[("x1t", i2)], w=[("xres", ti)])
                        if self.debug:
                            Dq("sp", lambda e, i2=i2, rows=rows: e.dma_start(out=d["dbg_x1"].ap()[rows, :], in_=x1t[i2][:]), r=[("x1t", i2)], w=[("dbgx1", ti)])
                        C("act", lambda e, i2=i2: e.activation(out=x1b[i2][:], in_=x1t[i2][:], func=AF.Copy), r=[("x1t", i2)], w=[("x1b", i2)])
                        for hf in range(2):
                            bk = 4 + hf
                            C("pe", lambda e, i2=i2, hf=hf, bk=bk: [e.transpose(out=ps[:, bk, c * 128:(c + 1) * 128], in_=x1t[i2][:, (hf * 4 + c) * 128:(hf * 4 + c + 1) * 128],
                                                                               identity=self.identf[:]) for c in range(4)][-1],
                              r=[("x1t", i2), "k1"], w=[("ps", bk)])
                            C("act", lambda e, i2=i2, hf=hf, bk=bk: e.activation(out=x1T[i2][:, hf * 4:(hf + 1) * 4, :], in_=ps[:, bk, :].rearrange("p (c t) -> p c t", c=4),
                                                                                 func=AF.Copy),
                              r=[("ps", bk)], w=[("x1T", i2, hf)])
                        C("pe", lambda e, i2=i2: [e.matmul(out=ps[:, 6, 0:E], lhsT=x1T[i2][:, kc, :], rhs=wr[:, kc, :], start=(kc == 0), stop=(kc == 7)) for kc in range(8)][-1],
                          r=[("x1T", i2, 0), ("x1T", i2, 1), "wr"], w=[("ps", 6)])
                        C("dve", lambda e, i2=i2: e.tensor_tensor(out=Lt[i2][:], in0=ps[:, 6, 0:E], in1=brb[:], op=ALU.add), r=[("ps", 6), "brb"], w=[tg + "L"])
                        C("dve", lambda e, i2=i2: e.max(out=mx8[i2][:], in_=Lt[i2][:]), r=[tg + "L"], w=[tg + "mx"])
                        C("dve", lambda e, i2=i2: e.tensor_scalar(out=Mk[i2][:], in0=Lt[i2][:], scalar1=mx8[i2][:, 3:4], scalar2=None, op0=ALU.is_ge),
                          r=[tg + "L", tg + "mx"], w=[tg + "Mk"])
                        C("dve", lambda e, i2=i2: e.tensor_scalar(out=nv0[i2][:], in0=mx8[i2][:, 0:1], scalar1=-1.0, scalar2=None, op0=ALU.mult),
                          r=[tg + "mx"], w=[tg + "nv"])
                        C("act", lambda e, i2=i2: e.activation(out=ek[i2][:], in_=mx8[i2][:, 0:4], func=AF.Exp, bias=nv0[i2][:], scale=1.0, accum_out=gs[i2][:]),
                          r=[tg + "mx", tg + "nv"], w=[tg + "ek"])
                        C("dve", lambda e, i2=i2: e.reciprocal(out=gs[i2][:], in_=gs[i2][:]), r=[tg + "ek"], w=[tg + "gs"])
                        C("dve", lambda e, i2=i2, ti=ti: e.tensor_scalar(out=self.gate[:, ti, :], in0=ek[i2][:], scalar1=gs[i2][:], scalar2=None, op0=ALU.mult),
                          r=[tg + "ek", tg + "gs"], w=[("gate", ti)])
                        C("pe", lambda e, i2=i2: [e.matmul(out=ps[:, 7, 0:E], lhsT=self.ustrict[:], rhs=Mk[i2][:], start=True, stop=False),
                                                  e.matmul(out=ps[:, 7, 0:E], lhsT=self.ones[:], rhs=self.macc[:], start=False, stop=True)][-1],
                          r=[tg + "Mk", "macc", "k3", "k4"], w=[("ps", 7)])
                        C("dve", lambda e, i2=i2: e.scalar_tensor_tensor(out=posc[i2][:, 0, :], in0=ps[:, 7, 0:E], scalar=float(CAP - 1), in1=self.ec[:],
                                                                         op0=ALU.min, op1=ALU.add),
                          r=[("ps", 7), "k5"], w=[tg + "pc"])
                        C("dve", lambda e, i2=i2: e.tensor_tensor(out=self.macc[:], in0=self.macc[:], in1=Mk[i2][:], op=ALU.add), r=["macc", tg + "Mk"], w=["macc"])
                        C("dve", lambda e, i2=i2: [e.tensor_scalar(out=oh[i2][:, k, :], in0=Lt[i2][:], scalar1=mx8[i2][:, k:k + 1], scalar2=None, op0=ALU.is_equal)
                                                   for k in range(4)][-1],
                          r=[tg + "L", tg + "mx"], w=[tg + "oh"])
                        C("dve", lambda e, i2=i2: e.tensor_tensor(out=oh[i2][:], in0=oh[i2][:], in1=posc[i2][:].to_broadcast([128, 4, E]), op=ALU.mult),
                          r=[tg + "oh", tg + "pc"], w=[tg + "oh"])
                        C("dve", lambda e, i2=i2: e.tensor_reduce(out=dstf[i2][:], in_=oh[i2][:], axis=AX.X, op=ALU.add), r=[tg + "oh"], w=[tg + "df"])
                        C("dve", lambda e, i2=i2, ti=ti: e.tensor_copy(out=self.desti[:, ti, :], in_=dstf[i2][:]), r=[tg + "df"], w=[("desti", ti)])
                        for k in range(4):
                            Dq("pool", lambda e, i2=i2, ti=ti, k=k: e.indirect_dma_start(
                                out=d["xsbuf"].ap(), out_offset=bass.IndirectOffsetOnAxis(ap=self.desti[:, ti, k:k + 1], axis=0),
                                in_=x1b[i2][:], in_offset=None),
                               r=[("x1b", i2), ("desti", ti)], w=[("xsd", ti, k)])
                    self.phase_end("A3b")

    def moe(self, l):
        nc, d, C, Dq, sb = self.nc, self.d, self.C, self.Dq, self.sb
        ps, psb = self.ps, self.psb
        with ExitStack() as es:
            wgu = [sb(es, "wgu%d" % i, [128, 8, 2 * D], BF16) for i in range(2)]
            wdn = [sb(es, "wdn%d" % i, [128, 8, D], BF16) for i in range(2)]
            xs = sb(es, "xs", [128, NR, D], BF16)
            xsT = sb(es, "xsT", [128, 8, CAP], BF16)
            actT = sb(es, "actT", [128, 8, CAP], BF16)
            hg = [sb(es, "hg%d" % i, [128, 2, CAP // 2], F32) for i in range(2)]
            hu = [sb(es, "hu%d" % i, [128, 2, CAP // 2], F32) for i in range(2)]
            sgm = [sb(es, "sgm%d" % i, [128, 2, CAP // 2], F32) for i in range(2)]
            tt_ = [sb(es, "tt%d" % i, [128, 2, CAP // 2], F32) for i in range(2)]
            yst = [sb(es, "yst%d" % i, [128, D], F32) for i in range(2)]
            bdn = [sb(es, "bdn%d" % i, [128, D], F32) for i in range(2)]
            bgu = sb(es, "bgu", [128, E, 16], F32)
            Dq("sp", lambda e: e.dma_start(out=bgu[:], in_=d["b_gu"].ap()[l]), w=["bgu"])
            C("dve", lambda e: e.tensor_scalar(out=bgu[:, :, 8:16], in0=bgu[:, :, 8:16], scalar1=1.0, scalar2=None, op0=ALU.add), r=["bgu"], w=["bgu"])
            H = CAP // 2

            def load_w(e_):
                s = e_ % 2
                gv = d["w_gu"].ap()[l, e_].rearrange("(kc p) n -> p kc n", p=128)
                dv = d["w_down"].ap()[l, e_].rearrange("(kc p) n -> p kc n", p=128)
                for q in range(4):
                    Dq("pool", lambda e, s=s, q=q, gv=gv: e.dma_start(out=wgu[s][:, 2 * q:2 * q + 2, :], in_=gv[:, 2 * q:2 * q + 2, :]), w=[("wgu", s, q)])
                for q in range(2):
                    Dq("pool", lambda e, s=s, q=q, dv=dv: e.dma_start(out=wdn[s][:, 4 * q:4 * q + 4, :], in_=dv[:, 4 * q:4 * q + 4, :]), w=[("wdn", s, q)])
                Dq("sp", lambda e, s=s, e_=e_: e.dma_start(out=bdn[s][:], in_=d["b_down"].ap()[l, e_:e_ + 1].to_broadcast([128, D])), w=[("bdn", s)])

            xsd_keys = [("xsd", ti, k) for ti in range(NT) for k in range(4)]
            load_w(0)
            cnt = 0
            for e_ in range(E):
                s = e_ % 2
                if e_ + 1 < E:
                    load_w(e_ + 1)
                Dq("sp", lambda e, e_=e_: e.dma_start(out=xs[:], in_=d["xsbuf"].ap()[e_ * CAP:(e_ + 1) * CAP, :].rearrange("(r p) n -> p r n", p=128)),
                   w=["xs"])
                for r in range(NR):
                    bk = 6 + r % 2
                    C("pe", lambda e, r=r, bk=bk: [e.transpose(out=psb[:, bk, c * 128:(c + 1) * 128], in_=xs[:, r, c * 128:(c + 1) * 128], identity=self.identb[:])
                                                   for c in range(8)][-1],
                      r=["xs", "k0"], w=[("ps", bk)])
                    eng = "act" if r % 2 == 0 else "dve"
                    if eng == "act":
                        C("act", lambda e, r=r, bk=bk: e.activation(out=xsT[:, :, r * 128:(r + 1) * 128], in_=psb[:, bk, :].rearrange("p (c t) -> p c t", c=8), func=AF.Copy),
                          r=[("ps", bk)], w=[("xsT", r)])
                    else:
                        C("dve", lambda e, r=r, bk=bk: e.tensor_copy(out=xsT[:, :, r * 128:(r + 1) * 128], in_=psb[:, bk, :].rearrange("p (c t) -> p c t", c=8)),
                          r=[("ps", bk)], w=[("xsT", r)])
                xk = [("xsT", r) for r in range(NR)]
                for c in range(8):
                    i2 = cnt % 2
                    cnt += 1
                    hb = (c % 2) * 4
                    for gu in range(2):
                        col0 = gu * D + c * 128
                        for hf in range(2):
                            bk = hb + gu * 2 + hf
                            C("pe", lambda e, s=s, col0=col0, hf=hf, bk=bk: [e.matmul(out=ps[:, bk, 0:H], lhsT=wgu[s][:, kc, col0:col0 + 128],
                                                                                    rhs=xsT[:, kc, hf * H:(hf + 1) * H], start=(kc == 0), stop=(kc == 7)) for kc in range(8)][-1],
                              r=xk + [("wgu", s, q) for q in range(4)], w=[("ps", bk)])
                    C("dve", lambda e, i2=i2, e_=e_, c=c, hb=hb: e.tensor_scalar(out=hg[i2][:], in0=ps[:, hb:hb + 2, 0:H], scalar1=bgu[:, e_, c:c + 1], scalar2=SW_LIM,
                                                                         op0=ALU.add, op1=ALU.min),
                      r=[("ps", hb), ("ps", hb + 1), "bgu"], w=[("hg", i2)])
                    C("dve", lambda e, i2=i2, e_=e_, c=c, hb=hb: e.tensor_scalar(out=hu[i2][:], in0=ps[:, hb + 2:hb + 4, 0:H], scalar1=bgu[:, e_, 8 + c:9 + c], scalar2=SW_LIM + 1.0,
                                                                         op0=ALU.add, op1=ALU.min),
                      r=[("ps", hb + 2), ("ps", hb + 3), "bgu"], w=[("hu", i2)])
                    C("act", lambda e, i2=i2: e.activation(out=sgm[i2][:], in_=hg[i2][:], func=AF.Sigmoid, scale=SW_ALPHA), r=[("hg", i2)], w=[("sgm", i2)])
                    C("dve", lambda e, i2=i2: e.scalar_tensor_tensor(out=tt_[i2][:], in0=hu[i2][:], scalar=-SW_LIM + 1.0, in1=hg[i2][:], op0=ALU.max, op1=ALU.mult),
                      r=[("hu", i2), ("hg", i2)], w=[("tt", i2)])
                    C("pool", lambda e, i2=i2, c=c: e.tensor_tensor(out=actT[:, c, :].rearrange("p (a n) -> p a n", a=2), in0=tt_[i2][:], in1=sgm[i2][:], op=ALU.mult),
                      r=[("tt", i2), ("sgm", i2)], w=[("actT", c)])
                ak = [("actT", c) for c in range(8)]
                for r in range(NR):
                    ys = r % 2
                    for n in range(2):
                        bk = 4 + n
                        C("pe", lambda e, s=s, r=r, n=n, bk=bk: [e.matmul(out=ps[:, bk, :], lhsT=actT[:, c, r * 128:(r + 1) * 128], rhs=wdn[s][:, c, n * 512:(n + 1) * 512],
                                                                         start=(c == 0), stop=(c == 7)) for c in range(8)][-1],
                          r=ak + [("wdn", s, 0), ("wdn", s, 1)], w=[("ps", bk)])
                        C("dve", lambda e, s=s, ys=ys, n=n, bk=bk: e.tensor_tensor(out=yst[ys][:, n * 512:(n + 1) * 512], in0=ps[:, bk, :], in1=bdn[s][:, n * 512:(n + 1) * 512], op=ALU.add),
                          r=[("ps", bk), ("bdn", s)], w=[("yst", ys, n)])
                    Dq("sp", lambda e, ys=ys, e_=e_, r=r: e.dma_start(out=d["ybuf"].ap()[e_ * CAP + r * 128: e_ * CAP + (r + 1) * 128, :], in_=yst[ys][:]),
                       r=[("yst", ys, 0), ("yst", ys, 1)], w=[("ybuf", e_, r)])
            self.phase_end("moe")

    def combine(self, l, xdst):
        nc, d, C, Dq, sb = self.nc, self.d, self.C, self.Dq, self.sb
        with ExitStack() as es:
            gb = sb(es, "g2b", [128, D], F32)
            bb = sb(es, "b2b", [128, D], F32)
            Dq("sp", lambda e: e.dma_start(out=gb[:], in_=d["ln2_g"].ap()[l:l + 1].to_broadcast([128, D])), w=["lnp"])
            Dq("sp", lambda e: e.dma_start(out=bb[:], in_=d["ln2_b"].ap()[l:l + 1].to_broadcast([128, D])), w=["lnp"])
            yk = [[sb(es, "yk%d_%d" % (i, k), [128, D], F32) for k in range(4)] for i in range(2)]
            x1 = [sb(es, "cx1%d" % i, [128, D], F32) for i in range(2)]
            acc = [sb(es, "acc%d" % i, [128, D], F32) for i in range(2)]
            zt = [sb(es, "cz%d" % i, [128, D], F32) for i in range(2)]
            ot = [sb(es, "cot%d" % i, [128, D], F32) for i in range(2)]
            lnt = [(sb(es, "cst%d" % i, [128, 2, 6], F32), sb(es, "cmv%d" % i, [128, 2], F32), sb(es, "csd%d" % i, [128, 1], F32),
                    sb(es, "crstd%d" % i, [128, 1], F32), sb(es, "cxn%d" % i, [128, D], F32)) for i in range(2)]
            for ti in range(NT):
                i2 = ti % 2
                tg = "c%d_" % i2
                rows = slice(ti * 128, (ti + 1) * 128)
                for k in range(4):
                    gn = ti * 4 + k
                    Dq("pool", lambda e, i2=i2, ti=ti, k=k: e.indirect_dma_start(
                        out=yk[i2][k][:], out_offset=None, in_=d["ybuf"].ap(),
                        in_offset=bass.IndirectOffsetOnAxis(ap=self.desti[:, ti, k:k + 1], axis=0)),
                       r=[("gch", gn - 2)], w=[("yk", i2, k), ("gch", gn)])
                import os
                cut = os.environ.get("COMBINE_CUT", "full")
                if cut == "gather":
                    C("dve", lambda e, i2=i2: e.tensor_copy(out=ot[i2][:], in_=yk[i2][0][:]), r=[("yk", i2, 0)], w=[("cot", i2)])
                    for k in range(1, 4):
                        C("dve", lambda e, i2=i2, k=k: e.tensor_tensor(out=ot[i2][:], in0=ot[i2][:], in1=yk[i2][k][:], op=ALU.add), r=[("yk", i2, k), ("cot", i2)], w=[("cot", i2)])
                    Dq("sp", lambda e, i2=i2, rows=rows: e.dma_start(out=xdst.ap()[rows, :], in_=ot[i2][:]), r=[("cot", i2)], w=[("xdst", ti)])
                    continue
                Dq("sp", lambda e, i2=i2, rows=rows: e.dma_start(out=x1[i2][:], in_=d["xres"].ap()[rows, :]), w=[("cx1", i2)])
                C("dve", lambda e, i2=i2, ti=ti: e.tensor_scalar(out=acc[i2][:], in0=yk[i2][0][:], scalar1=self.gate[:, ti, 0:1], scalar2=None, op0=ALU.mult),
                  r=[("yk", i2, 0)], w=[("acc", i2)])
                for k in range(1, 4):
                    C("dve", lambda e, i2=i2, ti=ti, k=k: e.scalar_tensor_tensor(out=acc[i2][:], in0=yk[i2][k][:], scalar=self.gate[:, ti, k:k + 1], in1=acc[i2][:],
                                                                                 op0=ALU.mult, op1=ALU.add),
                      r=[("yk", i2, k), ("acc", i2)], w=[("acc", i2)])
                C("dve", lambda e, i2=i2: e.scalar_tensor_tensor(out=zt[i2][:], in0=x1[i2][:], scalar=ALPHA, in1=acc[i2][:], op0=ALU.mult, op1=ALU.add),
                  r=[("cx1", i2), ("acc", i2)], w=[tg + "zz"])
                if cut == "acc":
                    Dq("sp", lambda e, i2=i2, rows=rows: e.dma_start(out=xdst.ap()[rows, :], in_=zt[i2][:]), r=[tg + "zz"], w=[("xdst", ti)])
                    continue
                self.layer_norm_tile(lnt[i2], zt[i2], gb, bb, ot[i2][:], [tg + "zz"], ("cot", i2), tg)
                Dq("sp", lambda e, i2=i2, rows=rows: e.dma_start(out=xdst.ap()[rows, :], in_=ot[i2][:]), r=[("cot", i2)], w=[("xdst", ti)])
            self.phase_end("combine")


def _consts():
    c = {}
    c["c_identb"] = np.eye(128, dtype=np.float32).astype(ml_dtypes.bfloat16)
    c["c_identf"] = np.eye(128, dtype=np.float32)
    k = np.arange(128)[:, None]
    q = np.arange(128)[None, :]
    c["c_mask01"] = (q >= k).astype(np.float32).astype(ml_dtypes.bfloat16)
    c["c_ustrict"] = (k < q).astype(np.float32)
    c["c_ones"] = np.ones((128, 128), np.float32)
    c["c_ec"] = np.broadcast_to((np.arange(E, dtype=np.float32) * CAP)[None, :], (128, E)).copy()
    inv = (ROPE_THETA ** (-np.arange(0, 16, 2, dtype=np.float32) / np.float32(16))).astype(np.float32)
    c["c_invfreq"] = np.broadcast_to(inv[None, :], (128, 8)).copy()
    ic = np.zeros((4, 16), np.float32)
    for g, w in enumerate((2, 4, 8, 16)):
        ic[g] = 1.0 / np.minimum(np.arange(1, 17), w).astype(np.float32)
    c["c_invcnt"] = np.broadcast_to(ic[None], (128, 4, 16)).copy()
    return c


def _layout(inp):
    f = lambda a: np.ascontiguousarray(np.asarray(a))
    w_in = f(inp["w_in"])
    sh = {}
    sh["w_pool"] = f(w_in[:, :, 0:512])
    q = w_in[:, :, 512:1536].reshape(L, D, NH, 128)
    k = w_in[:, :, 1536:2560].reshape(L, D, NH, 128)
    v = w_in[:, :, 2560:3584].reshape(L, D, NH, 128)
    sh["w_qkv"] = f(np.concatenate([q, k, v], axis=-1).transpose(0, 2, 1, 3))
    sh["w_gate"] = f(w_in[:, :, 3584:5632])
    sh["pool_w"] = f(inp["pool_w"])
    sh["pool_scale"] = f(np.asarray(inp["pool_scale"]).reshape(L, 4, 128).transpose(0, 2, 1))
    sh["w_pb"] = f(inp["w_pool_branch"])
    sh["w_ab"] = f(inp["w_attn_branch"])
    sh["lam4"] = f(np.stack([np.asarray(inp["lambda_q1"]), np.asarray(inp["lambda_k1"]),
                             np.asarray(inp["lambda_q2"]), np.asarray(inp["lambda_k2"])], axis=1))
    sh["subln_w"] = f(inp["subln_w"])
    sh["w_out"] = f(inp["w_out"])
    for n in ("ln1_g", "ln1_b", "w_router", "b_router", "w_gu", "w_down", "b_down", "ln2_g", "ln2_b"):
        sh[n] = f(inp[n])
    sh["b_gu"] = f(np.asarray(inp["b_gu"]).reshape(L, E, 16, 128).transpose(0, 3, 1, 2))
    sh.update(_consts())
    return sh


_NC_CACHE = {}


def kernel(**inputs):
    shared = _layout(inputs)
    x = np.asarray(inputs["x"], dtype=np.float32).reshape(NCORES, T, D)
    pos = np.asarray(inputs["positions"]).astype(np.int32).reshape(NCORES, NSEQ, 16, 128).transpose(0, 1, 3, 2)
    if "nc" not in _NC_CACHE:
        _NC_CACHE["nc"] = Builder().build()
    nc = _NC_CACHE["nc"]
    in_maps = []
    for c in range(NCORES):
        m = dict(shared)
        m["x"] = np.ascontiguousarray(x[c])
        m["pos"] = np.ascontiguousarray(pos[c])
        in_maps.append(m)
    res = run_bass_kernel_spmd(nc, in_maps, core_ids=list(range(NCORES)))
    out = np.stack([np.asarray(r["y"]) for r in res.results], axis=0)
    return out.reshape(16, S, D).astype(np.float32)
```

```python
import math
from contextlib import ExitStack

import numpy as np
import ml_dtypes

import concourse.bass as bass
import concourse.mybir as mybir
from concourse.bass_utils import run_bass_kernel_spmd

F32 = mybir.dt.float32
BF16 = mybir.dt.bfloat16
I32 = mybir.dt.int32
AF = mybir.ActivationFunctionType
ALU = mybir.AluOpType
AX = mybir.AxisListType

NCORES = 8
D = 1024
S = 2048
NSEQ = 2
T = NSEQ * S
NT = T // 128
L = 2
NH = 8
E = 32
TOPK = 4
CAP = 768
NR = CAP // 128
LN_EPS = 1e-5
ALPHA = (2 * L) ** 0.25
ROPE_THETA = 500000.0
SW_ALPHA = 1.702
SW_LIM = 7.0
SAME_ENG_SYNC = True


class _Op:
    __slots__ = ("eng", "kind", "fn", "reads", "writes", "deps", "need", "sem", "val", "prev")

    def __init__(self, eng, kind, fn, reads, writes):
        self.eng, self.kind, self.fn = eng, kind, fn
        self.reads, self.writes = tuple(reads), tuple(writes)
        self.deps, self.need, self.sem, self.val, self.prev = (), False, None, 0, 0


class Prog:
    ENGS = ("pe", "act", "dve", "pool", "sp")

    def __init__(self, nc, es):
        self.nc = nc
        self.sems = []
        self.csem = {}
        for e in self.ENGS:
            self.csem[e] = len(self.sems)
            self.sems.append(es.enter_context(nc.semaphore("c_" + e)))
        self.ccnt = {e: 0 for e in self.ENGS}
        self.lanes = {}
        for q, n in (("sp", 14), ("pool", 14), ("act", 2)):
            self.lanes[q] = []
            for i in range(n):
                self.lanes[q].append(len(self.sems))
                self.sems.append(es.enter_context(nc.semaphore("l_%s%d" % (q, i))))
        self.lcnt = {}
        for q in self.lanes:
            for s in self.lanes[q]:
                self.lcnt[s] = 0
        self.lnext = {q: 0 for q in self.lanes}
        self.waited = {e: {} for e in self.ENGS}
        self.ops = []

    def op(self, eng, kind, fn, reads=(), writes=()):
        self.ops.append(_Op(eng, kind, fn, reads, writes))

    def flush(self):
        ops, self.ops = self.ops, []
        if not ops:
            return
        last_w, readers = {}, {}
        for i, op in enumerate(ops):
            deps = set()
            for k in op.reads:
                if k in last_w:
                    deps.add(last_w[k])
                if isinstance(k, tuple) and k[0] == "ps":
                    for r in readers.get(k, ()):
                        if ops[r].eng != op.eng:
                            deps.add(r)
            for k in op.writes:
                if k in last_w:
                    deps.add(last_w[k])
                for r in readers.get(k, ()):
                    deps.add(r)
            deps.discard(i)
            fd = []
            for d in deps:
                p = ops[d]
                if p.kind == "c" and op.kind == "c" and p.eng == op.eng:
                    if op.eng == "pe" or not SAME_ENG_SYNC:
                        continue
                fd.append(d)
            op.deps = fd
            for d in fd:
                ops[d].need = True
            for k in op.reads:
                readers.setdefault(k, []).append(i)
            for k in op.writes:
                last_w[k] = i
                readers[k] = []
        lastc = {}
        for i, op in enumerate(ops):
            if op.kind == "c" and op.fn is not None:
                lastc[op.eng] = i
        for i in lastc.values():
            ops[i].need = True
        for op in ops:
            if op.kind == "c":
                if op.need:
                    self.ccnt[op.eng] += 1
                    op.sem, op.val = self.csem[op.eng], self.ccnt[op.eng]
            else:
                q = self.lanes[op.eng]
                s = q[self.lnext[op.eng] % len(q)]
                self.lnext[op.eng] += 1
                op.prev = self.lcnt[s]
                self.lcnt[s] += 16
                op.sem, op.val = s, self.lcnt[s]
        final = {self.csem[e]: self.ccnt[e] for e in self.ENGS}
        final.update(self.lcnt)
        with self.nc.Block() as blk:
            decos = (("pe", blk.tensor), ("act", blk.scalar), ("dve", blk.vector),
                     ("pool", blk.gpsimd), ("sp", blk.sync))
            for ename, deco in decos:
                def body(eng, ename=ename):
                    self._emit(ename, eng, ops, final)
                deco(body)

    def _wait(self, ename, eng, s, v):
        if v <= 0:
            return
        w = self.waited[ename]
        if w.get(s, 0) >= v:
            return
        eng.wait_ge(self.sems[s], v)
        w[s] = v

    def _emit(self, ename, eng, ops, final):
        for op in ops:
            if op.eng != ename:
                continue
            need = {}
            for d in op.deps:
                p = ops[d]
                if need.get(p.sem, 0) < p.val:
                    need[p.sem] = p.val
            if op.kind == "d" and op.prev > 0:
                if need.get(op.sem, 0) < op.prev:
                    need[op.sem] = op.prev
            for s, v in need.items():
                self._wait(ename, eng, s, v)
            if op.fn is None:
                continue
            ins = op.fn(eng)
            if op.kind == "d":
                ins.then_inc(self.sems[op.sem], 16)
            elif op.need:
                ins.then_inc(self.sems[op.sem], 1)
        for s, v in final.items():
            self._wait(ename, eng, s, v)


class StopBuild(Exception):
    pass


class Builder:
    def __init__(self, n_layers=L, debug=False, stop_after=None):
        self.stop_after = stop_after
        self.n_layers = n_layers
        self.debug = debug
        self.nc = bass.Bass("TRN2", target_bir_lowering=False)
        self.es = ExitStack()

    def phase_end(self, name):
        if self.debug:
            print("phase", name, "sbuf cur", self._cur, "peak", self._peak)
        self._peak = self._cur
        self.P.flush()
        if self.stop_after == name:
            raise StopBuild()

    def C(self, eng, fn, r=(), w=()):
        self.P.op(eng, "c", fn, r, w)

    def Dq(self, q, fn, r=(), w=()):
        self.P.op(q, "d", fn, r, w)

    def sb(self, es, name, shape, dt):
        self._uid = getattr(self, "_uid", 0) + 1
        nb = int(np.prod(shape[1:])) * (4 if dt in (F32, I32) else 2)
        nb = (nb + 31) // 32 * 32
        self._cur = getattr(self, "_cur", 0) + nb
        self._peak = max(getattr(self, "_peak", 0), self._cur)
        t = es.enter_context(self.nc.sbuf_tensor("%s_u%d" % (name, self._uid), list(shape), dt))

        def _rel(nb=nb):
            self._cur -= nb
        es.callback(_rel)
        return t

    def din(self, name, shape, dt):
        return self.nc.dram_tensor(name, list(shape), dt, kind="ExternalInput")

    def build(self):
        nc = self.nc
        es = self.es
        nl = self.n_layers
        d = {}
        d["x"] = self.din("x", [T, D], F32)
        d["pos"] = self.din("pos", [NSEQ, 128, 16], I32)
        d["w_pool"] = self.din("w_pool", [L, D, 512], F32)
        d["w_qkv"] = self.din("w_qkv", [L, NH, D, 384], F32)
        d["w_gate"] = self.din("w_gate", [L, D, 2048], F32)
        d["pool_w"] = self.din("pool_w", [L, 4, 128, 128], F32)
        d["pool_scale"] = self.din("pool_scale", [L, 128, 4], F32)
        d["w_pb"] = self.din("w_pb", [L, 512, D], F32)
        d["w_ab"] = self.din("w_ab", [L, D, D], F32)
        d["lam4"] = self.din("lam4", [L, 4, 64], F32)
        d["subln_w"] = self.din("subln_w", [L, 128], F32)
        d["w_out"] = self.din("w_out", [L, D, D], F32)
        d["ln1_g"] = self.din("ln1_g", [L, D], F32)
        d["ln1_b"] = self.din("ln1_b", [L, D], F32)
        d["w_router"] = self.din("w_router", [L, D, E], F32)
        d["b_router"] = self.din("b_router", [L, E], F32)
        d["w_gu"] = self.din("w_gu", [L, E, D, 2 * D], F32)
        d["b_gu"] = self.din("b_gu", [L, 128, E, 16], F32)
        d["w_down"] = self.din("w_down", [L, E, D, D], F32)
        d["b_down"] = self.din("b_down", [L, E, D], F32)
        d["ln2_g"] = self.din("ln2_g", [L, D], F32)
        d["ln2_b"] = self.din("ln2_b", [L, D], F32)
        d["c_identb"] = self.din("c_identb", [128, 128], BF16)
        d["c_identf"] = self.din("c_identf", [128, 128], F32)
        d["c_mask01"] = self.din("c_mask01", [128, 128], BF16)
        d["c_ustrict"] = self.din("c_ustrict", [128, 128], F32)
        d["c_ones"] = self.din("c_ones", [128, 128], F32)
        d["c_ec"] = self.din("c_ec", [128, E], F32)
        d["c_invfreq"] = self.din("c_invfreq", [128, 8], F32)
        d["c_invcnt"] = self.din("c_invcnt", [128, 4, 16], F32)
        d["y"] = nc.dram_tensor("y", [T, D], F32, kind="ExternalOutput")
        d["xres"] = nc.dram_tensor("xres", [T, D], F32, kind="Internal")
        d["xcur"] = nc.dram_tensor("xcur", [T, D], F32, kind="Internal")
        d["xsbuf"] = nc.dram_tensor("xsbuf", [E * CAP, D], BF16, kind="Internal")
        d["ybuf"] = nc.dram_tensor("ybuf", [E * CAP, D], F32, kind="Internal")
        if self.debug:
            d["dbg_x1"] = nc.dram_tensor("dbg_x1", [T, D], F32, kind="ExternalOutput")
        self.d = d

        self.P = Prog(nc, es)
        self.ps = es.enter_context(nc.psum_tensor("ps", [128, 8, 512], F32))
        self.psb = self.ps.bitcast(BF16)
        sb = self.sb
        self.identb = sb(es, "identb", [128, 128], BF16)
        self.identf = sb(es, "identf", [128, 128], F32)
        self.mask01 = sb(es, "mask01", [128, 128], BF16)
        self.ustrict = sb(es, "ustrict", [128, 128], F32)
        self.ones = sb(es, "ones", [128, 128], F32)
        self.ec = sb(es, "ec", [128, E], F32)
        self.invfreq = sb(es, "invfreq", [128, 1, 8], F32)
        self.invcnt = sb(es, "invcnt", [128, 4, 16], F32)
        self.gate = sb(es, "gate", [128, NT, TOPK], F32)
        self.desti = sb(es, "desti", [128, NT, TOPK], I32)
        self.macc = sb(es, "macc", [128, E], F32)
        self.epsc = sb(es, "epsc", [128, 1], F32)

        def ld(dst, src, key):
            self.Dq("sp", lambda e: e.dma_start(out=dst, in_=src), w=[key])
        ld(self.identb[:], d["c_identb"].ap(), "k0")
        ld(self.identf[:], d["c_identf"].ap(), "k1")
        ld(self.mask01[:], d["c_mask01"].ap(), "k2")
        ld(self.ustrict[:], d["c_ustrict"].ap(), "k3")
        ld(self.ones[:], d["c_ones"].ap(), "k4")
        ld(self.ec[:], d["c_ec"].ap(), "k5")
        ld(self.invfreq[:, 0, :], d["c_invfreq"].ap(), "k6")
        ld(self.invcnt[:], d["c_invcnt"].ap(), "k7")
        self.C("dve", lambda e: e.memset(self.epsc[:], LN_EPS), w=["k8"])
        with ExitStack() as es2:
            z = self.sb(es2, "zrows", [128, 6, D], BF16)
            self.C("dve", lambda e: e.memset(z[:], 0.0), w=["z"])
            xv = d["xsbuf"].ap().rearrange("(n r p) d -> n p r d", p=128, r=6)
            for n in range(E * CAP // (128 * 6)):
                self.Dq("sp", lambda e, n=n: e.dma_start(out=xv[n], in_=z[:]), r=["z"], w=[("xs0", n)])
            self.phase_end("init")

        try:
            for l in range(nl):
                xsrc = d["x"] if l == 0 else d["xcur"]
                xdst = d["y"] if l == nl - 1 else d["xcur"]
                self.C("dve", lambda e: e.memset(self.macc[:], 0.0), w=["macc"])
                for b in range(NSEQ):
                    self.mixer(l, b, xsrc)
                self.moe(l)
                self.combine(l, xdst)
        except StopBuild:
            return nc
        self.es.close()
        return nc

    def layer_norm_tile(self, es_tiles, z, gb, bb, out, zkeys, outkey, tag):
        st, mv, sd, rstd, xn = es_tiles
        C = self.C
        C("dve", lambda e: [e.bn_stats(out=st[:, 0, :], in_=z[:, 0:512]),
                             e.bn_stats(out=st[:, 1, :], in_=z[:, 512:1024])][-1],
          r=list(zkeys), w=[tag + "st"])
        C("dve", lambda e: e.bn_aggr(out=mv[:], in_=st[:].rearrange("p a b -> p (a b)")), r=[tag + "st"], w=[tag + "mv"])
        C("act", lambda e: e.activation(out=sd[:], in_=mv[:, 1:2], func=AF.Sqrt, bias=self.epsc[:], scale=1.0),
          r=[tag + "mv"], w=[tag + "sd"])
        C("dve", lambda e: e.reciprocal(out=rstd[:], in_=sd[:]), r=[tag + "sd"], w=[tag + "rs"])
        C("dve", lambda e: e.tensor_scalar(out=xn[:], in0=z[:], scalar1=mv[:, 0:1], scalar2=rstd[:],
                                           op0=ALU.subtract, op1=ALU.mult), r=list(zkeys) + [tag + "mv", tag + "rs"], w=[tag + "xn"])
        C("pool", lambda e: e.tensor_tensor(out=xn[:], in0=xn[:], in1=gb[:], op=ALU.mult), r=[tag + "xn", "lnp"], w=[tag + "xn"])
        C("pool", lambda e: e.tensor_tensor(out=out, in0=xn[:], in1=bb[:], op=ALU.add), r=[tag + "xn", "lnp"], w=[outkey])

    def mixer(self, l, b, xsrc):
        nc, d, C, Dq, sb = self.nc, self.d, self.C, self.Dq, self.sb
        ps, psb = self.ps, self.psb
        tok0 = b * S
        lam_init = 0.8 - 0.6 * math.exp(-0.3 * l)
        if True:
            with ExitStack() as es_o:
                xT = sb(es_o, "xT", [128, 8, S], BF16)
                ypre = sb(es_o, "ypre", [128, 4, S], BF16)
                onT = sb(es_o, "onT", [128, 8, S], BF16)
                cos = sb(es_o, "cos", [128, 16, 8], F32)
                sin = sb(es_o, "sin", [128, 16, 8], F32)
                neglam = sb(es_o, "neglam", [128, 1], F32)
                sublnw = sb(es_o, "sublnw", [128, 1, 128], F32)
                with ExitStack() as es1:
                    xst = [sb(es1, "xst%d" % i, [128, D], F32) for i in range(2)]
                    xb = [sb(es1, "xb%d" % i, [128, D], BF16) for i in range(2)]
                    for tt in range(16):
                        i2 = tt % 2
                        Dq("sp", lambda e, tt=tt, i2=i2: e.dma_start(out=xst[i2][:], in_=xsrc.ap()[tok0 + tt * 128: tok0 + (tt + 1) * 128, :]),
                           w=[("xst", i2)])
                        C("act", lambda e, i2=i2: e.activation(out=xb[i2][:], in_=xst[i2][:], func=AF.Copy),
                          r=[("xst", i2)], w=[("xb", i2)])
                        C("pe", lambda e, i2=i2: [e.transpose(out=psb[:, i2, c * 128:(c + 1) * 128], in_=xb[i2][:, c * 128:(c + 1) * 128],
                                                              identity=self.identb[:]) for c in range(8)][-1],
                          r=[("xb", i2), "k0"], w=[("ps", i2)])
                        C("dve", lambda e, tt=tt, i2=i2: e.tensor_copy(out=xT[:, :, tt * 128:(tt + 1) * 128],
                                                                     in_=psb[:, i2, :].rearrange("p (c t) -> p c t", c=8)),
                          r=[("ps", i2)], w=[("xT", tt)])
                    posi = sb(es1, "posi", [128, 16], I32)
                    posf = sb(es1, "posf", [128, 16, 1], F32)
                    ang = sb(es1, "ang", [128, 16, 8], F32)
                    kf = sb(es1, "kf", [128, 16, 8], F32)
                    ki = sb(es1, "ki", [128, 16, 8], I32)
                    r2 = sb(es1, "r2", [128, 16, 8], F32)
                    yy = sb(es1, "yy", [128, 16, 8], F32)
                    mm = sb(es1, "mm", [128, 16, 8], F32)
                    Dq("sp", lambda e: e.dma_start(out=posi[:], in_=d["pos"].ap()[b]), w=["posi"])
                    C("dve", lambda e: e.tensor_copy(out=posf[:, :, 0], in_=posi[:]), r=["posi"], w=["posf"])
                    C("dve", lambda e: e.tensor_tensor(out=ang[:], in0=posf[:].to_broadcast([128, 16, 8]),
                                                       in1=self.invfreq[:].to_broadcast([128, 16, 8]), op=ALU.mult),
                      r=["posf", "k6"], w=["ang"])
                    TWO_PI = 2.0 * math.pi
                    C1 = 6.28125
                    C2 = TWO_PI - C1
                    C("dve", lambda e: e.tensor_scalar(out=kf[:], in0=ang[:], scalar1=1.0 / TWO_PI, scalar2=None, op0=ALU.mult),
                      r=["ang"], w=["kf"])
                    C("dve", lambda e: e.tensor_copy(out=ki[:], in_=kf[:]), r=["kf"], w=["ki"])
                    C("dve", lambda e: e.tensor_copy(out=kf[:], in_=ki[:]), r=["ki"], w=["kf"])
                    C("dve", lambda e: e.scalar_tensor_tensor(out=r2[:], in0=kf[:], scalar=-C1, in1=ang[:], op0=ALU.mult, op1=ALU.add),
                      r=["kf", "ang"], w=["r2"])
                    C("dve", lambda e: e.scalar_tensor_tensor(out=r2[:], in0=kf[:], scalar=-C2, in1=r2[:], op0=ALU.mult, op1=ALU.add),
                      r=["kf", "r2"], w=["r2"])
                    for shift, dst, nm in ((0.0, sin, "sin"), (math.pi / 2, cos, "cos")):
                        C("dve", lambda e, shift=shift: e.tensor_scalar(out=yy[:], in0=r2[:], scalar1=shift, scalar2=None, op0=ALU.add),
                          r=["r2"], w=["yy"])
                        C("dve", lambda e: e.tensor_scalar(out=mm[:], in0=yy[:], scalar1=math.pi, scalar2=None, op0=ALU.is_gt),
                          r=["yy"], w=["mm"])
                        C("dve", lambda e: e.scalar_tensor_tensor(out=yy[:], in0=mm[:], scalar=-TWO_PI, in1=yy[:], op0=ALU.mult, op1=ALU.add),
                          r=["mm", "yy"], w=["yy"])
                        C("dve", lambda e: e.tensor_scalar(out=mm[:], in0=yy[:], scalar1=-math.pi, scalar2=None, op0=ALU.is_lt),
                          r=["yy"], w=["mm"])
                        C("dve", lambda e: e.scalar_tensor_tensor(out=yy[:], in0=mm[:], scalar=TWO_PI, in1=yy[:], op0=ALU.mult, op1=ALU.add),
                          r=["mm", "yy"], w=["yy"])
                        C("dve", lambda e: e.tensor_scalar(out=yy[:], in0=yy[:], scalar1=math.pi, scalar2=-math.pi, op0=ALU.min, op1=ALU.max),
                          r=["yy"], w=["yy"])
                        C("act", lambda e, dst=dst: e.activation(out=dst[:], in_=yy[:], func=AF.Sin), r=["yy"], w=[nm])
                    l4 = sb(es1, "l4", [128, 4, 64], F32)
                    lp = sb(es1, "lp", [128, 2, 64], F32)
                    lsum = sb(es1, "lsum", [128, 2], F32)
                    lex = sb(es1, "lex", [128, 2], F32)
                    Dq("sp", lambda e: e.dma_start(out=l4[:], in_=d["lam4"].ap()[l:l + 1].to_broadcast([128, 4, 64])), w=["l4"])
                    C("dve", lambda e: e.tensor_tensor(out=lp[:], in0=l4[:, 0:4:2, :], in1=l4[:, 1:4:2, :], op=ALU.mult), r=["l4"], w=["lp"])
                    C("dve", lambda e: e.tensor_reduce(out=lsum[:], in_=lp[:], axis=AX.X, op=ALU.add), r=["lp"], w=["lsum"])
                    C("act", lambda e: e.activation(out=lex[:], in_=lsum[:], func=AF.Exp), r=["lsum"], w=["lex"])
                    C("dve", lambda e: e.tensor_tensor(out=neglam[:], in0=lex[:, 1:2], in1=lex[:, 0:1], op=ALU.subtract), r=["lex"], w=["neglam"])
                    C("dve", lambda e: e.tensor_scalar(out=neglam[:], in0=neglam[:], scalar1=-lam_init, scalar2=None, op0=ALU.add),
                      r=["neglam"], w=["neglam"])
                    Dq("sp", lambda e: e.dma_start(out=sublnw[:, 0, :], in_=d["subln_w"].ap()[l:l + 1].to_broadcast([128, 128])), w=["sublnw"])
                    C("act", lambda e: e.activation(out=sublnw[:], in_=sublnw[:], func=AF.Copy, scale=(1.0 - lam_init)), r=["sublnw"], w=["sublnw"])
                    wpool = sb(es1, "wpool", [128, 8, 512], BF16)
                    poolw = sb(es1, "poolw", [128, 4, 128], BF16)
                    pscale = sb(es1, "pscale", [128, 4], F32)
                    Dq("pool", lambda e: e.dma_start(out=wpool[:], in_=d["w_pool"].ap()[l].rearrange("(kc p) n -> p kc n", p=128)), w=["wpool"])
                    Dq("pool", lambda e: e.dma_start(out=poolw[:], in_=d["pool_w"].ap()[l].rearrange("g c n -> c g n")), w=["poolw"])
                    Dq("sp", lambda e: e.dma_start(out=pscale[:], in_=d["pool_scale"].ap()[l]), w=["pscale"])
                    upad = [sb(es1, "upad%d" % i, [128, 16 + S], F32) for i in range(2)]
                    sa = sb(es1, "sa", [128, 16 + S], F32)
                    sbb = sb(es1, "sbb", [128, 16 + S], F32)
                    dbf = sb(es1, "dbf", [128, S], BF16)
                    t16 = sb(es1, "t16", [128, 16], F32)
                    for i in range(2):
                        C("pool", lambda e, i=i: e.memset(upad[i][:, 0:16], 0.0), w=[("upz", i)])
                    C("pool", lambda e: e.memset(sa[:, 0:16], 0.0), w=["saz"])
                    C("pool", lambda e: e.memset(sbb[:, 0:16], 0.0), w=["sbz"])
                    for g in range(4):
                        u = upad[g % 2]
                        for tc in range(4):
                            bk = 2 + (g * 4 + tc) % 2
                            C("pe", lambda e, g=g, tc=tc, bk=bk: [e.matmul(out=ps[:, bk, :], lhsT=wpool[:, kc, g * 128:(g + 1) * 128],
                                                                          rhs=xT[:, kc, tc * 512:(tc + 1) * 512], start=(kc == 0), stop=(kc == 7))
                                                                 for kc in range(8)][-1],
                              r=[("xT", t) for t in range(tc * 4, tc * 4 + 4)] + ["wpool"], w=[("ps", bk)])
                            C("act", lambda e, u=u, tc=tc, bk=bk: e.activation(out=u[:, 16 + tc * 512: 16 + (tc + 1) * 512], in_=ps[:, bk, :], func=AF.Copy),
                              r=[("ps", bk)], w=[("u", g % 2)])
                        w = 2 ** (g + 1)
                        src, bufs = u, [sa, sbb]
                        sh = 1
                        for st in range(g + 1):
                            dst = bufs[st % 2]
                            C("dve", lambda e, src=src, dst=dst, sh=sh: e.tensor_tensor(out=dst[:, 16:16 + S], in0=src[:, 16:16 + S],
                                                                                         in1=src[:, 16 - sh:16 - sh + S], op=ALU.add),
                              r=[("u", g % 2), ("upz", g % 2), "saz", "sbz", "sa", "sbb"], w=["sa" if st % 2 == 0 else "sbb"])
                            src = dst
                            sh *= 2
                        sfin = src
                        C("dve", lambda e, sfin=sfin, u=u, w=w: e.scalar_tensor_tensor(out=dbf[:], in0=sfin[:, 16:16 + S], scalar=1.0 / w,
                                                                                        in1=u[:, 16:16 + S], op0=ALU.mult, op1=ALU.subtract),
                          r=["sa", "sbb", ("u", g % 2)], w=["dbf"])
                        C("dve", lambda e, sfin=sfin, g=g: e.tensor_tensor(out=t16[:], in0=sfin[:, 16:32], in1=self.invcnt[:, g, :], op=ALU.mult),
                          r=["sa", "sbb", "k7"], w=["t16"])
                        C("dve", lambda e, u=u: e.tensor_tensor(out=dbf[:, 0:16], in0=t16[:], in1=u[:, 16:32], op=ALU.subtract),
                          r=["t16", ("u", g % 2), "dbf"], w=["dbf"])
                        for tc in range(4):
                            bk = 4 + tc % 2
                            C("pe", lambda e, g=g, tc=tc, bk=bk: e.matmul(out=ps[:, bk, :], lhsT=poolw[:, g, :], rhs=dbf[:, tc * 512:(tc + 1) * 512],
                                                                          start=True, stop=True),
                              r=["dbf", "poolw"], w=[("ps", bk)])
                            C("act", lambda e, g=g, tc=tc, bk=bk: e.activation(out=ypre[:, g, tc * 512:(tc + 1) * 512], in_=ps[:, bk, :], func=AF.Identity,
                                                                                scale=pscale[:, g:g + 1]),
                              r=[("ps", bk), "pscale"], w=[("ypre", g, tc)])
                    self.phase_end("A1")
                with ExitStack() as es2:
                    wqkv = [sb(es2, "wqkv%d" % i, [128, 8, 384], BF16) for i in range(2)]
                    vaug = [sb(es2, "vaug%d" % i, [128, 16, 132], BF16) for i in range(2)]
                    qT = [sb(es2, "qT%d" % i, [128, S], BF16) for i in range(2)]
                    kT = [[sb(es2, "kT%d_%d" % (i, m), [128, S], BF16) for m in range(2)] for i in range(2)]
                    PT = [sb(es2, "PT%d" % i, [128, 16, 512], BF16) for i in range(2)]
                    ost = sb(es2, "ost", [128, 16, 2, 132], F32)
                    rtmp = [sb(es2, "rtmp%d" % i, [128, 4, 4, 8], F32) for i in range(4)]
                    qkb = [sb(es2, "qkb%d" % i, [128, 256], BF16) for i in range(4)]
                    rl = sb(es2, "rl", [128, 16, 2, 1], F32)
                    r1n = sb(es2, "r1n", [128, 16, 1], F32)
                    t0 = sb(es2, "t0", [128, 8, 128], F32)
                    t1 = sb(es2, "t1", [128, 8, 128], F32)
                    ss = sb(es2, "ss", [128, 16], F32)
                    rs = sb(es2, "rs", [128, 16, 1], F32)
                    onb = sb(es2, "onb", [128, 16, 128], BF16)
                    for i in range(2):
                        C("pool", lambda e, i=i: e.memset(vaug[i][:, :, 128:132], 1.0), w=[("vone", i)])
                        C("pool", lambda e, i=i: e.memset(kT[i][0][64:128, :], 0.0), w=[("kz", i, 0)])
                        C("pool", lambda e, i=i: e.memset(kT[i][1][0:64, :], 0.0), w=[("kz", i, 1)])
                    for h in range(NH):
                        hs = h % 2
                        Dq("pool", lambda e, h=h, hs=hs: e.dma_start(out=wqkv[hs][:], in_=d["w_qkv"].ap()[l, h].rearrange("(kc p) n -> p kc n", p=128)),
                           w=[("wqkv", hs)])
                        for tt in range(16):
                            i2 = tt % 4
                            bk = i2
                            C("pe", lambda e, tt=tt, bk=bk, hs=hs: [e.matmul(out=ps[:, bk, 0:384], lhsT=xT[:, kc, tt * 128:(tt + 1) * 128],
                                                                            rhs=wqkv[hs][:, kc, :], start=(kc == 0), stop=(kc == 7)) for kc in range(8)][-1],
                              r=[("xT", tt), ("wqkv", hs)], w=[("ps", bk)])
                            qk4 = ps[:, bk, 0:256].rearrange("p (a d) -> p a d", a=4)
                            cb = cos[:, tt:tt + 1, :].to_broadcast([128, 4, 8])
                            sn = sin[:, tt:tt + 1, :].to_broadcast([128, 4, 8])
                            rt = rtmp[i2]
                            C("dve", lambda e, qk4=qk4, cb=cb, sn=sn, rt=rt: [
                                e.tensor_tensor(out=rt[:, 0], in0=qk4[:, :, 0:8], in1=cb, op=ALU.mult),
                                e.tensor_tensor(out=rt[:, 1], in0=qk4[:, :, 8:16], in1=sn, op=ALU.mult),
                                e.tensor_tensor(out=rt[:, 2], in0=qk4[:, :, 8:16], in1=cb, op=ALU.mult),
                                e.tensor_tensor(out=rt[:, 3], in0=qk4[:, :, 0:8], in1=sn, op=ALU.mult)][-1],
                              r=[("ps", bk), "cos", "sin"], w=[("rt", i2)])
                            C("act", lambda e, bk=bk, i2=i2: e.activation(out=qkb[i2][:], in_=ps[:, bk, 0:256], func=AF.Copy),
                              r=[("ps", bk)], w=[("qkb", i2)])
                            C("act", lambda e, bk=bk, tt=tt, hs=hs: e.activation(out=vaug[hs][:, tt, 0:128], in_=ps[:, bk, 256:384], func=AF.Copy),
                              r=[("ps", bk)], w=[("v", hs, tt)])
                            qb4 = qkb[i2][:].rearrange("p (a d) -> p a d", a=4)
                            C("dve", lambda e, qb4=qb4, rt=rt: [
                                e.tensor_tensor(out=qb4[:, :, 0:8], in0=rt[:, 0], in1=rt[:, 1], op=ALU.subtract),
                                e.tensor_tensor(out=qb4[:, :, 8:16], in0=rt[:, 2], in1=rt[:, 3], op=ALU.add)][-1],
                              r=[("rt", i2), ("qkb", i2)], w=[("qkb", i2)])
                            bk2 = 4 + i2
                            C("pe", lambda e, bk2=bk2, i2=i2: [e.transpose(out=psb[:, bk2, 0:128], in_=qkb[i2][:, 0:128], identity=self.identb[:]),
                                                               e.transpose(out=psb[:, bk2, 128:256], in_=qkb[i2][:, 128:256], identity=self.identb[:])][-1],
                              r=[("qkb", i2), "k0"], w=[("ps", bk2)])
                            C("dve", lambda e, bk2=bk2, tt=tt, hs=hs: [e.tensor_copy(out=qT[hs][:, tt * 128:(tt + 1) * 128], in_=psb[:, bk2, 0:128]),
                                                                       e.tensor_copy(out=kT[hs][0][0:64, tt * 128:(tt + 1) * 128], in_=psb[0:64, bk2, 128:256]),
                                                                       e.tensor_copy(out=kT[hs][1][64:128, tt * 128:(tt + 1) * 128], in_=psb[64:128, bk2, 128:256])][-1],
                              r=[("ps", bk2), ("kz", hs, 0), ("kz", hs, 1)], w=[("qk", hs, tt)])
                        if h == 0 and self.stop_after == "A2a":
                            self.phase_end("A2a")
                        chunks = [(m, qc) for qc in range(4) for m in range(2)]
                        sbank = [0]

                        def scores(m, qc, hs=hs):
                            nkb = 4 * qc + 4
                            for kb in range(nkb):
                                jp = kb - 4 * qc
                                q0 = (4 * qc + max(jp, 0)) * 128
                                n = (4 * qc + 4) * 128 - q0
                                bk = sbank[0] % 4
                                sbank[0] += 1
                                C("pe", lambda e, m=m, kb=kb, q0=q0, n=n, bk=bk: e.matmul(
                                    out=ps[:, bk, 0:n], lhsT=kT[hs][m][:, kb * 128:(kb + 1) * 128],
                                    rhs=qT[hs][:, q0:q0 + n], start=True, stop=True),
                                  r=[("qk", hs, t) for t in range(q0 // 128, 4 * qc + 4)] + [("qk", hs, kb)], w=[("ps", bk)])
                                C("act", lambda e, m=m, kb=kb, n=n, bk=bk: e.activation(out=PT[m][:, kb, 0:n], in_=ps[:, bk, 0:n], func=AF.Exp, scale=0.125),
                                  r=[("ps", bk)], w=[("PT", m, kb)])
                                if jp >= 0:
                                    C("pool", lambda e, m=m, kb=kb: e.tensor_tensor(out=PT[m][:, kb, 0:128], in0=PT[m][:, kb, 0:128],
                                                                                    in1=self.mask01[:], op=ALU.mult),
                                      r=[("PT", m, kb), "k2"], w=[("PT", m, kb)])

                        obank = [0]

                        def av(m, qc, hs=hs):
                            for j in range(4):
                                qb = 4 * qc + j
                                bk = 4 + obank[0] % 4
                                obank[0] += 1

                                def f(e, m=m, qc=qc, j=j, qb=qb, bk=bk):
                                    ins = None
                                    for kb in range(qb + 1):
                                        col = (j - max(kb - 4 * qc, 0)) * 128
                                        ins = e.matmul(out=ps[:, bk, 0:130], lhsT=PT[m][:, kb, col:col + 128], rhs=vaug[hs][:, kb, 0:130],
                                                       start=(kb == 0), stop=(kb == qb))
                                    return ins
                                C("pe", f, r=[("PT", m, kb) for kb in range(qb + 1)] + [("v", hs, kb) for kb in range(qb + 1)] + [("vone", hs)],
                                  w=[("ps", bk)])
                                C("act", lambda e, m=m, qb=qb, bk=bk: e.activation(out=ost[:, qb, m, 0:129], in_=ps[:, bk, 0:129], func=AF.Copy),
                                  r=[("ps", bk)], w=[("ost", qb, m)])

                        scores(*chunks[0])
                        for ci in range(len(chunks)):
                            if ci + 1 < len(chunks):
                                scores(*chunks[ci + 1])
                            av(*chunks[ci])
                        if h == 0 and self.stop_after == "A2b":
                            self.phase_end("A2b")
                        ostk = [("ost", qb, m) for qb in range(16) for m in range(2)]
                        C("dve", lambda e: e.reciprocal(out=rl[:], in_=ost[:, :, :, 128:129]), r=ostk, w=["rl"])
                        C("dve", lambda e: e.tensor_scalar(out=r1n[:], in0=rl[:, :, 1, :], scalar1=neglam[:, 0:1], scalar2=None, op0=ALU.mult),
                          r=["rl", "neglam"], w=["r1n"])
                        for hf in range(2):
                            qs = slice(hf * 8, hf * 8 + 8)
                            C("dve", lambda e, qs=qs: e.tensor_tensor(out=t0[:], in0=ost[:, qs, 0, 0:128], in1=rl[:, qs, 0, :].to_broadcast([128, 8, 128]), op=ALU.mult),
                              r=ostk + ["rl", "t0"], w=["t0"])
                            C("dve", lambda e, qs=qs: e.tensor_tensor(out=t1[:], in0=ost[:, qs, 1, 0:128], in1=r1n[:, qs, :].to_broadcast([128, 8, 128]), op=ALU.mult),
                              r=ostk + ["r1n", "t1"], w=["t1"])
                            C("dve", lambda e: e.tensor_tensor(out=t0[:], in0=t0[:], in1=t1[:], op=ALU.add), r=["t0", "t1"], w=["t0"])
                            C("dve", lambda e: e.tensor_tensor(out=t1[:], in0=t0[:], in1=t0[:], op=ALU.mult), r=["t0", "t1"], w=["t1"])
                            C("dve", lambda e, qs=qs: e.tensor_reduce(out=ss[:, qs], in_=t1[:], axis=AX.X, op=ALU.add), r=["t1"], w=["ss"])
                            C("act", lambda e, qs=qs: e.activation(out=rs[:, qs, 0], in_=ss[:, qs], func=AF.Sqrt, bias=self.epsc[:], scale=1.0 / 128.0),
                              r=["ss", "k8"], w=["rs"])
                            C("dve", lambda e, qs=qs: e.reciprocal(out=rs[:, qs, :], in_=rs[:, qs, :]), r=["rs"], w=["rs"])
                            C("dve", lambda e, qs=qs: e.tensor_tensor(out=t1[:], in0=t0[:], in1=rs[:, qs, :].to_broadcast([128, 8, 128]), op=ALU.mult),
                              r=["t0", "rs", "t1"], w=["t1"])
                            C("dve", lambda e, qs=qs: e.tensor_tensor(out=onb[:, qs, :], in0=t1[:], in1=sublnw[:].to_broadcast([128, 8, 128]), op=ALU.mult),
                              r=["t1", "sublnw", "onb"], w=["onb"])
                        if h == 0 and self.stop_after == "A2c":
                            self.phase_end("A2c")
                        for half in range(2):
                            bk = 2 + half
                            C("pe", lambda e, half=half, bk=bk: [e.transpose(out=psb[:, bk, c * 128:(c + 1) * 128], in_=onb[:, half * 8 + c, :],
                                                                             identity=self.identb[:]) for c in range(8)][-1],
                              r=["onb", "k0"], w=[("ps", bk)])
                            C("act", lambda e, half=half, bk=bk, h=h: e.activation(out=onT[:, h, half * 1024:(half + 1) * 1024], in_=psb[:, bk, :], func=AF.Copy),
                              r=[("ps", bk)], w=[("onT", h, half)])
                    self.phase_end("A2")
                merged = sb(es_o, "merged", [128, 8, S], BF16)
                with ExitStack() as es3:
                    wg = [sb(es3, "wg%d" % i, [128, 8, 2, 128], BF16) for i in range(2)]
                    wab = [sb(es3, "wab%d" % i, [128, 8, 128], BF16) for i in range(2)]
                    wpb = [sb(es3, "wpb%d" % i, [128, 4, 128], BF16) for i in range(2)]
                    sg = [[sb(es3, "sg%d_%d" % (i, k), [128, 512], F32) for k in range(2)] for i in range(2)]
                    mt = [[sb(es3, "mt%d_%d" % (i, k), [128, 512], F32) for k in range(2)] for i in range(2)]
                    wgv = d["w_gate"].ap()[l].rearrange("(kc p) (br jj n) -> p kc br jj n", p=128, br=2, jj=8)
                    wabv = d["w_ab"].ap()[l].rearrange("(kc p) (jj n) -> p kc jj n", p=128, jj=8)
                    wpbv = d["w_pb"].ap()[l].rearrange("(kc p) (jj n) -> p kc jj n", p=128, jj=8)
                    it = 0
                    for j in range(8):
                        js = j % 2
                        for br in range(2):
                            Dq("pool", lambda e, j=j, js=js, br=br: e.dma_start(out=wg[js][:, :, br, :], in_=wgv[:, :, br, j, :]), w=[("wg", js, br)])
                        Dq("pool", lambda e, j=j, js=js: e.dma_start(out=wab[js][:], in_=wabv[:, :, j, :]), w=[("wab", js)])
                        Dq("pool", lambda e, j=j, js=js: e.dma_start(out=wpb[js][:], in_=wpbv[:, :, j, :]), w=[("wpb", js)])
                        for tc in range(4):
                            s2 = it % 2
                            it += 1
                            b0 = s2 * 4
                            tsl = slice(tc * 512, (tc + 1) * 512)
                            C("pe", lambda e, js=js, tsl=tsl, b0=b0: [e.matmul(out=ps[:, b0, :], lhsT=wpb[js][:, g, :], rhs=ypre[:, g, tsl],
                                                                              start=(g == 0), stop=(g == 3)) for g in range(4)][-1],
                              r=[("wpb", js)], w=[("ps", b0)])
                            C("pe", lambda e, js=js, tsl=tsl, b0=b0: [e.matmul(out=ps[:, b0 + 1, :], lhsT=wab[js][:, c, :], rhs=onT[:, c, tsl],
                                                                              start=(c == 0), stop=(c == 7)) for c in range(8)][-1],
                              r=[("wab", js)], w=[("ps", b0 + 1)])
                            for br in range(2):
                                C("pe", lambda e, js=js, tsl=tsl, b0=b0, br=br: [e.matmul(out=ps[:, b0 + 2 + br, :], lhsT=wg[js][:, kc, br, :], rhs=xT[:, kc, tsl],
                                                                                         start=(kc == 0), stop=(kc == 7)) for kc in range(8)][-1],
                                  r=[("wg", js, br)], w=[("ps", b0 + 2 + br)])
                                C("act", lambda e, s2=s2, b0=b0, br=br: e.activation(out=sg[s2][br][:], in_=ps[:, b0 + 2 + br, :], func=AF.Sigmoid),
                                  r=[("ps", b0 + 2 + br)], w=[("sg", s2, br)])
                                C("dve", lambda e, s2=s2, b0=b0, br=br: e.tensor_tensor(out=mt[s2][br][:], in0=sg[s2][br][:], in1=ps[:, b0 + br, :], op=ALU.mult),
                                  r=[("sg", s2, br), ("ps", b0 + br)], w=[("mt", s2, br)])
                            C("pool", lambda e, s2=s2, j=j, tsl=tsl: e.tensor_tensor(out=merged[:, j, tsl], in0=mt[s2][0][:], in1=mt[s2][1][:], op=ALU.add),
                              r=[("mt", s2, 0), ("mt", s2, 1)], w=[("merged", j, tsl.start)])
                    self.phase_end("A3a")
                with ExitStack() as es4:
                    wout = sb(es4, "wout", [128, 8, D], BF16)
                    wr = sb(es4, "wr", [128, 8, E], F32)
                    brb = sb(es4, "brb", [128, E], F32)
                    gb = sb(es4, "g1b", [128, D], F32)
                    bb = sb(es4, "b1b", [128, D], F32)
                    Dq("pool", lambda e: e.dma_start(out=wout[:], in_=d["w_out"].ap()[l].rearrange("(kc p) n -> p kc n", p=128)), w=["wout"])
                    Dq("sp", lambda e: e.dma_start(out=wr[:], in_=d["w_router"].ap()[l].rearrange("(kc p) n -> p kc n", p=128)), w=["wr"])
                    Dq("sp", lambda e: e.dma_start(out=brb[:], in_=d["b_router"].ap()[l:l + 1].to_broadcast([128, E])), w=["brb"])
                    Dq("sp", lambda e: e.dma_start(out=gb[:], in_=d["ln1_g"].ap()[l:l + 1].to_broadcast([128, D])), w=["lnp"])
                    Dq("sp", lambda e: e.dma_start(out=bb[:], in_=d["ln1_b"].ap()[l:l + 1].to_broadcast([128, D])), w=["lnp"])
                    xin = [sb(es4, "xin%d" % i, [128, D], F32) for i in range(2)]
                    zt = [sb(es4, "zt%d" % i, [128, D], F32) for i in range(2)]
                    x1t = [sb(es4, "x1t%d" % i, [128, D], F32) for i in range(2)]
                    x1b = [sb(es4, "x1b%d" % i, [128, D], BF16) for i in range(2)]
                    x1T = [sb(es4, "x1T%d" % i, [128, 8, 128], F32) for i in range(2)]
                    lnt = [(sb(es4, "st%d" % i, [128, 2, 6], F32), sb(es4, "mv%d" % i, [128, 2], F32), sb(es4, "sd%d" % i, [128, 1], F32),
                            sb(es4, "rstd%d" % i, [128, 1], F32), sb(es4, "xn%d" % i, [128, D], F32)) for i in range(2)]
                    Lt = [sb(es4, "Lt%d" % i, [128, E], F32) for i in range(2)]
                    mx8 = [sb(es4, "mx8%d" % i, [128, 8], F32) for i in range(2)]
                    Mk = [sb(es4, "Mk%d" % i, [128, E], F32) for i in range(2)]
                    nv0 = [sb(es4, "nv0%d" % i, [128, 1], F32) for i in range(2)]
                    ek = [sb(es4, "ek%d" % i, [128, 4], F32) for i in range(2)]
                    gs = [sb(es4, "gs%d" % i, [128, 1], F32) for i in range(2)]
                    posc = [sb(es4, "posc%d" % i, [128, 1, E], F32) for i in range(2)]
                    oh = [sb(es4, "oh%d" % i, [128, 4, E], F32) for i in range(2)]
                    dstf = [sb(es4, "dstf%d" % i, [128, 4], F32) for i in range(2)]
                    for tt in range(16):
                        i2 = tt % 2
                        ti = b * 16 + tt
                        tg = "n%d_" % i2
                        rows = slice(tok0 + tt * 128, tok0 + (tt + 1) * 128)
                        Dq("sp", lambda e, i2=i2, rows=rows: e.dma_start(out=xin[i2][:], in_=xsrc.ap()[rows, :]), w=[("xin", i2)])
                        for n in range(2):
                            C("pe", lambda e, tt=tt, n=n, i2=i2: [e.matmul(out=ps[:, i2 * 2 + n, :], lhsT=merged[:, j, tt * 128:(tt + 1) * 128],
                                                                         rhs=wout[:, j, n * 512:(n + 1) * 512], start=(j == 0), stop=(j == 7)) for j in range(8)][-1],
                              r=["wout"], w=[("ps", i2 * 2 + n)])
                            C("dve", lambda e, n=n, i2=i2: e.scalar_tensor_tensor(out=zt[i2][:, n * 512:(n + 1) * 512], in0=xin[i2][:, n * 512:(n + 1) * 512], scalar=ALPHA,
                                                                                  in1=ps[:, i2 * 2 + n, :], op0=ALU.mult, op1=ALU.add),
                              r=[("xin", i2), ("ps", i2 * 2 + n)], w=[(tg + "z", n)])
                        self.layer_norm_tile(lnt[i2], zt[i2], gb, bb, x1t[i2][:], [(tg + "z", 0), (tg + "z", 1)], ("x1t", i2), tg)
                        Dq("sp", lambda e, i2=i2, rows=rows: e.dma_start(out=d["xres"].ap()[rows, :], in_=x1t[i2][:]), r=[("x1t", i2)], w=[("xres", ti)])
                        if self.debug:
                            Dq("sp", lambda e, i2=i2, rows=rows: e.dma_start(out=d["dbg_x1"].ap()[rows, :], in_=x1t[i2][:]), r=[("x1t", i2)], w=[("dbgx1", ti)])
                        C("act", lambda e, i2=i2: e.activation(out=x1b[i2][:], in_=x1t[i2][:], func=AF.Copy), r=[("x1t", i2)], w=[("x1b", i2)])
                        for hf in range(2):
                            bk = 4 + hf
                            C("pe", lambda e, i2=i2, hf=hf, bk=bk: [e.transpose(out=ps[:, bk, c * 128:(c + 1) * 128], in_=x1t[i2][:, (hf * 4 + c) * 128:(hf * 4 + c + 1) * 128],
                                                                               identity=self.identf[:]) for c in range(4)][-1],
                              r=[("x1t", i2), "k1"], w=[("ps", bk)])
                            C("act", lambda e, i2=i2, hf=hf, bk=bk: e.activation(out=x1T[i2][:, hf * 4:(hf + 1) * 4, :], in_=ps[:, bk, :].rearrange("p (c t) -> p c t", c=4),
                                                                                 func=AF.Copy),
                              r=[("ps", bk)], w=[("x1T", i2, hf)])
                        C("pe", lambda e, i2=i2: [e.matmul(out=ps[:, 6, 0:E], lhsT=x1T[i2][:, kc, :], rhs=wr[:, kc, :], start=(kc == 0), stop=(kc == 7)) for kc in range(8)][-1],
                          r=[("x1T", i2, 0), ("x1T", i2, 1), "wr"], w=[("ps", 6)])
                        C("dve", lambda e, i2=i2: e.tensor_tensor(out=Lt[i2][:], in0=ps[:, 6, 0:E], in1=brb[:], op=ALU.add), r=[("ps", 6), "brb"], w=[tg + "L"])
                        C("dve", lambda e, i2=i2: e.max(out=mx8[i2][:], in_=Lt[i2][:]), r=[tg + "L"], w=[tg + "mx"])
                        C("dve", lambda e, i2=i2: e.tensor_scalar(out=Mk[i2][:], in0=Lt[i2][:], scalar1=mx8[i2][:, 3:4], scalar2=None, op0=ALU.is_ge),
                          r=[tg + "L", tg + "mx"], w=[tg + "Mk"])
                        C("dve", lambda e, i2=i2: e.tensor_scalar(out=nv0[i2][:], in0=mx8[i2][:, 0:1], scalar1=-1.0, scalar2=None, op0=ALU.mult),
                          r=[tg + "mx"], w=[tg + "nv"])
                        C("act", lambda e, i2=i2: e.activation(out=ek[i2][:], in_=mx8[i2][:, 0:4], func=AF.Exp, bias=nv0[i2][:], scale=1.0, accum_out=gs[i2][:]),
                          r=[tg + "mx", tg + "nv"], w=[tg + "ek"])
                        C("dve", lambda e, i2=i2: e.reciprocal(out=gs[i2][:], in_=gs[i2][:]), r=[tg + "ek"], w=[tg + "gs"])
                        C("dve", lambda e, i2=i2, ti=ti: e.tensor_scalar(out=self.gate[:, ti, :], in0=ek[i2][:], scalar1=gs[i2][:], scalar2=None, op0=ALU.mult),
                          r=[tg + "ek", tg + "gs"], w=[("gate", ti)])
                        C("pe", lambda e, i2=i2: [e.matmul(out=ps[:, 7, 0:E], lhsT=self.ustrict[:], rhs=Mk[i2][:], start=True, stop=False),
                                                  e.matmul(out=ps[:, 7, 0:E], lhsT=self.ones[:], rhs=self.macc[:], start=False, stop=True)][-1],
                          r=[tg + "Mk", "macc", "k3", "k4"], w=[("ps", 7)])
                        C("dve", lambda e, i2=i2: e.scalar_tensor_tensor(out=posc[i2][:, 0, :], in0=ps[:, 7, 0:E], scalar=float(CAP - 1), in1=self.ec[:],
                                                                         op0=ALU.min, op1=ALU.add),
                          r=[("ps", 7), "k5"], w=[tg + "pc"])
                        C("dve", lambda e, i2=i2: e.tensor_tensor(out=self.macc[:], in0=self.macc[:], in1=Mk[i2][:], op=ALU.add), r=["macc", tg + "Mk"], w=["macc"])
                        C("dve", lambda e, i2=i2: [e.tensor_scalar(out=oh[i2][:, k, :], in0=Lt[i2][:], scalar1=mx8[i2][:, k:k + 1], scalar2=None, op0=ALU.is_equal)
                                                   for k in range(4)][-1],
                          r=[tg + "L", tg + "mx"], w=[tg + "oh"])
                        C("dve", lambda e, i2=i2: e.tensor_tensor(out=oh[i2][:], in0=oh[i2][:], in1=posc[i2][:].to_broadcast([128, 4, E]), op=ALU.mult),
                          r=[tg + "oh", tg + "pc"], w=[tg + "oh"])
                        C("dve", lambda e, i2=i2: e.tensor_reduce(out=dstf[i2][:], in_=oh[i2][:], axis=AX.X, op=ALU.add), r=[tg + "oh"], w=[tg + "df"])
                        C("dve", lambda e, i2=i2, ti=ti: e.tensor_copy(out=self.desti[:, ti, :], in_=dstf[i2][:]), r=[tg + "df"], w=[("desti", ti)])
                        for k in range(4):
                            Dq("pool", lambda e, i2=i2, ti=ti, k=k: e.indirect_dma_start(
                                out=d["xsbuf"].ap(), out_offset=bass.IndirectOffsetOnAxis(ap=self.desti[:, ti, k:k + 1], axis=0),
                                in_=x1b[i2][:], in_offset=None),
                               r=[("x1b", i2), ("desti", ti)], w=[("xsd", ti, k)])
                    self.phase_end("A3b")

    def moe(self, l):
        nc, d, C, Dq, sb = self.nc, self.d, self.C, self.Dq, self.sb
        ps, psb = self.ps, self.psb
        with ExitStack() as es:
            wgu = [sb(es, "wgu%d" % i, [128, 8, 2 * D], BF16) for i in range(2)]
            wdn = [sb(es, "wdn%d" % i, [128, 8, D], BF16) for i in range(2)]
            xs = [sb(es, "xs%d" % i, [128, NR, D], BF16) for i in range(2)]
            xsT = [sb(es, "xsT%d" % i, [128, 8, CAP], BF16) for i in range(2)]
            actT = sb(es, "actT", [128, 8, CAP], BF16)
            hg = [sb(es, "hg%d" % i, [128, 2, CAP // 2], F32) for i in range(2)]
            hu = [sb(es, "hu%d" % i, [128, 2, CAP // 2], F32) for i in range(2)]
            sgm = [sb(es, "sgm%d" % i, [128, 2, CAP // 2], F32) for i in range(2)]
            tt_ = [sb(es, "tt%d" % i, [128, 2, CAP // 2], F32) for i in range(2)]
            yst = [sb(es, "yst%d" % i, [128, D], F32) for i in range(2)]
            bdn = [sb(es, "bdn%d" % i, [128, D], F32) for i in range(2)]
            bgu = sb(es, "bgu", [128, E, 16], F32)
            Dq("sp", lambda e: e.dma_start(out=bgu[:], in_=d["b_gu"].ap()[l]), w=["bgu"])
            C("dve", lambda e: e.tensor_scalar(out=bgu[:, :, 8:16], in0=bgu[:, :, 8:16], scalar1=1.0, scalar2=None, op0=ALU.add), r=["bgu"], w=["bgu"])
            H = CAP // 2

            def load_w(e_):
                s = e_ % 2
                gv = d["w_gu"].ap()[l, e_].rearrange("(kc p) n -> p kc n", p=128)
                dv = d["w_down"].ap()[l, e_].rearrange("(kc p) n -> p kc n", p=128)
                for q in range(4):
                    Dq("pool", lambda e, s=s, q=q, gv=gv: e.dma_start(out=wgu[s][:, 2 * q:2 * q + 2, :], in_=gv[:, 2 * q:2 * q + 2, :]), w=[("wgu", s, q)])
                for q in range(2):
                    Dq("pool", lambda e, s=s, q=q, dv=dv: e.dma_start(out=wdn[s][:, 4 * q:4 * q + 4, :], in_=dv[:, 4 * q:4 * q + 4, :]), w=[("wdn", s, q)])
                Dq("sp", lambda e, s=s, e_=e_: e.dma_start(out=bdn[s][:], in_=d["b_down"].ap()[l, e_:e_ + 1].to_broadcast([128, D])), w=[("bdn", s)])

            def load_xs(e_):
                xb_ = e_ % 2
                Dq("sp", lambda e, e_=e_, xb_=xb_: e.dma_start(out=xs[xb_][:], in_=d["xsbuf"].ap()[e_ * CAP:(e_ + 1) * CAP, :].rearrange("(r p) n -> p r n", p=128)),
                   w=[("xs", xb_)])

            def transposes(e_):
                xb_ = e_ % 2
                for r in range(NR):
                    bk = 6 + r % 2
                    C("pe", lambda e, r=r, bk=bk, xb_=xb_: [e.transpose(out=psb[:, bk, c * 128:(c + 1) * 128], in_=xs[xb_][:, r, c * 128:(c + 1) * 128], identity=self.identb[:])
                                                           for c in range(8)][-1],
                      r=[("xs", xb_), "k0"], w=[("ps", bk)])
                    C("act", lambda e, r=r, bk=bk, xb_=xb_: e.activation(out=xsT[xb_][:, :, r * 128:(r + 1) * 128], in_=psb[:, bk, :].rearrange("p (c t) -> p c t", c=8), func=AF.Copy),
                      r=[("ps", bk)], w=[("xsT", xb_, r)])

            load_w(0)
            load_xs(0)
            transposes(0)
            cnt = 0
            for e_ in range(E):
                s = e_ % 2
                xb = e_ % 2
                if e_ + 1 < E:
                    load_w(e_ + 1)
                    load_xs(e_ + 1)
                xk = [("xsT", xb, r) for r in range(NR)]
                for c in range(8):
                    i2 = cnt % 2
                    cnt += 1
                    hb = (c % 2) * 4
                    for gu in range(2):
                        col0 = gu * D + c * 128
                        for hf in range(2):
                            bk = hb + gu * 2 + hf
                            C("pe", lambda e, s=s, col0=col0, hf=hf, bk=bk, xb=xb: [e.matmul(out=ps[:, bk, 0:H], lhsT=wgu[s][:, kc, col0:col0 + 128],
                                                                                    rhs=xsT[xb][:, kc, hf * H:(hf + 1) * H], start=(kc == 0), stop=(kc == 7)) for kc in range(8)][-1],
                              r=xk + [("wgu", s, q) for q in range(4)], w=[("ps", bk)])
                    C("dve", lambda e, i2=i2, e_=e_, c=c, hb=hb: e.tensor_scalar(out=hg[i2][:], in0=ps[:, hb:hb + 2, 0:H], scalar1=bgu[:, e_, c:c + 1], scalar2=SW_LIM,
                                                                         op0=ALU.add, op1=ALU.min),
                      r=[("ps", hb), ("ps", hb + 1), "bgu"], w=[("hg", i2)])
                    C("dve", lambda e, i2=i2, e_=e_, c=c, hb=hb: e.tensor_scalar(out=hu[i2][:], in0=ps[:, hb + 2:hb + 4, 0:H], scalar1=bgu[:, e_, 8 + c:9 + c], scalar2=SW_LIM + 1.0,
                                                                         op0=ALU.add, op1=ALU.min),
                      r=[("ps", hb + 2), ("ps", hb + 3), "bgu"], w=[("hu", i2)])
                    C("act", lambda e, i2=i2: e.activation(out=sgm[i2][:], in_=hg[i2][:], func=AF.Sigmoid, scale=SW_ALPHA), r=[("hg", i2)], w=[("sgm", i2)])
                    C("dve", lambda e, i2=i2: e.scalar_tensor_tensor(out=tt_[i2][:], in0=hu[i2][:], scalar=-SW_LIM + 1.0, in1=hg[i2][:], op0=ALU.max, op1=ALU.mult),
                      r=[("hu", i2), ("hg", i2)], w=[("tt", i2)])
                    C("pool", lambda e, i2=i2, c=c: e.tensor_tensor(out=actT[:, c, :].rearrange("p (a n) -> p a n", a=2), in0=tt_[i2][:], in1=sgm[i2][:], op=ALU.mult),
                      r=[("tt", i2), ("sgm", i2)], w=[("actT", c)])
                if e_ + 1 < E:
                    transposes(e_ + 1)
                ak = [("actT", c) for c in range(8)]
                for r in range(NR):
                    ys = r % 2
                    for n in range(2):
                        bk = 4 + n
                        C("pe", lambda e, s=s, r=r, n=n, bk=bk: [e.matmul(out=ps[:, bk, :], lhsT=actT[:, c, r * 128:(r + 1) * 128], rhs=wdn[s][:, c, n * 512:(n + 1) * 512],
                                                                         start=(c == 0), stop=(c == 7)) for c in range(8)][-1],
                          r=ak + [("wdn", s, 0), ("wdn", s, 1)], w=[("ps", bk)])
                        C("dve", lambda e, s=s, ys=ys, n=n, bk=bk: e.tensor_tensor(out=yst[ys][:, n * 512:(n + 1) * 512], in0=ps[:, bk, :], in1=bdn[s][:, n * 512:(n + 1) * 512], op=ALU.add),
                          r=[("ps", bk), ("bdn", s)], w=[("yst", ys, n)])
                    Dq("sp", lambda e, ys=ys, e_=e_, r=r: e.dma_start(out=d["ybuf"].ap()[e_ * CAP + r * 128: e_ * CAP + (r + 1) * 128, :], in_=yst[ys][:]),
                       r=[("yst", ys, 0), ("yst", ys, 1)], w=[("ybuf", e_, r)])
            self.phase_end("moe")

    def combine(self, l, xdst):
        nc, d, C, Dq, sb = self.nc, self.d, self.C, self.Dq, self.sb
        with ExitStack() as es:
            gb = sb(es, "g2b", [128, D], F32)
            bb = sb(es, "b2b", [128, D], F32)
            Dq("sp", lambda e: e.dma_start(out=gb[:], in_=d["ln2_g"].ap()[l:l + 1].to_broadcast([128, D])), w=["lnp"])
            Dq("sp", lambda e: e.dma_start(out=bb[:], in_=d["ln2_b"].ap()[l:l + 1].to_broadcast([128, D])), w=["lnp"])
            yk = [[sb(es, "yk%d_%d" % (i, k), [128, D], F32) for k in range(4)] for i in range(2)]
            x1 = [sb(es, "cx1%d" % i, [128, D], F32) for i in range(2)]
            acc = [sb(es, "acc%d" % i, [128, D], F32) for i in range(2)]
            zt = [sb(es, "cz%d" % i, [128, D], F32) for i in range(2)]
            ot = [sb(es, "cot%d" % i, [128, D], F32) for i in range(2)]
            lnt = [(sb(es, "cst%d" % i, [128, 2, 6], F32), sb(es, "cmv%d" % i, [128, 2], F32), sb(es, "csd%d" % i, [128, 1], F32),
                    sb(es, "crstd%d" % i, [128, 1], F32), sb(es, "cxn%d" % i, [128, D], F32)) for i in range(2)]
            for ti in range(NT):
                i2 = ti % 2
                tg = "c%d_" % i2
                rows = slice(ti * 128, (ti + 1) * 128)
                for k in range(4):
                    gn = ti * 4 + k
                    Dq("pool", lambda e, i2=i2, ti=ti, k=k: e.indirect_dma_start(
                        out=yk[i2][k][:], out_offset=None, in_=d["ybuf"].ap(),
                        in_offset=bass.IndirectOffsetOnAxis(ap=self.desti[:, ti, k:k + 1], axis=0)),
                       r=[("gch", gn - 2)], w=[("yk", i2, k), ("gch", gn)])
                import os
                cut = os.environ.get("COMBINE_CUT", "full")
                if cut == "gather":
                    C("dve", lambda e, i2=i2: e.tensor_copy(out=ot[i2][:], in_=yk[i2][0][:]), r=[("yk", i2, 0)], w=[("cot", i2)])
                    for k in range(1, 4):
                        C("dve", lambda e, i2=i2, k=k: e.tensor_tensor(out=ot[i2][:], in0=ot[i2][:], in1=yk[i2][k][:], op=ALU.add), r=[("yk", i2, k), ("cot", i2)], w=[("cot", i2)])
                    Dq("sp", lambda e, i2=i2, rows=rows: e.dma_start(out=xdst.ap()[rows, :], in_=ot[i2][:]), r=[("cot", i2)], w=[("xdst", ti)])
                    continue
                Dq("sp", lambda e, i2=i2, rows=rows: e.dma_start(out=x1[i2][:], in_=d["xres"].ap()[rows, :]), w=[("cx1", i2)])
                C("dve", lambda e, i2=i2, ti=ti: e.tensor_scalar(out=acc[i2][:], in0=yk[i2][0][:], scalar1=self.gate[:, ti, 0:1], scalar2=None, op0=ALU.mult),
                  r=[("yk", i2, 0)], w=[("acc", i2)])
                for k in range(1, 4):
                    C("dve", lambda e, i2=i2, ti=ti, k=k: e.scalar_tensor_tensor(out=acc[i2][:], in0=yk[i2][k][:], scalar=self.gate[:, ti, k:k + 1], in1=acc[i2][:],
                                                                                 op0=ALU.mult, op1=ALU.add),
                      r=[("yk", i2, k), ("acc", i2)], w=[("acc", i2)])
                C("dve", lambda e, i2=i2: e.scalar_tensor_tensor(out=zt[i2][:], in0=x1[i2][:], scalar=ALPHA, in1=acc[i2][:], op0=ALU.mult, op1=ALU.add),
                  r=[("cx1", i2), ("acc", i2)], w=[tg + "zz"])
                if cut == "acc":
                    Dq("sp", lambda e, i2=i2, rows=rows: e.dma_start(out=xdst.ap()[rows, :], in_=zt[i2][:]), r=[tg + "zz"], w=[("xdst", ti)])
                    continue
                self.layer_norm_tile(lnt[i2], zt[i2], gb, bb, ot[i2][:], [tg + "zz"], ("cot", i2), tg)
                Dq("sp", lambda e, i2=i2, rows=rows: e.dma_start(out=xdst.ap()[rows, :], in_=ot[i2][:]), r=[("cot", i2)], w=[("xdst", ti)])
            self.phase_end("combine")


def _consts():
    c = {}
    c["c_identb"] = np.eye(128, dtype=np.float32).astype(ml_dtypes.bfloat16)
    c["c_identf"] = np.eye(128, dtype=np.float32)
    k = np.arange(128)[:, None]
    q = np.arange(128)[None, :]
    c["c_mask01"] = (q >= k).astype(np.float32).astype(ml_dtypes.bfloat16)
    c["c_ustrict"] = (k < q).astype(np.float32)
    c["c_ones"] = np.ones((128, 128), np.float32)
    c["c_ec"] = np.broadcast_to((np.arange(E, dtype=np.float32) * CAP)[None, :], (128, E)).copy()
    inv = (ROPE_THETA ** (-np.arange(0, 16, 2, dtype=np.float32) / np.float32(16))).astype(np.float32)
    c["c_invfreq"] = np.broadcast_to(inv[None, :], (128, 8)).copy()
    ic = np.zeros((4, 16), np.float32)
    for g, w in enumerate((2, 4, 8, 16)):
        ic[g] = 1.0 / np.minimum(np.arange(1, 17), w).astype(np.float32)
    c["c_invcnt"] = np.broadcast_to(ic[None], (128, 4, 16)).copy()
    return c


def _layout(inp):
    f = lambda a: np.ascontiguousarray(np.asarray(a))
    w_in = f(inp["w_in"])
    sh = {}
    sh["w_pool"] = f(w_in[:, :, 0:512])
    q = w_in[:, :, 512:1536].reshape(L, D, NH, 128)
    k = w_in[:, :, 1536:2560].reshape(L, D, NH, 128)
    v = w_in[:, :, 2560:3584].reshape(L, D, NH, 128)
    sh["w_qkv"] = f(np.concatenate([q, k, v], axis=-1).transpose(0, 2, 1, 3))
    sh["w_gate"] = f(w_in[:, :, 3584:5632])
    sh["pool_w"] = f(inp["pool_w"])
    sh["pool_scale"] = f(np.asarray(inp["pool_scale"]).reshape(L, 4, 128).transpose(0, 2, 1))
    sh["w_pb"] = f(inp["w_pool_branch"])
    sh["w_ab"] = f(inp["w_attn_branch"])
    sh["lam4"] = f(np.stack([np.asarray(inp["lambda_q1"]), np.asarray(inp["lambda_k1"]),
                             np.asarray(inp["lambda_q2"]), np.asarray(inp["lambda_k2"])], axis=1))
    sh["subln_w"] = f(inp["subln_w"])
    sh["w_out"] = f(inp["w_out"])
    for n in ("ln1_g", "ln1_b", "w_router", "b_router", "w_gu", "w_down", "b_down", "ln2_g", "ln2_b"):
        sh[n] = f(inp[n])
    sh["b_gu"] = f(np.asarray(inp["b_gu"]).reshape(L, E, 16, 128).transpose(0, 3, 1, 2))
    sh.update(_consts())
    return sh


_NC_CACHE = {}


def kernel(**inputs):
    shared = _layout(inputs)
    x = np.asarray(inputs["x"], dtype=np.float32).reshape(NCORES, T, D)
    pos = np.asarray(inputs["positions"]).astype(np.int32).reshape(NCORES, NSEQ, 16, 128).transpose(0, 1, 3, 2)
    if "nc" not in _NC_CACHE:
        _NC_CACHE["nc"] = Builder().build()
    nc = _NC_CACHE["nc"]
    in_maps = []
    for c in range(NCORES):
        m = dict(shared)
        m["x"] = np.ascontiguousarray(x[c])
        m["pos"] = np.ascontiguousarray(pos[c])
        in_maps.append(m)
    res = run_bass_kernel_spmd(nc, in_maps, core_ids=list(range(NCORES)))
    out = np.stack([np.asarray(r["y"]) for r in res.results], axis=0)
    return out.reshape(16, S, D).astype(np.float32)
```

```python
import math
from contextlib import ExitStack

import numpy as np
import ml_dtypes

import concourse.bass as bass
import concourse.mybir as mybir
from concourse.bass_utils import run_bass_kernel_spmd

F32 = mybir.dt.float32
BF16 = mybir.dt.bfloat16
I32 = mybir.dt.int32
AF = mybir.ActivationFunctionType
ALU = mybir.AluOpType
AX = mybir.AxisListType

NCORES = 8
D = 1024
S = 2048
NSEQ = 2
T = NSEQ * S
NT = T // 128
L = 2
NH = 8
E = 32
TOPK = 4
CAP = 640
NR = CAP // 128
LN_EPS = 1e-5
ALPHA = (2 * L) ** 0.25
ROPE_THETA = 500000.0
SW_ALPHA = 1.702
SW_LIM = 7.0
SAME_ENG_SYNC = True


class _Op:
    __slots__ = ("eng", "kind", "fn", "reads", "writes", "deps", "need", "sem", "val", "prev")

    def __init__(self, eng, kind, fn, reads, writes):
        self.eng, self.kind, self.fn = eng, kind, fn
        self.reads, self.writes = tuple(reads), tuple(writes)
        self.deps, self.need, self.sem, self.val, self.prev = (), False, None, 0, 0


class Prog:
    ENGS = ("pe", "act", "dve", "pool", "sp")

    def __init__(self, nc, es):
        self.nc = nc
        self.sems = []
        self.csem = {}
        for e in self.ENGS:
            self.csem[e] = len(self.sems)
            self.sems.append(es.enter_context(nc.semaphore("c_" + e)))
        self.ccnt = {e: 0 for e in self.ENGS}
        self.lanes = {}
        for q, n in (("sp", 14), ("pool", 14), ("act", 2)):
            self.lanes[q] = []
            for i in range(n):
                self.lanes[q].append(len(self.sems))
                self.sems.append(es.enter_context(nc.semaphore("l_%s%d" % (q, i))))
        self.lcnt = {}
        for q in self.lanes:
            for s in self.lanes[q]:
                self.lcnt[s] = 0
        self.lnext = {q: 0 for q in self.lanes}
        self.waited = {e: {} for e in self.ENGS}
        self.ops = []

    def op(self, eng, kind, fn, reads=(), writes=()):
        self.ops.append(_Op(eng, kind, fn, reads, writes))

    def flush(self):
        ops, self.ops = self.ops, []
        if not ops:
            return
        last_w, readers = {}, {}
        for i, op in enumerate(ops):
            deps = set()
            for k in op.reads:
                if k in last_w:
                    deps.add(last_w[k])
                if isinstance(k, tuple) and k[0] == "ps":
                    for r in readers.get(k, ()):
                        if ops[r].eng != op.eng:
                            deps.add(r)
            for k in op.writes:
                if k in last_w:
                    deps.add(last_w[k])
                for r in readers.get(k, ()):
                    deps.add(r)
            deps.discard(i)
            fd = []
            for d in deps:
                p = ops[d]
                if p.kind == "c" and op.kind == "c" and p.eng == op.eng:
                    if op.eng == "pe" or not SAME_ENG_SYNC:
                        continue
                fd.append(d)
            op.deps = fd
            for d in fd:
                ops[d].need = True
            for k in op.reads:
                readers.setdefault(k, []).append(i)
            for k in op.writes:
                last_w[k] = i
                readers[k] = []
        lastc = {}
        for i, op in enumerate(ops):
            if op.kind == "c" and op.fn is not None:
                lastc[op.eng] = i
        for i in lastc.values():
            ops[i].need = True
        for op in ops:
            if op.kind == "c":
                if op.need:
                    self.ccnt[op.eng] += 1
                    op.sem, op.val = self.csem[op.eng], self.ccnt[op.eng]
            else:
                q = self.lanes[op.eng]
                s = q[self.lnext[op.eng] % len(q)]
                self.lnext[op.eng] += 1
                op.prev = self.lcnt[s]
                self.lcnt[s] += 16
                op.sem, op.val = s, self.lcnt[s]
        final = {self.csem[e]: self.ccnt[e] for e in self.ENGS}
        final.update(self.lcnt)
        with self.nc.Block() as blk:
            decos = (("pe", blk.tensor), ("act", blk.scalar), ("dve", blk.vector),
                     ("pool", blk.gpsimd), ("sp", blk.sync))
            for ename, deco in decos:
                def body(eng, ename=ename):
                    self._emit(ename, eng, ops, final)
                deco(body)

    def _wait(self, ename, eng, s, v):
        if v <= 0:
            return
        w = self.waited[ename]
        if w.get(s, 0) >= v:
            return
        eng.wait_ge(self.sems[s], v)
        w[s] = v

    def _emit(self, ename, eng, ops, final):
        for op in ops:
            if op.eng != ename:
                continue
            need = {}
            for d in op.deps:
                p = ops[d]
                if need.get(p.sem, 0) < p.val:
                    need[p.sem] = p.val
            if op.kind == "d" and op.prev > 0:
                if need.get(op.sem, 0) < op.prev:
                    need[op.sem] = op.prev
            for s, v in need.items():
                self._wait(ename, eng, s, v)
            if op.fn is None:
                continue
            ins = op.fn(eng)
            if op.kind == "d":
                ins.then_inc(self.sems[op.sem], 16)
            elif op.need:
                ins.then_inc(self.sems[op.sem], 1)
        for s, v in final.items():
            self._wait(ename, eng, s, v)


class StopBuild(Exception):
    pass


class Builder:
    def __init__(self, n_layers=L, debug=False, stop_after=None):
        self.stop_after = stop_after
        self.n_layers = n_layers
        self.debug = debug
        self.nc = bass.Bass("TRN2", target_bir_lowering=False)
        self.es = ExitStack()

    def phase_end(self, name):
        if self.debug:
            print("phase", name, "sbuf cur", self._cur, "peak", self._peak)
        self._peak = self._cur
        self.P.flush()
        if self.stop_after == name:
            raise StopBuild()

    def C(self, eng, fn, r=(), w=()):
        self.P.op(eng, "c", fn, r, w)

    def Dq(self, q, fn, r=(), w=()):
        self.P.op(q, "d", fn, r, w)

    def sb(self, es, name, shape, dt):
        self._uid = getattr(self, "_uid", 0) + 1
        nb = int(np.prod(shape[1:])) * (4 if dt in (F32, I32) else 2)
        nb = (nb + 31) // 32 * 32
        self._cur = getattr(self, "_cur", 0) + nb
        self._peak = max(getattr(self, "_peak", 0), self._cur)
        t = es.enter_context(self.nc.sbuf_tensor("%s_u%d" % (name, self._uid), list(shape), dt))

        def _rel(nb=nb):
            self._cur -= nb
        es.callback(_rel)
        return t

    def din(self, name, shape, dt):
        return self.nc.dram_tensor(name, list(shape), dt, kind="ExternalInput")

    def build(self):
        nc = self.nc
        es = self.es
        nl = self.n_layers
        d = {}
        d["x"] = self.din("x", [T, D], F32)
        d["pos"] = self.din("pos", [NSEQ, 128, 16], I32)
        d["w_pool"] = self.din("w_pool", [L, D, 512], F32)
        d["w_qkv"] = self.din("w_qkv", [L, NH, D, 384], F32)
        d["w_gate"] = self.din("w_gate", [L, D, 2048], F32)
        d["pool_w"] = self.din("pool_w", [L, 4, 128, 128], F32)
        d["pool_scale"] = self.din("pool_scale", [L, 128, 4], F32)
        d["w_pb"] = self.din("w_pb", [L, 512, D], F32)
        d["w_ab"] = self.din("w_ab", [L, D, D], F32)
        d["lam4"] = self.din("lam4", [L, 4, 64], F32)
        d["subln_w"] = self.din("subln_w", [L, 128], F32)
        d["w_out"] = self.din("w_out", [L, D, D], F32)
        d["ln1_g"] = self.din("ln1_g", [L, D], F32)
        d["ln1_b"] = self.din("ln1_b", [L, D], F32)
        d["w_router"] = self.din("w_router", [L, D, E], F32)
        d["b_router"] = self.din("b_router", [L, E], F32)
        d["w_gu"] = self.din("w_gu", [L, E, D, 2 * D], F32)
        d["b_gu"] = self.din("b_gu", [L, 128, E, 16], F32)
        d["w_down"] = self.din("w_down", [L, E, D, D], F32)
        d["b_down"] = self.din("b_down", [L, E, D], F32)
        d["ln2_g"] = self.din("ln2_g", [L, D], F32)
        d["ln2_b"] = self.din("ln2_b", [L, D], F32)
        d["c_identb"] = self.din("c_identb", [128, 128], BF16)
        d["c_identf"] = self.din("c_identf", [128, 128], F32)
        d["c_mask01"] = self.din("c_mask01", [128, 128], BF16)
        d["c_ustrict"] = self.din("c_ustrict", [128, 128], F32)
        d["c_ones"] = self.din("c_ones", [128, 128], F32)
        d["c_ec"] = self.din("c_ec", [128, E], F32)
        d["c_invfreq"] = self.din("c_invfreq", [128, 8], F32)
        d["c_invcnt"] = self.din("c_invcnt", [128, 4, 16], F32)
        d["y"] = nc.dram_tensor("y", [T, D], F32, kind="ExternalOutput")
        d["xres"] = nc.dram_tensor("xres", [T, D], F32, kind="Internal")
        d["xcur"] = nc.dram_tensor("xcur", [T, D], F32, kind="Internal")
        d["xsbuf"] = nc.dram_tensor("xsbuf", [E * CAP, D], BF16, kind="Internal")
        d["ybuf"] = nc.dram_tensor("ybuf", [E * CAP, D], F32, kind="Internal")
        if self.debug:
            d["dbg_x1"] = nc.dram_tensor("dbg_x1", [T, D], F32, kind="ExternalOutput")
        self.d = d

        self.P = Prog(nc, es)
        self.ps = es.enter_context(nc.psum_tensor("ps", [128, 8, 512], F32))
        self.psb = self.ps.bitcast(BF16)
        sb = self.sb
        self.identb = sb(es, "identb", [128, 128], BF16)
        self.identf = sb(es, "identf", [128, 128], F32)
        self.mask01 = sb(es, "mask01", [128, 128], BF16)
        self.ustrict = sb(es, "ustrict", [128, 128], F32)
        self.ones = sb(es, "ones", [128, 128], F32)
        self.ec = sb(es, "ec", [128, E], F32)
        self.invfreq = sb(es, "invfreq", [128, 1, 8], F32)
        self.invcnt = sb(es, "invcnt", [128, 4, 16], F32)
        self.gate = sb(es, "gate", [128, NT, TOPK], F32)
        self.desti = sb(es, "desti", [128, NT, TOPK], I32)
        self.macc = sb(es, "macc", [128, E], F32)
        self.epsc = sb(es, "epsc", [128, 1], F32)

        def ld(dst, src, key):
            self.Dq("sp", lambda e: e.dma_start(out=dst, in_=src), w=[key])
        ld(self.identb[:], d["c_identb"].ap(), "k0")
        ld(self.identf[:], d["c_identf"].ap(), "k1")
        ld(self.mask01[:], d["c_mask01"].ap(), "k2")
        ld(self.ustrict[:], d["c_ustrict"].ap(), "k3")
        ld(self.ones[:], d["c_ones"].ap(), "k4")
        ld(self.ec[:], d["c_ec"].ap(), "k5")
        ld(self.invfreq[:, 0, :], d["c_invfreq"].ap(), "k6")
        ld(self.invcnt[:], d["c_invcnt"].ap(), "k7")
        self.C("dve", lambda e: e.memset(self.epsc[:], LN_EPS), w=["k8"])
        with ExitStack() as es2:
            z = self.sb(es2, "zrows", [128, NR, D], BF16)
            self.C("dve", lambda e: e.memset(z[:], 0.0), w=["z"])
            xv = d["xsbuf"].ap().rearrange("(n r p) d -> n p r d", p=128, r=NR)
            for n in range(E * CAP // (128 * NR)):
                self.Dq("sp", lambda e, n=n: e.dma_start(out=xv[n], in_=z[:]), r=["z"], w=[("xs0", n)])
            self.phase_end("init")

        try:
            for l in range(nl):
                xsrc = d["x"] if l == 0 else d["xcur"]
                xdst = d["y"] if l == nl - 1 else d["xcur"]
                self.C("dve", lambda e: e.memset(self.macc[:], 0.0), w=["macc"])
                for b in range(NSEQ):
                    self.mixer(l, b, xsrc)
                self.moe(l)
                self.combine(l, xdst)
        except StopBuild:
            return nc
        self.es.close()
        return nc

    def layer_norm_tile(self, es_tiles, z, gb, bb, out, zkeys, outkey, tag):
        st, mv, sd, rstd, xn = es_tiles
        C = self.C
        C("dve", lambda e: [e.bn_stats(out=st[:, 0, :], in_=z[:, 0:512]),
                             e.bn_stats(out=st[:, 1, :], in_=z[:, 512:1024])][-1],
          r=list(zkeys), w=[tag + "st"])
        C("dve", lambda e: e.bn_aggr(out=mv[:], in_=st[:].rearrange("p a b -> p (a b)")), r=[tag + "st"], w=[tag + "mv"])
        C("act", lambda e: e.activation(out=sd[:], in_=mv[:, 1:2], func=AF.Sqrt, bias=self.epsc[:], scale=1.0),
          r=[tag + "mv"], w=[tag + "sd"])
        C("dve", lambda e: e.reciprocal(out=rstd[:], in_=sd[:]), r=[tag + "sd"], w=[tag + "rs"])
        C("dve", lambda e: e.tensor_scalar(out=xn[:], in0=z[:], scalar1=mv[:, 0:1], scalar2=rstd[:],
                                           op0=ALU.subtract, op1=ALU.mult), r=list(zkeys) + [tag + "mv", tag + "rs"], w=[tag + "xn"])
        C("pool", lambda e: e.tensor_tensor(out=xn[:], in0=xn[:], in1=gb[:], op=ALU.mult), r=[tag + "xn", "lnp"], w=[tag + "xn"])
        C("pool", lambda e: e.tensor_tensor(out=out, in0=xn[:], in1=bb[:], op=ALU.add), r=[tag + "xn", "lnp"], w=[outkey])

    def mixer(self, l, b, xsrc):
        nc, d, C, Dq, sb = self.nc, self.d, self.C, self.Dq, self.sb
        ps, psb = self.ps, self.psb
        tok0 = b * S
        lam_init = 0.8 - 0.6 * math.exp(-0.3 * l)
        if True:
            with ExitStack() as es_o:
                xT = sb(es_o, "xT", [128, 8, S], BF16)
                ypre = sb(es_o, "ypre", [128, 4, S], BF16)
                onT = sb(es_o, "onT", [128, 8, S], BF16)
                cos = sb(es_o, "cos", [128, 16, 8], F32)
                sin = sb(es_o, "sin", [128, 16, 8], F32)
                neglam = sb(es_o, "neglam", [128, 1], F32)
                sublnw = sb(es_o, "sublnw", [128, 1, 128], F32)
                with ExitStack() as es1:
                    xst = [sb(es1, "xst%d" % i, [128, D], F32) for i in range(2)]
                    xb = [sb(es1, "xb%d" % i, [128, D], BF16) for i in range(2)]
                    for tt in range(16):
                        i2 = tt % 2
                        Dq("sp", lambda e, tt=tt, i2=i2: e.dma_start(out=xst[i2][:], in_=xsrc.ap()[tok0 + tt * 128: tok0 + (tt + 1) * 128, :]),
                           w=[("xst", i2)])
                        C("act", lambda e, i2=i2: e.activation(out=xb[i2][:], in_=xst[i2][:], func=AF.Copy),
                          r=[("xst", i2)], w=[("xb", i2)])
                        C("pe", lambda e, i2=i2: [e.transpose(out=psb[:, i2, c * 128:(c + 1) * 128], in_=xb[i2][:, c * 128:(c + 1) * 128],
                                                              identity=self.identb[:]) for c in range(8)][-1],
                          r=[("xb", i2), "k0"], w=[("ps", i2)])
                        C("dve", lambda e, tt=tt, i2=i2: e.tensor_copy(out=xT[:, :, tt * 128:(tt + 1) * 128],
                                                                     in_=psb[:, i2, :].rearrange("p (c t) -> p c t", c=8)),
                          r=[("ps", i2)], w=[("xT", tt)])
                    posi = sb(es1, "posi", [128, 16], I32)
                    posf = sb(es1, "posf", [128, 16, 1], F32)
                    ang = sb(es1, "ang", [128, 16, 8], F32)
                    kf = sb(es1, "kf", [128, 16, 8], F32)
                    ki = sb(es1, "ki", [128, 16, 8], I32)
                    r2 = sb(es1, "r2", [128, 16, 8], F32)
                    yy = sb(es1, "yy", [128, 16, 8], F32)
                    mm = sb(es1, "mm", [128, 16, 8], F32)
                    Dq("sp", lambda e: e.dma_start(out=posi[:], in_=d["pos"].ap()[b]), w=["posi"])
                    C("dve", lambda e: e.tensor_copy(out=posf[:, :, 0], in_=posi[:]), r=["posi"], w=["posf"])
                    C("dve", lambda e: e.tensor_tensor(out=ang[:], in0=posf[:].to_broadcast([128, 16, 8]),
                                                       in1=self.invfreq[:].to_broadcast([128, 16, 8]), op=ALU.mult),
                      r=["posf", "k6"], w=["ang"])
                    TWO_PI = 2.0 * math.pi
                    C1 = 6.28125
                    C2 = TWO_PI - C1
                    C("dve", lambda e: e.tensor_scalar(out=kf[:], in0=ang[:], scalar1=1.0 / TWO_PI, scalar2=None, op0=ALU.mult),
                      r=["ang"], w=["kf"])
                    C("dve", lambda e: e.tensor_copy(out=ki[:], in_=kf[:]), r=["kf"], w=["ki"])
                    C("dve", lambda e: e.tensor_copy(out=kf[:], in_=ki[:]), r=["ki"], w=["kf"])
                    C("dve", lambda e: e.scalar_tensor_tensor(out=r2[:], in0=kf[:], scalar=-C1, in1=ang[:], op0=ALU.mult, op1=ALU.add),
                      r=["kf", "ang"], w=["r2"])
                    C("dve", lambda e: e.scalar_tensor_tensor(out=r2[:], in0=kf[:], scalar=-C2, in1=r2[:], op0=ALU.mult, op1=ALU.add),
                      r=["kf", "r2"], w=["r2"])
                    for shift, dst, nm in ((0.0, sin, "sin"), (math.pi / 2, cos, "cos")):
                        C("dve", lambda e, shift=shift: e.tensor_scalar(out=yy[:], in0=r2[:], scalar1=shift, scalar2=None, op0=ALU.add),
                          r=["r2"], w=["yy"])
                        C("dve", lambda e: e.tensor_scalar(out=mm[:], in0=yy[:], scalar1=math.pi, scalar2=None, op0=ALU.is_gt),
                          r=["yy"], w=["mm"])
                        C("dve", lambda e: e.scalar_tensor_tensor(out=yy[:], in0=mm[:], scalar=-TWO_PI, in1=yy[:], op0=ALU.mult, op1=ALU.add),
                          r=["mm", "yy"], w=["yy"])
                        C("dve", lambda e: e.tensor_scalar(out=mm[:], in0=yy[:], scalar1=-math.pi, scalar2=None, op0=ALU.is_lt),
                          r=["yy"], w=["mm"])
                        C("dve", lambda e: e.scalar_tensor_tensor(out=yy[:], in0=mm[:], scalar=TWO_PI, in1=yy[:], op0=ALU.mult, op1=ALU.add),
                          r=["mm", "yy"], w=["yy"])
                        C("dve", lambda e: e.tensor_scalar(out=yy[:], in0=yy[:], scalar1=math.pi, scalar2=-math.pi, op0=ALU.min, op1=ALU.max),
                          r=["yy"], w=["yy"])
                        C("act", lambda e, dst=dst: e.activation(out=dst[:], in_=yy[:], func=AF.Sin), r=["yy"], w=[nm])
                    l4 = sb(es1, "l4", [128, 4, 64], F32)
                    lp = sb(es1, "lp", [128, 2, 64], F32)
                    lsum = sb(es1, "lsum", [128, 2], F32)
                    lex = sb(es1, "lex", [128, 2], F32)
                    Dq("sp", lambda e: e.dma_start(out=l4[:], in_=d["lam4"].ap()[l:l + 1].to_broadcast([128, 4, 64])), w=["l4"])
                    C("dve", lambda e: e.tensor_tensor(out=lp[:], in0=l4[:, 0:4:2, :], in1=l4[:, 1:4:2, :], op=ALU.mult), r=["l4"], w=["lp"])
                    C("dve", lambda e: e.tensor_reduce(out=lsum[:], in_=lp[:], axis=AX.X, op=ALU.add), r=["lp"], w=["lsum"])
                    C("act", lambda e: e.activation(out=lex[:], in_=lsum[:], func=AF.Exp), r=["lsum"], w=["lex"])
                    C("dve", lambda e: e.tensor_tensor(out=neglam[:], in0=lex[:, 1:2], in1=lex[:, 0:1], op=ALU.subtract), r=["lex"], w=["neglam"])
                    C("dve", lambda e: e.tensor_scalar(out=neglam[:], in0=neglam[:], scalar1=-lam_init, scalar2=None, op0=ALU.add),
                      r=["neglam"], w=["neglam"])
                    Dq("sp", lambda e: e.dma_start(out=sublnw[:, 0, :], in_=d["subln_w"].ap()[l:l + 1].to_broadcast([128, 128])), w=["sublnw"])
                    C("act", lambda e: e.activation(out=sublnw[:], in_=sublnw[:], func=AF.Copy, scale=(1.0 - lam_init)), r=["sublnw"], w=["sublnw"])
                    wpool = sb(es1, "wpool", [128, 8, 512], BF16)
                    poolw = sb(es1, "poolw", [128, 4, 128], BF16)
                    pscale = sb(es1, "pscale", [128, 4], F32)
                    Dq("pool", lambda e: e.dma_start(out=wpool[:], in_=d["w_pool"].ap()[l].rearrange("(kc p) n -> p kc n", p=128)), w=["wpool"])
                    Dq("pool", lambda e: e.dma_start(out=poolw[:], in_=d["pool_w"].ap()[l].rearrange("g c n -> c g n")), w=["poolw"])
                    Dq("sp", lambda e: e.dma_start(out=pscale[:], in_=d["pool_scale"].ap()[l]), w=["pscale"])
                    upad = [sb(es1, "upad%d" % i, [128, 16 + S], F32) for i in range(2)]
                    sa = sb(es1, "sa", [128, 16 + S], F32)
                    sbb = sb(es1, "sbb", [128, 16 + S], F32)
                    dbf = sb(es1, "dbf", [128, S], BF16)
                    t16 = sb(es1, "t16", [128, 16], F32)
                    for i in range(2):
                        C("pool", lambda e, i=i: e.memset(upad[i][:, 0:16], 0.0), w=[("upz", i)])
                    C("pool", lambda e: e.memset(sa[:, 0:16], 0.0), w=["saz"])
                    C("pool", lambda e: e.memset(sbb[:, 0:16], 0.0), w=["sbz"])
                    for g in range(4):
                        u = upad[g % 2]
                        for tc in range(4):
                            bk = 2 + (g * 4 + tc) % 2
                            C("pe", lambda e, g=g, tc=tc, bk=bk: [e.matmul(out=ps[:, bk, :], lhsT=wpool[:, kc, g * 128:(g + 1) * 128],
                                                                          rhs=xT[:, kc, tc * 512:(tc + 1) * 512], start=(kc == 0), stop=(kc == 7))
                                                                 for kc in range(8)][-1],
                              r=[("xT", t) for t in range(tc * 4, tc * 4 + 4)] + ["wpool"], w=[("ps", bk)])
                            C("act", lambda e, u=u, tc=tc, bk=bk: e.activation(out=u[:, 16 + tc * 512: 16 + (tc + 1) * 512], in_=ps[:, bk, :], func=AF.Copy),
                              r=[("ps", bk)], w=[("u", g % 2)])
                        w = 2 ** (g + 1)
                        src, bufs = u, [sa, sbb]
                        sh = 1
                        for st in range(g + 1):
                            dst = bufs[st % 2]
                            C("dve", lambda e, src=src, dst=dst, sh=sh: e.tensor_tensor(out=dst[:, 16:16 + S], in0=src[:, 16:16 + S],
                                                                                         in1=src[:, 16 - sh:16 - sh + S], op=ALU.add),
                              r=[("u", g % 2), ("upz", g % 2), "saz", "sbz", "sa", "sbb"], w=["sa" if st % 2 == 0 else "sbb"])
                            src = dst
                            sh *= 2
                        sfin = src
                        C("dve", lambda e, sfin=sfin, u=u, w=w: e.scalar_tensor_tensor(out=dbf[:], in0=sfin[:, 16:16 + S], scalar=1.0 / w,
                                                                                        in1=u[:, 16:16 + S], op0=ALU.mult, op1=ALU.subtract),
                          r=["sa", "sbb", ("u", g % 2)], w=["dbf"])
                        C("dve", lambda e, sfin=sfin, g=g: e.tensor_tensor(out=t16[:], in0=sfin[:, 16:32], in1=self.invcnt[:, g, :], op=ALU.mult),
                          r=["sa", "sbb", "k7"], w=["t16"])
                        C("dve", lambda e, u=u: e.tensor_tensor(out=dbf[:, 0:16], in0=t16[:], in1=u[:, 16:32], op=ALU.subtract),
                          r=["t16", ("u", g % 2), "dbf"], w=["dbf"])
                        for tc in range(4):
                            bk = 4 + tc % 2
                            C("pe", lambda e, g=g, tc=tc, bk=bk: e.matmul(out=ps[:, bk, :], lhsT=poolw[:, g, :], rhs=dbf[:, tc * 512:(tc + 1) * 512],
                                                                          start=True, stop=True),
                              r=["dbf", "poolw"], w=[("ps", bk)])
                            C("act", lambda e, g=g, tc=tc, bk=bk: e.activation(out=ypre[:, g, tc * 512:(tc + 1) * 512], in_=ps[:, bk, :], func=AF.Identity,
                                                                                scale=pscale[:, g:g + 1]),
                              r=[("ps", bk), "pscale"], w=[("ypre", g, tc)])
                    self.phase_end("A1")
                with ExitStack() as es2:
                    wqkv = [sb(es2, "wqkv%d" % i, [128, 8, 384], BF16) for i in range(2)]
                    vaug = [sb(es2, "vaug%d" % i, [128, 16, 132], BF16) for i in range(2)]
                    qT = [sb(es2, "qT%d" % i, [128, S], BF16) for i in range(2)]
                    kT = [[sb(es2, "kT%d_%d" % (i, m), [128, S], BF16) for m in range(2)] for i in range(2)]
                    PT = [sb(es2, "PT%d" % i, [128, 16, 512], BF16) for i in range(2)]
                    ost = sb(es2, "ost", [128, 16, 2, 132], F32)
                    rtmp = [sb(es2, "rtmp%d" % i, [128, 4, 4, 8], F32) for i in range(4)]
                    qkb = [sb(es2, "qkb%d" % i, [128, 256], BF16) for i in range(4)]
                    rl = sb(es2, "rl", [128, 16, 2, 1], F32)
                    r1n = sb(es2, "r1n", [128, 16, 1], F32)
                    t0 = sb(es2, "t0", [128, 8, 128], F32)
                    t1 = sb(es2, "t1", [128, 8, 128], F32)
                    ss = sb(es2, "ss", [128, 16], F32)
                    rs = sb(es2, "rs", [128, 16, 1], F32)
                    onb = sb(es2, "onb", [128, 16, 128], BF16)
                    for i in range(2):
                        C("pool", lambda e, i=i: e.memset(vaug[i][:, :, 128:132], 1.0), w=[("vone", i)])
                        C("pool", lambda e, i=i: e.memset(kT[i][0][64:128, :], 0.0), w=[("kz", i, 0)])
                        C("pool", lambda e, i=i: e.memset(kT[i][1][0:64, :], 0.0), w=[("kz", i, 1)])
                    for h in range(NH):
                        hs = h % 2
                        Dq("pool", lambda e, h=h, hs=hs: e.dma_start(out=wqkv[hs][:], in_=d["w_qkv"].ap()[l, h].rearrange("(kc p) n -> p kc n", p=128)),
                           w=[("wqkv", hs)])
                        for tt in range(16):
                            i2 = tt % 4
                            bk = i2
                            C("pe", lambda e, tt=tt, bk=bk, hs=hs: [e.matmul(out=ps[:, bk, 0:384], lhsT=xT[:, kc, tt * 128:(tt + 1) * 128],
                                                                            rhs=wqkv[hs][:, kc, :], start=(kc == 0), stop=(kc == 7)) for kc in range(8)][-1],
                              r=[("xT", tt), ("wqkv", hs)], w=[("ps", bk)])
                            qk4 = ps[:, bk, 0:256].rearrange("p (a d) -> p a d", a=4)
                            cb = cos[:, tt:tt + 1, :].to_broadcast([128, 4, 8])
                            sn = sin[:, tt:tt + 1, :].to_broadcast([128, 4, 8])
                            rt = rtmp[i2]
                            C("dve", lambda e, qk4=qk4, cb=cb, sn=sn, rt=rt: [
                                e.tensor_tensor(out=rt[:, 0], in0=qk4[:, :, 0:8], in1=cb, op=ALU.mult),
                                e.tensor_tensor(out=rt[:, 1], in0=qk4[:, :, 8:16], in1=sn, op=ALU.mult),
                                e.tensor_tensor(out=rt[:, 2], in0=qk4[:, :, 8:16], in1=cb, op=ALU.mult),
                                e.tensor_tensor(out=rt[:, 3], in0=qk4[:, :, 0:8], in1=sn, op=ALU.mult)][-1],
                              r=[("ps", bk), "cos", "sin"], w=[("rt", i2)])
                            C("act", lambda e, bk=bk, i2=i2: e.activation(out=qkb[i2][:], in_=ps[:, bk, 0:256], func=AF.Copy),
                              r=[("ps", bk)], w=[("qkb", i2)])
                            C("act", lambda e, bk=bk, tt=tt, hs=hs: e.activation(out=vaug[hs][:, tt, 0:128], in_=ps[:, bk, 256:384], func=AF.Copy),
                              r=[("ps", bk)], w=[("v", hs, tt)])
                            qb4 = qkb[i2][:].rearrange("p (a d) -> p a d", a=4)
                            C("dve", lambda e, qb4=qb4, rt=rt: [
                                e.tensor_tensor(out=qb4[:, :, 0:8], in0=rt[:, 0], in1=rt[:, 1], op=ALU.subtract),
                                e.tensor_tensor(out=qb4[:, :, 8:16], in0=rt[:, 2], in1=rt[:, 3], op=ALU.add)][-1],
                              r=[("rt", i2), ("qkb", i2)], w=[("qkb", i2)])
                            bk2 = 4 + i2
                            C("pe", lambda e, bk2=bk2, i2=i2: [e.transpose(out=psb[:, bk2, 0:128], in_=qkb[i2][:, 0:128], identity=self.identb[:]),
                                                               e.transpose(out=psb[:, bk2, 128:256], in_=qkb[i2][:, 128:256], identity=self.identb[:])][-1],
                              r=[("qkb", i2), "k0"], w=[("ps", bk2)])
                            C("dve", lambda e, bk2=bk2, tt=tt, hs=hs: [e.tensor_copy(out=qT[hs][:, tt * 128:(tt + 1) * 128], in_=psb[:, bk2, 0:128]),
                                                                       e.tensor_copy(out=kT[hs][0][0:64, tt * 128:(tt + 1) * 128], in_=psb[0:64, bk2, 128:256]),
                                                                       e.tensor_copy(out=kT[hs][1][64:128, tt * 128:(tt + 1) * 128], in_=psb[64:128, bk2, 128:256])][-1],
                              r=[("ps", bk2), ("kz", hs, 0), ("kz", hs, 1)], w=[("qk", hs, tt)])
                        if h == 0 and self.stop_after == "A2a":
                            self.phase_end("A2a")
                        chunks = [(m, qc) for qc in range(4) for m in range(2)]
                        sbank = [0]

                        def scores(m, qc, hs=hs):
                            nkb = 4 * qc + 4
                            for kb in range(nkb):
                                jp = kb - 4 * qc
                                q0 = (4 * qc + max(jp, 0)) * 128
                                n = (4 * qc + 4) * 128 - q0
                                bk = sbank[0] % 4
                                sbank[0] += 1
                                C("pe", lambda e, m=m, kb=kb, q0=q0, n=n, bk=bk: e.matmul(
                                    out=ps[:, bk, 0:n], lhsT=kT[hs][m][:, kb * 128:(kb + 1) * 128],
                                    rhs=qT[hs][:, q0:q0 + n], start=True, stop=True),
                                  r=[("qk", hs, t) for t in range(q0 // 128, 4 * qc + 4)] + [("qk", hs, kb)], w=[("ps", bk)])
                                C("act", lambda e, m=m, kb=kb, n=n, bk=bk: e.activation(out=PT[m][:, kb, 0:n], in_=ps[:, bk, 0:n], func=AF.Exp, scale=0.125),
                                  r=[("ps", bk)], w=[("PT", m, kb)])
                                if jp >= 0:
                                    C("pool", lambda e, m=m, kb=kb: e.tensor_tensor(out=PT[m][:, kb, 0:128], in0=PT[m][:, kb, 0:128],
                                                                                    in1=self.mask01[:], op=ALU.mult),
                                      r=[("PT", m, kb), "k2"], w=[("PT", m, kb)])

                        obank = [0]

                        def av(m, qc, hs=hs):
                            for j in range(4):
                                qb = 4 * qc + j
                                bk = 4 + obank[0] % 4
                                obank[0] += 1

                                def f(e, m=m, qc=qc, j=j, qb=qb, bk=bk):
                                    ins = None
                                    for kb in range(qb + 1):
                                        col = (j - max(kb - 4 * qc, 0)) * 128
                                        ins = e.matmul(out=ps[:, bk, 0:130], lhsT=PT[m][:, kb, col:col + 128], rhs=vaug[hs][:, kb, 0:130],
                                                       start=(kb == 0), stop=(kb == qb))
                                    return ins
                                C("pe", f, r=[("PT", m, kb) for kb in range(qb + 1)] + [("v", hs, kb) for kb in range(qb + 1)] + [("vone", hs)],
                                  w=[("ps", bk)])
                                C("act", lambda e, m=m, qb=qb, bk=bk: e.activation(out=ost[:, qb, m, 0:129], in_=ps[:, bk, 0:129], func=AF.Copy),
                                  r=[("ps", bk)], w=[("ost", qb, m)])

                        scores(*chunks[0])
                        for ci in range(len(chunks)):
                            if ci + 1 < len(chunks):
                                scores(*chunks[ci + 1])
                            av(*chunks[ci])
                        if h == 0 and self.stop_after == "A2b":
                            self.phase_end("A2b")
                        ostk = [("ost", qb, m) for qb in range(16) for m in range(2)]
                        C("dve", lambda e: e.reciprocal(out=rl[:], in_=ost[:, :, :, 128:129]), r=ostk, w=["rl"])
                        C("dve", lambda e: e.tensor_scalar(out=r1n[:], in0=rl[:, :, 1, :], scalar1=neglam[:, 0:1], scalar2=None, op0=ALU.mult),
                          r=["rl", "neglam"], w=["r1n"])
                        for hf in range(2):
                            qs = slice(hf * 8, hf * 8 + 8)
                            C("dve", lambda e, qs=qs: e.tensor_tensor(out=t0[:], in0=ost[:, qs, 0, 0:128], in1=rl[:, qs, 0, :].to_broadcast([128, 8, 128]), op=ALU.mult),
                              r=ostk + ["rl", "t0"], w=["t0"])
                            C("dve", lambda e, qs=qs: e.tensor_tensor(out=t1[:], in0=ost[:, qs, 1, 0:128], in1=r1n[:, qs, :].to_broadcast([128, 8, 128]), op=ALU.mult),
                              r=ostk + ["r1n", "t1"], w=["t1"])
                            C("dve", lambda e: e.tensor_tensor(out=t0[:], in0=t0[:], in1=t1[:], op=ALU.add), r=["t0", "t1"], w=["t0"])
                            C("dve", lambda e: e.tensor_tensor(out=t1[:], in0=t0[:], in1=t0[:], op=ALU.mult), r=["t0", "t1"], w=["t1"])
                            C("dve", lambda e, qs=qs: e.tensor_reduce(out=ss[:, qs], in_=t1[:], axis=AX.X, op=ALU.add), r=["t1"], w=["ss"])
                            C("act", lambda e, qs=qs: e.activation(out=rs[:, qs, 0], in_=ss[:, qs], func=AF.Sqrt, bias=self.epsc[:], scale=1.0 / 128.0),
                              r=["ss", "k8"], w=["rs"])
                            C("dve", lambda e, qs=qs: e.reciprocal(out=rs[:, qs, :], in_=rs[:, qs, :]), r=["rs"], w=["rs"])
                            C("dve", lambda e, qs=qs: e.tensor_tensor(out=t1[:], in0=t0[:], in1=rs[:, qs, :].to_broadcast([128, 8, 128]), op=ALU.mult),
                              r=["t0", "rs", "t1"], w=["t1"])
                            C("dve", lambda e, qs=qs: e.tensor_tensor(out=onb[:, qs, :], in0=t1[:], in1=sublnw[:].to_broadcast([128, 8, 128]), op=ALU.mult),
                              r=["t1", "sublnw", "onb"], w=["onb"])
                        if h == 0 and self.stop_after == "A2c":
                            self.phase_end("A2c")
                        for half in range(2):
                            bk = 2 + half
                            C("pe", lambda e, half=half, bk=bk: [e.transpose(out=psb[:, bk, c * 128:(c + 1) * 128], in_=onb[:, half * 8 + c, :],
                                                                             identity=self.identb[:]) for c in range(8)][-1],
                              r=["onb", "k0"], w=[("ps", bk)])
                            C("act", lambda e, half=half, bk=bk, h=h: e.activation(out=onT[:, h, half * 1024:(half + 1) * 1024], in_=psb[:, bk, :], func=AF.Copy),
                              r=[("ps", bk)], w=[("onT", h, half)])
                    self.phase_end("A2")
                merged = sb(es_o, "merged", [128, 8, S], BF16)
                with ExitStack() as es3:
                    wg = [sb(es3, "wg%d" % i, [128, 8, 2, 128], BF16) for i in range(2)]
                    wab = [sb(es3, "wab%d" % i, [128, 8, 128], BF16) for i in range(2)]
                    wpb = [sb(es3, "wpb%d" % i, [128, 4, 128], BF16) for i in range(2)]
                    sg = [[sb(es3, "sg%d_%d" % (i, k), [128, 512], F32) for k in range(2)] for i in range(2)]
                    mt = [[sb(es3, "mt%d_%d" % (i, k), [128, 512], F32) for k in range(2)] for i in range(2)]
                    wgv = d["w_gate"].ap()[l].rearrange("(kc p) (br jj n) -> p kc br jj n", p=128, br=2, jj=8)
                    wabv = d["w_ab"].ap()[l].rearrange("(kc p) (jj n) -> p kc jj n", p=128, jj=8)
                    wpbv = d["w_pb"].ap()[l].rearrange("(kc p) (jj n) -> p kc jj n", p=128, jj=8)
                    it = 0
                    for j in range(8):
                        js = j % 2
                        for br in range(2):
                            Dq("pool", lambda e, j=j, js=js, br=br: e.dma_start(out=wg[js][:, :, br, :], in_=wgv[:, :, br, j, :]), w=[("wg", js, br)])
                        Dq("pool", lambda e, j=j, js=js: e.dma_start(out=wab[js][:], in_=wabv[:, :, j, :]), w=[("wab", js)])
                        Dq("pool", lambda e, j=j, js=js: e.dma_start(out=wpb[js][:], in_=wpbv[:, :, j, :]), w=[("wpb", js)])
                        for tc in range(4):
                            s2 = it % 2
                            it += 1
                            b0 = s2 * 4
                            tsl = slice(tc * 512, (tc + 1) * 512)
                            C("pe", lambda e, js=js, tsl=tsl, b0=b0: [e.matmul(out=ps[:, b0, :], lhsT=wpb[js][:, g, :], rhs=ypre[:, g, tsl],
                                                                              start=(g == 0), stop=(g == 3)) for g in range(4)][-1],
                              r=[("wpb", js)], w=[("ps", b0)])
                            C("pe", lambda e, js=js, tsl=tsl, b0=b0: [e.matmul(out=ps[:, b0 + 1, :], lhsT=wab[js][:, c, :], rhs=onT[:, c, tsl],
                                                                              start=(c == 0), stop=(c == 7)) for c in range(8)][-1],
                              r=[("wab", js)], w=[("ps", b0 + 1)])
                            for br in range(2):
                                C("pe", lambda e, js=js, tsl=tsl, b0=b0, br=br: [e.matmul(out=ps[:, b0 + 2 + br, :], lhsT=wg[js][:, kc, br, :], rhs=xT[:, kc, tsl],
                                                                                         start=(kc == 0), stop=(kc == 7)) for kc in range(8)][-1],
                                  r=[("wg", js, br)], w=[("ps", b0 + 2 + br)])
                                C("act", lambda e, s2=s2, b0=b0, br=br: e.activation(out=sg[s2][br][:], in_=ps[:, b0 + 2 + br, :], func=AF.Sigmoid),
                                  r=[("ps", b0 + 2 + br)], w=[("sg", s2, br)])
                                C("dve", lambda e, s2=s2, b0=b0, br=br: e.tensor_tensor(out=mt[s2][br][:], in0=sg[s2][br][:], in1=ps[:, b0 + br, :], op=ALU.mult),
                                  r=[("sg", s2, br), ("ps", b0 + br)], w=[("mt", s2, br)])
                            C("pool", lambda e, s2=s2, j=j, tsl=tsl: e.tensor_tensor(out=merged[:, j, tsl], in0=mt[s2][0][:], in1=mt[s2][1][:], op=ALU.add),
                              r=[("mt", s2, 0), ("mt", s2, 1)], w=[("merged", j, tsl.start)])
                    self.phase_end("A3a")
                with ExitStack() as es4:
                    wout = sb(es4, "wout", [128, 8, D], BF16)
                    wr = sb(es4, "wr", [128, 8, E], F32)
                    brb = sb(es4, "brb", [128, E], F32)
                    gb = sb(es4, "g1b", [128, D], F32)
                    bb = sb(es4, "b1b", [128, D], F32)
                    Dq("pool", lambda e: e.dma_start(out=wout[:], in_=d["w_out"].ap()[l].rearrange("(kc p) n -> p kc n", p=128)), w=["wout"])
                    Dq("sp", lambda e: e.dma_start(out=wr[:], in_=d["w_router"].ap()[l].rearrange("(kc p) n -> p kc n", p=128)), w=["wr"])
                    Dq("sp", lambda e: e.dma_start(out=brb[:], in_=d["b_router"].ap()[l:l + 1].to_broadcast([128, E])), w=["brb"])
                    Dq("sp", lambda e: e.dma_start(out=gb[:], in_=d["ln1_g"].ap()[l:l + 1].to_broadcast([128, D])), w=["lnp"])
                    Dq("sp", lambda e: e.dma_start(out=bb[:], in_=d["ln1_b"].ap()[l:l + 1].to_broadcast([128, D])), w=["lnp"])
                    xin = [sb(es4, "xin%d" % i, [128, D], F32) for i in range(2)]
                    zt = [sb(es4, "zt%d" % i, [128, D], F32) for i in range(2)]
                    x1t = [sb(es4, "x1t%d" % i, [128, D], F32) for i in range(2)]
                    x1b = [sb(es4, "x1b%d" % i, [128, D], BF16) for i in range(2)]
                    x1T = [sb(es4, "x1T%d" % i, [128, 8, 128], F32) for i in range(2)]
                    lnt = [(sb(es4, "st%d" % i, [128, 2, 6], F32), sb(es4, "mv%d" % i, [128, 2], F32), sb(es4, "sd%d" % i, [128, 1], F32),
                            sb(es4, "rstd%d" % i, [128, 1], F32), sb(es4, "xn%d" % i, [128, D], F32)) for i in range(2)]
                    Lt = [sb(es4, "Lt%d" % i, [128, E], F32) for i in range(2)]
                    mx8 = [sb(es4, "mx8%d" % i, [128, 8], F32) for i in range(2)]
                    Mk = [sb(es4, "Mk%d" % i, [128, E], F32) for i in range(2)]
                    nv0 = [sb(es4, "nv0%d" % i, [128, 1], F32) for i in range(2)]
                    ek = [sb(es4, "ek%d" % i, [128, 4], F32) for i in range(2)]
                    gs = [sb(es4, "gs%d" % i, [128, 1], F32) for i in range(2)]
                    posc = [sb(es4, "posc%d" % i, [128, 1, E], F32) for i in range(2)]
                    oh = [sb(es4, "oh%d" % i, [128, 4, E], F32) for i in range(2)]
                    dstf = [sb(es4, "dstf%d" % i, [128, 4], F32) for i in range(2)]
                    for tt in range(16):
                        i2 = tt % 2
                        ti = b * 16 + tt
                        tg = "n%d_" % i2
                        rows = slice(tok0 + tt * 128, tok0 + (tt + 1) * 128)
                        Dq("sp", lambda e, i2=i2, rows=rows: e.dma_start(out=xin[i2][:], in_=xsrc.ap()[rows, :]), w=[("xin", i2)])
                        for n in range(2):
                            C("pe", lambda e, tt=tt, n=n, i2=i2: [e.matmul(out=ps[:, i2 * 2 + n, :], lhsT=merged[:, j, tt * 128:(tt + 1) * 128],
                                                                         rhs=wout[:, j, n * 512:(n + 1) * 512], start=(j == 0), stop=(j == 7)) for j in range(8)][-1],
                              r=["wout"], w=[("ps", i2 * 2 + n)])
                            C("dve", lambda e, n=n, i2=i2: e.scalar_tensor_tensor(out=zt[i2][:, n * 512:(n + 1) * 512], in0=xin[i2][:, n * 512:(n + 1) * 512], scalar=ALPHA,
                                                                                  in1=ps[:, i2 * 2 + n, :], op0=ALU.mult, op1=ALU.add),
                              r=[("xin", i2), ("ps", i2 * 2 + n)], w=[(tg + "z", n)])
                        self.layer_norm_tile(lnt[i2], zt[i2], gb, bb, x1t[i2][:], [(tg + "z", 0), (tg + "z", 1)], ("x1t", i2), tg)
                        Dq("sp", lambda e, i2=i2, rows=rows: e.dma_start(out=d["xres"].ap()[rows, :], in_=x1t[i2][:]), r=[("x1t", i2)], w=[("xres", ti)])
                        if self.debug:
                            Dq("sp", lambda e, i2=i2, rows=rows: e.dma_start(out=d["dbg_x1"].ap()[rows, :], in_=x1t[i2][:]), r=[("x1t", i2)], w=[("dbgx1", ti)])
                        C("act", lambda e, i2=i2: e.activation(out=x1b[i2][:], in_=x1t[i2][:], func=AF.Copy), r=[("x1t", i2)], w=[("x1b", i2)])
                        for hf in range(2):
                            bk = 4 + hf
                            C("pe", lambda e, i2=i2, hf=hf, bk=bk: [e.transpose(out=ps[:, bk, c * 128:(c + 1) * 128], in_=x1t[i2][:, (hf * 4 + c) * 128:(hf * 4 + c + 1) * 128],
                                                                               identity=self.identf[:]) for c in range(4)][-1],
                              r=[("x1t", i2), "k1"], w=[("ps", bk)])
                            C("act", lambda e, i2=i2, hf=hf, bk=bk: e.activation(out=x1T[i2][:, hf * 4:(hf + 1) * 4, :], in_=ps[:, bk, :].rearrange("p (c t) -> p c t", c=4),
                                                                                 func=AF.Copy),
                              r=[("ps", bk)], w=[("x1T", i2, hf)])
                        C("pe", lambda e, i2=i2: [e.matmul(out=ps[:, 6, 0:E], lhsT=x1T[i2][:, kc, :], rhs=wr[:, kc, :], start=(kc == 0), stop=(kc == 7)) for kc in range(8)][-1],
                          r=[("x1T", i2, 0), ("x1T", i2, 1), "wr"], w=[("ps", 6)])
                        C("dve", lambda e, i2=i2: e.tensor_tensor(out=Lt[i2][:], in0=ps[:, 6, 0:E], in1=brb[:], op=ALU.add), r=[("ps", 6), "brb"], w=[tg + "L"])
                        C("dve", lambda e, i2=i2: e.max(out=mx8[i2][:], in_=Lt[i2][:]), r=[tg + "L"], w=[tg + "mx"])
                        C("dve", lambda e, i2=i2: e.tensor_scalar(out=Mk[i2][:], in0=Lt[i2][:], scalar1=mx8[i2][:, 3:4], scalar2=None, op0=ALU.is_ge),
                          r=[tg + "L", tg + "mx"], w=[tg + "Mk"])
                        C("dve", lambda e, i2=i2: e.tensor_scalar(out=nv0[i2][:], in0=mx8[i2][:, 0:1], scalar1=-1.0, scalar2=None, op0=ALU.mult),
                          r=[tg + "mx"], w=[tg + "nv"])
                        C("act", lambda e, i2=i2: e.activation(out=ek[i2][:], in_=mx8[i2][:, 0:4], func=AF.Exp, bias=nv0[i2][:], scale=1.0, accum_out=gs[i2][:]),
                          r=[tg + "mx", tg + "nv"], w=[tg + "ek"])
                        C("dve", lambda e, i2=i2: e.reciprocal(out=gs[i2][:], in_=gs[i2][:]), r=[tg + "ek"], w=[tg + "gs"])
                        C("dve", lambda e, i2=i2, ti=ti: e.tensor_scalar(out=self.gate[:, ti, :], in0=ek[i2][:], scalar1=gs[i2][:], scalar2=None, op0=ALU.mult),
                          r=[tg + "ek", tg + "gs"], w=[("gate", ti)])
                        C("pe", lambda e, i2=i2: [e.matmul(out=ps[:, 7, 0:E], lhsT=self.ustrict[:], rhs=Mk[i2][:], start=True, stop=False),
                                                  e.matmul(out=ps[:, 7, 0:E], lhsT=self.ones[:], rhs=self.macc[:], start=False, stop=True)][-1],
                          r=[tg + "Mk", "macc", "k3", "k4"], w=[("ps", 7)])
                        C("dve", lambda e, i2=i2: e.scalar_tensor_tensor(out=posc[i2][:, 0, :], in0=ps[:, 7, 0:E], scalar=float(CAP - 1), in1=self.ec[:],
                                                                         op0=ALU.min, op1=ALU.add),
                          r=[("ps", 7), "k5"], w=[tg + "pc"])
                        C("dve", lambda e, i2=i2: e.tensor_tensor(out=self.macc[:], in0=self.macc[:], in1=Mk[i2][:], op=ALU.add), r=["macc", tg + "Mk"], w=["macc"])
                        C("dve", lambda e, i2=i2: [e.tensor_scalar(out=oh[i2][:, k, :], in0=Lt[i2][:], scalar1=mx8[i2][:, k:k + 1], scalar2=None, op0=ALU.is_equal)
                                                   for k in range(4)][-1],
                          r=[tg + "L", tg + "mx"], w=[tg + "oh"])
                        C("dve", lambda e, i2=i2: e.tensor_tensor(out=oh[i2][:], in0=oh[i2][:], in1=posc[i2][:].to_broadcast([128, 4, E]), op=ALU.mult),
                          r=[tg + "oh", tg + "pc"], w=[tg + "oh"])
                        C("dve", lambda e, i2=i2: e.tensor_reduce(out=dstf[i2][:], in_=oh[i2][:], axis=AX.X, op=ALU.add), r=[tg + "oh"], w=[tg + "df"])
                        C("dve", lambda e, i2=i2, ti=ti: e.tensor_copy(out=self.desti[:, ti, :], in_=dstf[i2][:]), r=[tg + "df"], w=[("desti", ti)])
                        for k in range(4):
                            Dq("pool", lambda e, i2=i2, ti=ti, k=k: e.indirect_dma_start(
                                out=d["xsbuf"].ap(), out_offset=bass.IndirectOffsetOnAxis(ap=self.desti[:, ti, k:k + 1], axis=0),
                                in_=x1b[i2][:], in_offset=None),
                               r=[("x1b", i2), ("desti", ti)], w=[("xsd", ti, k)])
                    self.phase_end("A3b")

    def moe(self, l):
        nc, d, C, Dq, sb = self.nc, self.d, self.C, self.Dq, self.sb
        ps, psb = self.ps, self.psb
        with ExitStack() as es:
            wgu = [sb(es, "wgu%d" % i, [128, 8, 2 * D], BF16) for i in range(2)]
            wdn = [sb(es, "wdn%d" % i, [128, 8, D], BF16) for i in range(2)]
            xs = [sb(es, "xs%d" % i, [128, NR, D], BF16) for i in range(2)]
            xsT = [sb(es, "xsT%d" % i, [128, 8, CAP], BF16) for i in range(2)]
            actT = sb(es, "actT", [128, 8, CAP], BF16)
            hg = [sb(es, "hg%d" % i, [128, 2, CAP // 2], F32) for i in range(2)]
            hu = [sb(es, "hu%d" % i, [128, 2, CAP // 2], F32) for i in range(2)]
            sgm = [sb(es, "sgm%d" % i, [128, 2, CAP // 2], F32) for i in range(2)]
            tt_ = [sb(es, "tt%d" % i, [128, 2, CAP // 2], F32) for i in range(2)]
            yst = [sb(es, "yst%d" % i, [128, D], F32) for i in range(2)]
            bdn = [sb(es, "bdn%d" % i, [128, D], F32) for i in range(2)]
            bgu = sb(es, "bgu", [128, E, 16], F32)
            Dq("sp", lambda e: e.dma_start(out=bgu[:], in_=d["b_gu"].ap()[l]), w=["bgu"])
            C("dve", lambda e: e.tensor_scalar(out=bgu[:, :, 8:16], in0=bgu[:, :, 8:16], scalar1=1.0, scalar2=None, op0=ALU.add), r=["bgu"], w=["bgu"])
            H = CAP // 2

            def load_w(e_):
                s = e_ % 2
                gv = d["w_gu"].ap()[l, e_].rearrange("(kc p) n -> p kc n", p=128)
                dv = d["w_down"].ap()[l, e_].rearrange("(kc p) n -> p kc n", p=128)
                for q in range(4):
                    Dq("pool", lambda e, s=s, q=q, gv=gv: e.dma_start(out=wgu[s][:, 2 * q:2 * q + 2, :], in_=gv[:, 2 * q:2 * q + 2, :]), w=[("wgu", s, q)])
                for q in range(2):
                    Dq("pool", lambda e, s=s, q=q, dv=dv: e.dma_start(out=wdn[s][:, 4 * q:4 * q + 4, :], in_=dv[:, 4 * q:4 * q + 4, :]), w=[("wdn", s, q)])
                Dq("sp", lambda e, s=s, e_=e_: e.dma_start(out=bdn[s][:], in_=d["b_down"].ap()[l, e_:e_ + 1].to_broadcast([128, D])), w=[("bdn", s)])

            def load_xs(e_):
                xb_ = e_ % 2
                Dq("sp", lambda e, e_=e_, xb_=xb_: e.dma_start(out=xs[xb_][:], in_=d["xsbuf"].ap()[e_ * CAP:(e_ + 1) * CAP, :].rearrange("(r p) n -> p r n", p=128)),
                   w=[("xs", xb_)])

            def transposes(e_):
                xb_ = e_ % 2
                for r in range(NR):
                    bk = 6 + r % 2
                    C("pe", lambda e, r=r, bk=bk, xb_=xb_: [e.transpose(out=psb[:, bk, c * 128:(c + 1) * 128], in_=xs[xb_][:, r, c * 128:(c + 1) * 128], identity=self.identb[:])
                                                           for c in range(8)][-1],
                      r=[("xs", xb_), "k0"], w=[("ps", bk)])
                    C("act", lambda e, r=r, bk=bk, xb_=xb_: e.activation(out=xsT[xb_][:, :, r * 128:(r + 1) * 128], in_=psb[:, bk, :].rearrange("p (c t) -> p c t", c=8), func=AF.Copy),
                      r=[("ps", bk)], w=[("xsT", xb_, r)])

            load_w(0)
            load_xs(0)
            transposes(0)
            cnt = 0
            for e_ in range(E):
                s = e_ % 2
                xb = e_ % 2
                if e_ + 1 < E:
                    load_w(e_ + 1)
                    load_xs(e_ + 1)
                xk = [("xsT", xb, r) for r in range(NR)]
                for c in range(8):
                    i2 = cnt % 2
                    cnt += 1
                    hb = (c % 2) * 4
                    for gu in range(2):
                        col0 = gu * D + c * 128
                        for hf in range(2):
                            bk = hb + gu * 2 + hf
                            C("pe", lambda e, s=s, col0=col0, hf=hf, bk=bk, xb=xb: [e.matmul(out=ps[:, bk, 0:H], lhsT=wgu[s][:, kc, col0:col0 + 128],
                                                                                    rhs=xsT[xb][:, kc, hf * H:(hf + 1) * H], start=(kc == 0), stop=(kc == 7)) for kc in range(8)][-1],
                              r=xk + [("wgu", s, q) for q in range(4)], w=[("ps", bk)])
                    C("dve", lambda e, i2=i2, e_=e_, c=c, hb=hb: e.tensor_scalar(out=hg[i2][:], in0=ps[:, hb:hb + 2, 0:H], scalar1=bgu[:, e_, c:c + 1], scalar2=SW_LIM,
                                                                         op0=ALU.add, op1=ALU.min),
                      r=[("ps", hb), ("ps", hb + 1), "bgu"], w=[("hg", i2)])
                    C("dve", lambda e, i2=i2, e_=e_, c=c, hb=hb: e.tensor_scalar(out=hu[i2][:], in0=ps[:, hb + 2:hb + 4, 0:H], scalar1=bgu[:, e_, 8 + c:9 + c], scalar2=SW_LIM + 1.0,
                                                                         op0=ALU.add, op1=ALU.min),
                      r=[("ps", hb + 2), ("ps", hb + 3), "bgu"], w=[("hu", i2)])
                    C("act", lambda e, i2=i2: e.activation(out=sgm[i2][:], in_=hg[i2][:], func=AF.Sigmoid, scale=SW_ALPHA), r=[("hg", i2)], w=[("sgm", i2)])
                    C("dve", lambda e, i2=i2: e.scalar_tensor_tensor(out=tt_[i2][:], in0=hu[i2][:], scalar=-SW_LIM + 1.0, in1=hg[i2][:], op0=ALU.max, op1=ALU.mult),
                      r=[("hu", i2), ("hg", i2)], w=[("tt", i2)])
                    C("pool", lambda e, i2=i2, c=c: e.tensor_tensor(out=actT[:, c, :].rearrange("p (a n) -> p a n", a=2), in0=tt_[i2][:], in1=sgm[i2][:], op=ALU.mult),
                      r=[("tt", i2), ("sgm", i2)], w=[("actT", c)])
                if e_ + 1 < E:
                    transposes(e_ + 1)
                ak = [("actT", c) for c in range(8)]
                for r in range(NR):
                    ys = r % 2
                    for n in range(2):
                        bk = 4 + n
                        C("pe", lambda e, s=s, r=r, n=n, bk=bk: [e.matmul(out=ps[:, bk, :], lhsT=actT[:, c, r * 128:(r + 1) * 128], rhs=wdn[s][:, c, n * 512:(n + 1) * 512],
                                                                         start=(c == 0), stop=(c == 7)) for c in range(8)][-1],
                          r=ak + [("wdn", s, 0), ("wdn", s, 1)], w=[("ps", bk)])
                        C("dve", lambda e, s=s, ys=ys, n=n, bk=bk: e.tensor_tensor(out=yst[ys][:, n * 512:(n + 1) * 512], in0=ps[:, bk, :], in1=bdn[s][:, n * 512:(n + 1) * 512], op=ALU.add),
                          r=[("ps", bk), ("bdn", s)], w=[("yst", ys, n)])
                    Dq("sp", lambda e, ys=ys, e_=e_, r=r: e.dma_start(out=d["ybuf"].ap()[e_ * CAP + r * 128: e_ * CAP + (r + 1) * 128, :], in_=yst[ys][:]),
                       r=[("yst", ys, 0), ("yst", ys, 1)], w=[("ybuf", e_, r)])
            self.phase_end("moe")

    def combine(self, l, xdst):
        nc, d, C, Dq, sb = self.nc, self.d, self.C, self.Dq, self.sb
        with ExitStack() as es:
            gb = sb(es, "g2b", [128, D], F32)
            bb = sb(es, "b2b", [128, D], F32)
            Dq("sp", lambda e: e.dma_start(out=gb[:], in_=d["ln2_g"].ap()[l:l + 1].to_broadcast([128, D])), w=["lnp"])
            Dq("sp", lambda e: e.dma_start(out=bb[:], in_=d["ln2_b"].ap()[l:l + 1].to_broadcast([128, D])), w=["lnp"])
            yk = [[sb(es, "yk%d_%d" % (i, k), [128, D], F32) for k in range(4)] for i in range(2)]
            x1 = [sb(es, "cx1%d" % i, [128, D], F32) for i in range(2)]
            acc = [sb(es, "acc%d" % i, [128, D], F32) for i in range(2)]
            zt = [sb(es, "cz%d" % i, [128, D], F32) for i in range(2)]
            ot = [sb(es, "cot%d" % i, [128, D], F32) for i in range(2)]
            lnt = [(sb(es, "cst%d" % i, [128, 2, 6], F32), sb(es, "cmv%d" % i, [128, 2], F32), sb(es, "csd%d" % i, [128, 1], F32),
                    sb(es, "crstd%d" % i, [128, 1], F32), sb(es, "cxn%d" % i, [128, D], F32)) for i in range(2)]
            for ti in range(NT):
                i2 = ti % 2
                tg = "c%d_" % i2
                rows = slice(ti * 128, (ti + 1) * 128)
                for k in range(4):
                    gn = ti * 4 + k
                    Dq("pool", lambda e, i2=i2, ti=ti, k=k: e.indirect_dma_start(
                        out=yk[i2][k][:], out_offset=None, in_=d["ybuf"].ap(),
                        in_offset=bass.IndirectOffsetOnAxis(ap=self.desti[:, ti, k:k + 1], axis=0)),
                       r=[("gch", gn - 2)], w=[("yk", i2, k), ("gch", gn)])
                import os
                cut = os.environ.get("COMBINE_CUT", "full")
                if cut == "gather":
                    C("dve", lambda e, i2=i2: e.tensor_copy(out=ot[i2][:], in_=yk[i2][0][:]), r=[("yk", i2, 0)], w=[("cot", i2)])
                    for k in range(1, 4):
                        C("dve", lambda e, i2=i2, k=k: e.tensor_tensor(out=ot[i2][:], in0=ot[i2][:], in1=yk[i2][k][:], op=ALU.add), r=[("yk", i2, k), ("cot", i2)], w=[("cot", i2)])
                    Dq("sp", lambda e, i2=i2, rows=rows: e.dma_start(out=xdst.ap()[rows, :], in_=ot[i2][:]), r=[("cot", i2)], w=[("xdst", ti)])
                    continue
                Dq("sp", lambda e, i2=i2, rows=rows: e.dma_start(out=x1[i2][:], in_=d["xres"].ap()[rows, :]), w=[("cx1", i2)])
                C("dve", lambda e, i2=i2, ti=ti: e.tensor_scalar(out=acc[i2][:], in0=yk[i2][0][:], scalar1=self.gate[:, ti, 0:1], scalar2=None, op0=ALU.mult),
                  r=[("yk", i2, 0)], w=[("acc", i2)])
                for k in range(1, 4):
                    C("dve", lambda e, i2=i2, ti=ti, k=k: e.scalar_tensor_tensor(out=acc[i2][:], in0=yk[i2][k][:], scalar=self.gate[:, ti, k:k + 1], in1=acc[i2][:],
                                                                                 op0=ALU.mult, op1=ALU.add),
                      r=[("yk", i2, k), ("acc", i2)], w=[("acc", i2)])
                C("dve", lambda e, i2=i2: e.scalar_tensor_tensor(out=zt[i2][:], in0=x1[i2][:], scalar=ALPHA, in1=acc[i2][:], op0=ALU.mult, op1=ALU.add),
                  r=[("cx1", i2), ("acc", i2)], w=[tg + "zz"])
                if cut == "acc":
                    Dq("sp", lambda e, i2=i2, rows=rows: e.dma_start(out=xdst.ap()[rows, :], in_=zt[i2][:]), r=[tg + "zz"], w=[("xdst", ti)])
                    continue
                self.layer_norm_tile(lnt[i2], zt[i2], gb, bb, ot[i2][:], [tg + "zz"], ("cot", i2), tg)
                Dq("sp", lambda e, i2=i2, rows=rows: e.dma_start(out=xdst.ap()[rows, :], in_=ot[i2][:]), r=[("cot", i2)], w=[("xdst", ti)])
            self.phase_end("combine")


def _consts():
    c = {}
    c["c_identb"] = np.eye(128, dtype=np.float32).astype(ml_dtypes.bfloat16)
    c["c_identf"] = np.eye(128, dtype=np.float32)
    k = np.arange(128)[:, None]
    q = np.arange(128)[None, :]
    c["c_mask01"] = (q >= k).astype(np.float32).astype(ml_dtypes.bfloat16)
    c["c_ustrict"] = (k < q).astype(np.float32)
    c["c_ones"] = np.ones((128, 128), np.float32)
    c["c_ec"] = np.broadcast_to((np.arange(E, dtype=np.float32) * CAP)[None, :], (128, E)).copy()
    inv = (ROPE_THETA ** (-np.arange(0, 16, 2, dtype=np.float32) / np.float32(16))).astype(np.float32)
    c["c_invfreq"] = np.broadcast_to(inv[None, :], (128, 8)).copy()
    ic = np.zeros((4, 16), np.float32)
    for g, w in enumerate((2, 4, 8, 16)):
        ic[g] = 1.0 / np.minimum(np.arange(1, 17), w).astype(np.float32)
    c["c_invcnt"] = np.broadcast_to(ic[None], (128, 4, 16)).copy()
    return c


def _layout(inp):
    f = lambda a: np.ascontiguousarray(np.asarray(a))
    w_in = f(inp["w_in"])
    sh = {}
    sh["w_pool"] = f(w_in[:, :, 0:512])
    q = w_in[:, :, 512:1536].reshape(L, D, NH, 128)
    k = w_in[:, :, 1536:2560].reshape(L, D, NH, 128)
    v = w_in[:, :, 2560:3584].reshape(L, D, NH, 128)
    sh["w_qkv"] = f(np.concatenate([q, k, v], axis=-1).transpose(0, 2, 1, 3))
    sh["w_gate"] = f(w_in[:, :, 3584:5632])
    sh["pool_w"] = f(inp["pool_w"])
    sh["pool_scale"] = f(np.asarray(inp["pool_scale"]).reshape(L, 4, 128).transpose(0, 2, 1))
    sh["w_pb"] = f(inp["w_pool_branch"])
    sh["w_ab"] = f(inp["w_attn_branch"])
    sh["lam4"] = f(np.stack([np.asarray(inp["lambda_q1"]), np.asarray(inp["lambda_k1"]),
                             np.asarray(inp["lambda_q2"]), np.asarray(inp["lambda_k2"])], axis=1))
    sh["subln_w"] = f(inp["subln_w"])
    sh["w_out"] = f(inp["w_out"])
    for n in ("ln1_g", "ln1_b", "w_router", "b_router", "w_gu", "w_down", "b_down", "ln2_g", "ln2_b"):
        sh[n] = f(inp[n])
    sh["b_gu"] = f(np.asarray(inp["b_gu"]).reshape(L, E, 16, 128).transpose(0, 3, 1, 2))
    sh.update(_consts())
    return sh


_NC_CACHE = {}


def kernel(**inputs):
    shared = _layout(inputs)
    x = np.asarray(inputs["x"], dtype=np.float32).reshape(NCORES, T, D)
    pos = np.asarray(inputs["positions"]).astype(np.int32).reshape(NCORES, NSEQ, 16, 128).transpose(0, 1, 3, 2)
    if "nc" not in _NC_CACHE:
        _NC_CACHE["nc"] = Builder().build()
    nc = _NC_CACHE["nc"]
    in_maps = []
    for c in range(NCORES):
        m = dict(shared)
        m["x"] = np.ascontiguousarray(x[c])
        m["pos"] = np.ascontiguousarray(pos[c])
        in_maps.append(m)
    res = run_bass_kernel_spmd(nc, in_maps, core_ids=list(range(NCORES)))
    out = np.stack([np.asarray(r["y"]) for r in res.results], axis=0)
    return out.reshape(16, S, D).astype(np.float32)
```
